# Optimizing a Trainium2 kernel written in Bass

```python
import jax
import jax.numpy as jnp
from jax import lax
import numpy as np

D_MODEL = 1024
BATCH = 8
SEQ = 4096
DEPTH = 2

NORM_EPS = 1e-6
MASK_VALUE = -1e30
MIN_GATE = 1e-30
HG_HEADS = 8
HG_DK = 128
HG_DV = 128
HG_WIDTH = HG_HEADS * HG_DK
HG_CHUNK = 32
RW_HEAD_SIZE = 64
RW_HEADS = 16
RW_WIDTH = RW_HEADS * RW_HEAD_SIZE
RW_LORA_W = 64
RW_LORA_A = 64
RW_LORA_G = 128
RW_LORA_V = 32
RW_LNX_EPS = 1e-5 * RW_HEAD_SIZE
RW_IN = 3 * RW_WIDTH + RW_LORA_W + RW_LORA_A + RW_LORA_G
MLA_HEADS = 8
MLA_Q_RANK = 384
MLA_KV_RANK = 256
MLA_NOPE = 128
MLA_ROPE = 64
MLA_V = 128
MLA_WIDTH = MLA_HEADS * MLA_V
ROPE_THETA = 10000.0
Q_BLOCK = 128
N_BRANCH = 3
D_FF = ((-(-8 * D_MODEL // 3) + 255) // 256) * 256
IN_SIZES = (HG_WIDTH, HG_WIDTH, HG_WIDTH, HG_WIDTH, RW_IN, MLA_Q_RANK, MLA_KV_RANK, MLA_ROPE, N_BRANCH * D_MODEL)
IN_WIDTH = sum(IN_SIZES)
IN_SPLITS = tuple(int(c) for c in np.cumsum(IN_SIZES))
RW_SPLITS = tuple(int(c) for c in np.cumsum((RW_WIDTH, RW_WIDTH, RW_WIDTH, RW_LORA_W, RW_LORA_A)))

kernel_name = 'hybrid_hgrn2_rwkv7_mla_gated_block'


def rms_norm(x, g):
    xf = x.astype(jnp.float32)
    y = xf * lax.rsqrt(jnp.mean(xf * xf, axis=-1, keepdims=True) + NORM_EPS)
    return (y * g.astype(jnp.float32)).astype(x.dtype)


def token_shift(z, mu):
    z_prev = jnp.pad(z, ((0, 0), (1, 0), (0, 0)))[:, :-1]
    return z + (z_prev - z) * mu


def rope_angles(positions):
    inv_freq = ROPE_THETA ** (-jnp.arange(0, MLA_ROPE, 2, dtype=jnp.float32) / MLA_ROPE)
    ang = positions.astype(jnp.float32)[..., None] * inv_freq
    return jnp.cos(ang), jnp.sin(ang)


def apply_rope(x, cos, sin):
    x1, x2 = jnp.split(x, 2, axis=-1)
    return jnp.concatenate([x1 * cos - x2 * sin, x1 * sin + x2 * cos], axis=-1).astype(x.dtype)


def hgrn2_mixer(zq, zf, zi, zog, lower_bound, onorm_g):
    B, S, _ = zq.shape
    nc = S // HG_CHUNK
    zf32 = zf.astype(jnp.float32)
    f = lower_bound + (1.0 - lower_bound) * jax.nn.sigmoid(zf32)
    log_f = jnp.log(jnp.maximum(f, MIN_GATE))
    k = (1.0 - lower_bound) * jax.nn.sigmoid(-zf32)

    def chunks(t, d):
        return t.astype(jnp.float32).reshape(B, nc, HG_CHUNK, HG_HEADS, d).transpose(1, 0, 3, 2, 4)

    causal = jnp.tril(jnp.ones((HG_CHUNK, HG_CHUNK), dtype=bool))[:, :, None]

    def chunk_step(state, inp):
        q, kc, v, lf = inp
        b = jnp.cumsum(lf, axis=2)
        diff = b[:, :, :, None, :] - b[:, :, None, :, :]
        rel = jnp.where(causal, jnp.exp(jnp.where(causal, diff, 0.0)), 0.0)
        scores = jnp.einsum('bhtd,bhsd,bhtsd->bhts', q, kc, rel)
        o = jnp.einsum('bhts,bhsv->bhtv', scores, v) + jnp.einsum('bhtd,bhdv->bhtv', q * jnp.exp(b), state)
        b_last = b[:, :, -1:, :]
        state = jnp.exp(b_last[:, :, 0, :, None]) * state + jnp.einsum('bhsd,bhsv->bhdv', kc * jnp.exp(b_last - b), v)
        return state, o

    state0 = jnp.zeros((B, HG_HEADS, HG_DK, HG_DV), jnp.float32)
    _, o = lax.scan(chunk_step, state0, (chunks(zq, HG_DK), chunks(k, HG_DK), chunks(zi, HG_DV), chunks(log_f, HG_DK)))
    o = o.transpose(1, 0, 3, 2, 4).reshape(B, S, HG_HEADS, HG_DV)
    o = rms_norm(o, onorm_g).reshape(B, S, HG_HEADS * HG_DV)
    return (o * jax.nn.silu(zog.astype(jnp.float32))).astype(zq.dtype)


def rwkv7_mixer(zrw, mu, w0, w_up, a0, a_up, g_up, k_k, k_a, r_k, lnx_g, lnx_b, v_first, vres):
    B, S, _ = zrw.shape
    zs = token_shift(zrw, mu).astype(jnp.float32)
    r, k, v, zw, za, zg = jnp.split(zs, RW_SPLITS, axis=-1)
    w_log = -jax.nn.softplus(-(w0 + jnp.tanh(zw) @ w_up)) - 0.5
    decay = jnp.exp(-jnp.exp(w_log))
    a = jax.nn.sigmoid(a0 + za @ a_up)
    g = jax.nn.sigmoid(zg) @ g_up
    if vres is None:
        v_first = v
    else:
        zv, v0, v_up = vres
        v = v + (v_first - v) * jax.nn.sigmoid(v0 + zv @ v_up)

    def heads(t):
        return t.reshape(B, S, RW_HEADS, RW_HEAD_SIZE)

    kk = heads(k * k_k)
    kk = kk * lax.rsqrt(jnp.maximum(jnp.sum(kk * kk, axis=-1, keepdims=True), 1e-24))
    k = k * (1.0 + (a - 1.0) * k_a)
    rh, kh, vh, wh, ah = heads(r), heads(k), heads(v), heads(decay), heads(a)

    def step(state, inp):
        r_t, w_t, k_t, v_t, kk_t, a_t = inp
        sa = jnp.einsum('bhvk,bhk->bhv', state, -kk_t)
        state = (state * w_t[:, :, None, :] + sa[..., None] * (kk_t * a_t)[:, :, None, :]
                 + v_t[..., None] * k_t[:, :, None, :])
        return state, jnp.einsum('bhvk,bhk->bhv', state, r_t)

    state0 = jnp.zeros((B, RW_HEADS, RW_HEAD_SIZE, RW_HEAD_SIZE), jnp.float32)
    xs = tuple(t.transpose(1, 0, 2, 3) for t in (rh, wh, kh, vh, kk, ah))
    _, y = lax.scan(step, state0, xs)
    y = y.transpose(1, 0, 2, 3)
    mean = jnp.mean(y, axis=-1, keepdims=True)
    var = jnp.mean(jnp.square(y - mean), axis=-1, keepdims=True)
    y = ((y - mean) * lax.rsqrt(var + RW_LNX_EPS)).reshape(B, S, RW_WIDTH) * lnx_g + lnx_b
    bonus = jnp.sum(rh * kh * r_k.reshape(RW_HEADS, RW_HEAD_SIZE), axis=-1, keepdims=True) * vh
    y = (y + bonus.reshape(B, S, RW_WIDTH)) * g
    return y.astype(zrw.dtype), v_first


def mla_mixer(zcq, zckv, zkr, cos, sin, q_norm_g, w_uq, kv_norm_g, w_ukv):
    B, S, _ = zcq.shape
    q = (rms_norm(zcq, q_norm_g) @ w_uq).reshape(B, S, MLA_HEADS, MLA_NOPE + MLA_ROPE)
    q_nope = q[..., :MLA_NOPE]
    q_rope = apply_rope(q[..., MLA_NOPE:], cos[:, :, None, :], sin[:, :, None, :])
    kv = (rms_norm(zckv, kv_norm_g) @ w_ukv).reshape(B, S, MLA_HEADS, MLA_NOPE + MLA_V)
    k_nope = kv[..., :MLA_NOPE].transpose(0, 2, 1, 3)
    v = kv[..., MLA_NOPE:].transpose(0, 2, 1, 3)
    k_rope = apply_rope(zkr, cos, sin)
    nb = S // Q_BLOCK

    def q_blocks(t):
        return t.reshape(B, nb, Q_BLOCK, MLA_HEADS, t.shape[-1]).transpose(1, 0, 3, 2, 4)

    scale = (MLA_NOPE + MLA_ROPE) ** -0.5
    key_pos = jnp.arange(S)

    def attend_block(inp):
        qn, qr, blk = inp
        s = jnp.einsum('bhqd,bhkd->bhqk', qn, k_nope) + jnp.einsum('bhqd,bkd->bhqk', qr, k_rope)
        s = s.astype(jnp.float32) * scale
        q_pos = blk * Q_BLOCK + jnp.arange(Q_BLOCK)
        s = jnp.where(key_pos[None, :] <= q_pos[:, None], s, MASK_VALUE)
        p = jax.nn.softmax(s, axis=-1).astype(v.dtype)
        return jnp.einsum('bhqk,bhkd->bhqd', p, v)

    o = lax.map(attend_block, (q_blocks(q_nope), q_blocks(q_rope), jnp.arange(nb)))
    return o.transpose(1, 0, 3, 2, 4).reshape(B, S, MLA_WIDTH)


def setup_inputs(seed: int = 0) -> dict:
    key = jax.random.key(seed)
    ks = iter(jax.random.split(key, 48))
    L = DEPTH
    LV = DEPTH - 1
    D = D_MODEL

    def nrm(shape, scale):
        return jax.random.normal(next(ks), shape, jnp.float32) * scale

    def gain(shape):
        return 1.0 + nrm(shape, 0.05)

    x = nrm((BATCH, SEQ, D), 1.0)
    offsets = jax.random.randint(next(ks), (BATCH, 1), 0, 1024, dtype=jnp.int32)
    positions = offsets + jnp.arange(SEQ, dtype=jnp.int32)[None, :]
    return {
        'x': x,
        'positions': positions,
        'hgrn_lb_logits': nrm((L, HG_WIDTH), 0.5),
        'mix_pre_g': gain((L, D)),
        'mix_post_g': gain((L, D)),
        'ffn_pre_g': gain((L, D)),
        'ffn_post_g': gain((L, D)),
        'w_in': nrm((L, D, IN_WIDTH), D ** -0.5),
        'w_vres_down': nrm((LV, D, RW_LORA_V), D ** -0.5),
        'hgrn_onorm_g': gain((L, HG_DV)),
        'rwkv_mu': jax.random.uniform(next(ks), (L, RW_IN), jnp.float32, 0.1, 0.9),
        'rwkv_vres_mu': jax.random.uniform(next(ks), (LV, RW_LORA_V), jnp.float32, 0.1, 0.9),
        'rwkv_w0': nrm((L, RW_WIDTH), 0.5),
        'rwkv_w_up': nrm((L, RW_LORA_W, RW_WIDTH), 0.1),
        'rwkv_a0': nrm((L, RW_WIDTH), 0.2),
        'rwkv_a_up': nrm((L, RW_LORA_A, RW_WIDTH), 0.5 * RW_LORA_A ** -0.5),
        'rwkv_g_up': nrm((L, RW_LORA_G, RW_WIDTH), RW_LORA_G ** -0.5),
        'rwkv_v0': nrm((LV, RW_WIDTH), 0.2),
        'rwkv_v_up': nrm((LV, RW_LORA_V, RW_WIDTH), 0.5 * RW_LORA_V ** -0.5),
        'rwkv_k_k': 0.85 + nrm((L, RW_WIDTH), 0.05),
        'rwkv_k_a': 1.0 + nrm((L, RW_WIDTH), 0.05),
        'rwkv_r_k': nrm((L, RW_WIDTH), 0.1),
        'rwkv_lnx_g': gain((L, RW_WIDTH)),
        'rwkv_lnx_b': nrm((L, RW_WIDTH), 0.02),
        'mla_q_norm_g': gain((L, MLA_Q_RANK)),
        'mla_w_uq': nrm((L, MLA_Q_RANK, MLA_HEADS * (MLA_NOPE + MLA_ROPE)), MLA_Q_RANK ** -0.5),
        'mla_kv_norm_g': gain((L, MLA_KV_RANK)),
        'mla_w_ukv': nrm((L, MLA_KV_RANK, MLA_HEADS * (MLA_NOPE + MLA_V)), MLA_KV_RANK ** -0.5),
        'w_branch': nrm((L, N_BRANCH, HG_WIDTH, D), HG_WIDTH ** -0.5),
        'w_out': nrm((L, D, D), D ** -0.5),
        'w_ffn_in': nrm((L, D, 2 * D_FF), D ** -0.5),
        'w_ffn_out': nrm((L, D_FF, D), D_FF ** -0.5),
    }


def reference(x, positions, hgrn_lb_logits, mix_pre_g, mix_post_g, ffn_pre_g, ffn_post_g, w_in, w_vres_down,
              hgrn_onorm_g, rwkv_mu, rwkv_vres_mu, rwkv_w0, rwkv_w_up, rwkv_a0, rwkv_a_up, rwkv_g_up, rwkv_v0,
              rwkv_v_up, rwkv_k_k, rwkv_k_a, rwkv_r_k, rwkv_lnx_g, rwkv_lnx_b, mla_q_norm_g, mla_w_uq,
              mla_kv_norm_g, mla_w_ukv, w_branch, w_out, w_ffn_in, w_ffn_out):
    probs = jax.nn.softmax(hgrn_lb_logits.astype(jnp.float32), axis=0)
    lower_bounds = jnp.cumsum(probs, axis=0) - probs[0]
    cos, sin = rope_angles(positions)
    v_first = None
    for l in range(DEPTH):
        h = rms_norm(x, mix_pre_g[l])
        w_cols = w_in[l] if l == 0 else jnp.concatenate([w_in[l], w_vres_down[l - 1]], axis=1)
        z = h @ w_cols
        zq, zf, zi, zog, zrw, zcq, zckv, zkr, zgate, zvres = jnp.split(z, IN_SPLITS, axis=-1)

        o_hg = hgrn2_mixer(zq, zf, zi, zog, lower_bounds[l], hgrn_onorm_g[l])
        vres = None if l == 0 else (token_shift(zvres, rwkv_vres_mu[l - 1]), rwkv_v0[l - 1], rwkv_v_up[l - 1])
        o_rw, v_first = rwkv7_mixer(zrw, rwkv_mu[l], rwkv_w0[l], rwkv_w_up[l], rwkv_a0[l], rwkv_a_up[l],
                                    rwkv_g_up[l], rwkv_k_k[l], rwkv_k_a[l], rwkv_r_k[l], rwkv_lnx_g[l],
                                    rwkv_lnx_b[l], v_first, vres)
        o_mla = mla_mixer(zcq, zckv, zkr, cos, sin, mla_q_norm_g[l], mla_w_uq[l], mla_kv_norm_g[l], mla_w_ukv[l])

        g_hg, g_rw, g_mla = jnp.split(jax.nn.sigmoid(zgate), N_BRANCH, axis=-1)
        merged = (g_hg * (o_hg @ w_branch[l, 0]) + g_rw * (o_rw @ w_branch[l, 1])
                  + g_mla * (o_mla @ w_branch[l, 2]))
        x = x + rms_norm(merged @ w_out[l], mix_post_g[l])

        h = rms_norm(x, ffn_pre_g[l])
        gate, up = jnp.split(h @ w_ffn_in[l], 2, axis=-1)
        x = x + rms_norm((jax.nn.silu(gate) * up) @ w_ffn_out[l], ffn_post_g[l])
    return x
```

```python
import numpy as np
import concourse.bass as bass
import concourse.mybir as mybir
from concourse.bass_utils import run_bass_kernel_spmd

F32 = mybir.dt.float32
BF16 = mybir.dt.bfloat16
I32 = mybir.dt.int32
ALU = mybir.AluOpType
AF = mybir.ActivationFunctionType
AX = mybir.AxisListType

S_LEN = 4096
D = 1024
NT = S_LEN // 128
DEPTH = 2
D_FF = 2816
IN_W = 11200


class Res:
    __slots__ = ("w", "r")

    def __init__(self):
        self.w = None
        self.r = []


class Op:
    __slots__ = ("eng", "fn", "deps", "isdma", "sem", "val", "inc", "waits")

    def __init__(self, eng, fn, isdma):
        self.eng = eng
        self.fn = fn
        self.deps = set()
        self.isdma = isdma
        self.sem = None
        self.val = 0
        self.inc = False
        self.waits = []


class Tile:
    def __init__(self, t, nres):
        self.t = t
        self.res = [Res() for _ in range(nres)]

    def __getitem__(self, k):
        return self.t[k]


class Reg:
    def __init__(self, ap, off, n):
        self.ap, self.off, self.n = ap, off, n
        self.res = [Res()]

    def __getitem__(self, k):
        p, c = k
        a = 0 if c.start is None else c.start
        b = self.n if c.stop is None else c.stop
        return self.ap[p, self.off + a:self.off + b]


N_DMA_SEM = 24
N_HW_SEM = 16
COMPUTE = ("pe", "dve", "act", "pool")


class Sched:
    def __init__(self, nc):
        self.nc = nc
        self.ops = []
        self.base = set()
        self.last = {}
        self.dma_last = [None] * N_DMA_SEM
        self.dma_rr = 0
        self.dma_rr_sw = 0
        self.sb_top = 16640
        self.sb_peak = 0
        self.uid = 0

    def sb(self, shape, dtype, nres=1, name=None):
        self.uid += 1
        esz = {F32: 4, BF16: 2, I32: 4}[dtype]
        nb = int(np.prod(shape[1:])) * esz
        nb = (nb + 63) // 64 * 64
        off = self.sb_top
        self.sb_top += nb
        self.sb_peak = max(self.sb_peak, self.sb_top)
        assert self.sb_top <= 192 * 1024, ("SBUF overflow", self.sb_top)
        t = self.nc.alloc_sbuf_tensor_at(name or f"sb{self.uid}", list(shape), dtype, offset=off)
        return Tile(t, nres)

    def mark(self):
        return self.sb_top

    def release(self, mark):
        self.barrier()
        self.sb_top = mark

    def _rec(self, o, reads, writes):
        deps = set(self.base)
        for r in reads:
            if r.w is not None:
                deps.add(r.w)
        for w in writes:
            if w.w is not None:
                deps.add(w.w)
            deps.update(w.r)
        for r in reads:
            if o.isdma:
                r.r.append(o)
            else:
                r.r = [x for x in r.r if x.isdma or x.eng != o.eng]
                r.r.append(o)
        for w in writes:
            w.w = o
            w.r = []
        deps.discard(o)
        o.deps = deps
        self.ops.append(o)
        if not o.isdma:
            self.last[o.eng] = o
        return o

    def op(self, eng, fn, reads=(), writes=()):
        return self._rec(Op(eng, fn, False), reads, writes)

    def dma(self, q, out, in_, reads=(), writes=()):
        o = Op(q, lambda e: e.dma_start(out=out, in_=in_), True)
        if q == "pool":
            s = N_HW_SEM + self.dma_rr_sw
            self.dma_rr_sw = (self.dma_rr_sw + 1) % (N_DMA_SEM - N_HW_SEM)
        else:
            s = self.dma_rr
            self.dma_rr = (s + 1) % N_HW_SEM
        o.sem = s
        o = self._rec(o, reads, writes)
        if self.dma_last[s] is not None:
            o.deps.add(self.dma_last[s])
        self.dma_last[s] = o
        return o

    def barrier(self):
        b = set(self.last.values())
        b.update(d for d in self.dma_last if d is not None)
        self.base = b

    def emit(self, final_ops):
        nc = self.nc
        for o in self.ops:
            for d in o.deps:
                if d.eng == "pe" and o.eng == "pe" and not d.isdma and not o.isdma:
                    continue
                d.inc = True
        for o in final_ops:
            o.inc = True
        cnt = {e: 0 for e in COMPUTE}
        dcnt = [0] * N_DMA_SEM
        for o in self.ops:
            if o.isdma:
                dcnt[o.sem] += 16
                o.val = dcnt[o.sem]
                o.inc = True
            elif o.inc:
                cnt[o.eng] += 1
                o.val = cnt[o.eng]
        streams = {e: [] for e in ("pe", "dve", "act", "pool", "sp")}
        for o in self.ops:
            streams[o.eng].append(o)
        for e, lst in streams.items():
            have = {}
            for o in lst:
                need = {}
                for d in o.deps:
                    if d.eng == "pe" and o.eng == "pe" and not d.isdma and not o.isdma:
                        continue
                    key = ("d", d.sem) if d.isdma else ("c", d.eng)
                    if have.get(key, 0) < d.val and need.get(key, 0) < d.val:
                        need[key] = d.val
                for k, v in need.items():
                    have[k] = v
                o.waits = list(need.items())
        self.sem_max = dict(cnt)
        import contextlib
        with contextlib.ExitStack() as es:
            csem = {e: es.enter_context(nc.semaphore(f"s_{e}")) for e in COMPUTE}
            dsem = [es.enter_context(nc.semaphore(f"s_d{i}")) for i in range(N_DMA_SEM)]
            fin = es.enter_context(nc.semaphore("s_fin"))
            block = es.enter_context(nc.Block())

            def run(e, name):
                for o in streams[name]:
                    for (kind, k), v in o.waits:
                        e.wait_ge(csem[k] if kind == "c" else dsem[k], v)
                    ins = o.fn(e)
                    if o.isdma:
                        ins.then_inc(dsem[o.sem], 16)
                    elif o.inc:
                        ins.then_inc(csem[o.eng], 1)
                if name == "sp":
                    for o in final_ops:
                        if o.isdma:
                            e.wait_ge(dsem[o.sem], o.val)
                        else:
                            e.wait_ge(csem[o.eng], o.val)

            @block.tensor
            def _(e):
                run(e, "pe")

            @block.vector
            def _(e):
                run(e, "dve")

            @block.scalar
            def _(e):
                run(e, "act")

            @block.gpsimd
            def _(e):
                run(e, "pool")

            @block.sync
            def _(e):
                run(e, "sp")


VEC_SPEC = [
    ("mix_pre_g", 1024), ("mix_post_g", 1024), ("ffn_pre_g", 1024), ("ffn_post_g", 1024),
    ("lb0", 1024), ("lb1", 1024), ("onorm_g", 128), ("mu", 3328), ("vres_mu", 32),
    ("w0", 1024), ("a0", 1024), ("v0", 1024), ("k_k", 1024), ("k_a", 1024), ("r_k", 1024),
    ("lnx_g", 1024), ("lnx_b", 1024), ("q_norm_g", 384), ("kv_norm_g", 256),
]
VOFF = {}
_o = 0
for _n, _f in VEC_SPEC:
    VOFF[_n] = _o
    _o += (_f + 127) // 128
NV = _o


def pack_vecs(inp, l):
    lv = max(l - 1, 0)
    src = {
        "mix_pre_g": inp["mix_pre_g"][l], "mix_post_g": inp["mix_post_g"][l],
        "ffn_pre_g": inp["ffn_pre_g"][l], "ffn_post_g": inp["ffn_post_g"][l],
        "lb0": inp["hgrn_lb_logits"][0], "lb1": inp["hgrn_lb_logits"][1],
        "onorm_g": inp["hgrn_onorm_g"][l], "mu": inp["rwkv_mu"][l], "vres_mu": inp["rwkv_vres_mu"][lv],
        "w0": inp["rwkv_w0"][l], "a0": inp["rwkv_a0"][l], "v0": inp["rwkv_v0"][lv],
        "k_k": inp["rwkv_k_k"][l], "k_a": inp["rwkv_k_a"][l], "r_k": inp["rwkv_r_k"][l],
        "lnx_g": inp["rwkv_lnx_g"][l], "lnx_b": inp["rwkv_lnx_b"][l],
        "q_norm_g": inp["mla_q_norm_g"][l], "kv_norm_g": inp["mla_kv_norm_g"][l],
    }
    out = np.zeros((128, NV), np.float32)
    for n, f in VEC_SPEC:
        v = np.asarray(src[n], np.float32).reshape(-1)
        nch = (f + 127) // 128
        pad = np.zeros(nch * 128, np.float32)
        pad[:f] = v
        out[:, VOFF[n]:VOFF[n] + nch] = pad.reshape(nch, 128).T
    return out


N_ZC = 89


def zcols(fc):
    if fc < 63:
        return fc * 128, 128
    if fc == 63:
        return 8064, 64
    return 8128 + (fc - 64) * 128, 128


class Builder:
    def __init__(self, S_LEN=S_LEN, nlayers=DEPTH, debug=()):
        self.S = S_LEN
        self.TG = min(512, S_LEN)
        self.NTG = S_LEN // self.TG
        self.L = nlayers
        self.debug = set(debug)
        nc = bass.Bass("TRN2", target_bir_lowering=False)
        self.nc = nc
        self.sch = Sched(nc)
        S = S_LEN

        def di(name, shape, dt=F32):
            return nc.dram_tensor(name, list(shape), dt, kind="ExternalInput").ap()

        self.xT = di("xT", [D, S])
        self.pos = di("pos", [S], I32)
        self.vecs = di("vecs", [DEPTH, 128, NV])
        self.w_in = di("w_in", [DEPTH, D, IN_W])
        self.w_vres = di("w_vres", [D, 32])
        self.w_up = di("w_up", [DEPTH, 64, D])
        self.a_up = di("a_up", [DEPTH, 64, D])
        self.g_up = di("g_up", [DEPTH, 128, D])
        self.v_up = di("v_up", [32, D])
        self.w_uq = di("w_uq", [DEPTH, 384, 1536])
        self.w_ukv = di("w_ukv", [DEPTH, 256, 2048])
        self.w_branch = di("w_branch", [DEPTH, 3, D, D])
        self.w_out = di("w_out", [DEPTH, D, D])
        self.w_ffn_in = di("w_ffn_in", [DEPTH, D, 2 * D_FF])
        self.w_ffn_out = di("w_ffn_out", [DEPTH, D_FF, D])
        self.outT = nc.dram_tensor("outT", [D, S], F32, kind="ExternalOutput").ap()
        self.dbg = {}

        def dscr(name, shape, dt=F32):
            kind = "ExternalOutput" if name in self.debug else "Internal"
            t = nc.dram_tensor(name, list(shape), dt, kind=kind).ap()
            return t

        self.zT = dscr("zT", [N_ZC, 128, S])
        self.zT_res = [Res() for _ in range(N_ZC)]
        self.xcur = dscr("xcur", [8, 128, S])
        self.xcur_res = [Res() for _ in range(self.NTG)]
        self.vtok = dscr("vtok", [S, D], BF16)
        self.vtok_res = [Res() for _ in range(S // 128)]
        self.obr = dscr("obr", [3, 8, 128, S], BF16)
        self.obr_res = [[Res() for _ in range(8)] for _ in range(3)]
        self.aT = dscr("aT", [D_FF // 128, 128, S], BF16)
        self.aT_res = [Res() for _ in range(D_FF // 128)]
        self.vfirst = dscr("vfirst", [8, 128, S])
        self.vfirst_res = [Res() for _ in range(8)]
        self.final_ops = []

        sch = self.sch
        self.PS = [Tile(nc.alloc_psum_tensor(f"psb{i}", [128, 512], F32), 1) for i in range(8)]
        self.ones_bf = sch.sb([128, 128], BF16)
        self.ident_bf = sch.sb([128, 128], BF16)
        self.ident_f = sch.sb([128, 128], F32)
        self.eps6 = sch.sb([128, 1], F32)
        self.vec = [sch.sb([128, NV], F32) for _ in range(DEPTH)]
        sch.op("dve", lambda e: e.memset(self.ones_bf[:], 1.0), writes=self.ones_bf.res)
        sch.op("dve", lambda e: e.memset(self.eps6[:], 1e-6), writes=self.eps6.res)
        sch.op("pool", lambda e: e.memset(self.ident_f[:], 1.0), writes=self.ident_f.res)
        sch.op("pool", lambda e: e.affine_select(out=self.ident_f[:], in_=self.ident_f[:], pattern=[[-1, 128]],
                                                 compare_op=ALU.is_equal, fill=0.0, base=0, channel_multiplier=1),
               reads=self.ident_f.res, writes=self.ident_f.res)
        sch.op("dve", lambda e: e.tensor_copy(out=self.ident_bf[:], in_=self.ident_f[:]),
               reads=self.ident_f.res, writes=self.ident_bf.res)
        for l in range(DEPTH):
            sch.dma("sp", self.vec[l][:], self.vecs[l], writes=self.vec[l].res)

    def V(self, l, name, c=0, n=1):
        o = VOFF[name] + c
        return self.vec[l][:, o:o + n]

    def norm_hT(self, l, src, src_res, gname, hT):
        sch, TG = self.sch, self.TG
        m = sch.mark()
        xb = sch.sb([128, 2, 8, TG], F32, nres=2)
        sq = sch.sb([128, 2, 8, TG], BF16, nres=2)
        rs = sch.sb([128, 2, TG], F32, nres=2)
        for tg in range(self.NTG):
            s = tg % 2
            ts = slice(tg * TG, (tg + 1) * TG)
            sch.dma("sp", xb[:, s], src[:, :, ts].rearrange("c p s -> p c s"), reads=[src_res[tg]], writes=[xb.res[s]])
            sch.op("act", lambda e, s=s: e.activation(out=sq[:, s], in_=xb[:, s], func=AF.Square),
                   reads=[xb.res[s]], writes=[sq.res[s]])
            bank = self.PS[tg % 2]
            for kc in range(8):
                sch.op("pe", lambda e, s=s, kc=kc, bank=bank: e.matmul(bank[:, 0:TG], lhsT=self.ones_bf[:], rhs=sq[:, s, kc, :],
                                                                     start=(kc == 0), stop=(kc == 7)),
                       reads=[sq.res[s], self.ones_bf.res[0]], writes=bank.res)
            sch.op("act", lambda e, s=s, bank=bank: e.activation(out=rs[:, s], in_=bank[:, 0:TG], func=AF.Ln,
                                                                scale=1.0 / D, bias=self.eps6[:, 0:1]),
                   reads=bank.res + self.eps6.res, writes=[rs.res[s]])
            sch.op("act", lambda e, s=s: e.activation(out=rs[:, s], in_=rs[:, s], func=AF.Exp, scale=-0.5), reads=[rs.res[s]], writes=[rs.res[s]])
            for kc in range(8):
                sch.op("dve", lambda e, s=s, kc=kc, ts=ts: e.scalar_tensor_tensor(
                    out=hT[:, kc, ts], in0=xb[:, s, kc, :], scalar=self.V(l, gname, kc), in1=rs[:, s],
                    op0=ALU.mult, op1=ALU.mult),
                    reads=[xb.res[s], rs.res[s], self.vec[l].res[0]], writes=[hT.res[tg]])
        sch.release(m)

    def gemm(self, fcs, KC, kp, wsrc, rhs, evac, banks, post_fc=None, ntg=None):
        sch, TG = self.sch, self.TG
        ntg = self.NTG if ntg is None else ntg
        m = sch.mark()
        wb = sch.sb([128, 2, KC, 128], BF16, nres=2)
        it = 0
        for i, fc in enumerate(fcs):
            s = i % 2
            ap, ncols = wsrc(fc)
            sch.dma("pool", wb[0:kp, s, :, 0:ncols], ap, writes=[wb.res[s]])
            for tg in range(ntg):
                bank = banks[it % len(banks)]
                it += 1
                for kc in range(KC):
                    r_ap, r_res = rhs(kc, tg)
                    sch.op("pe", lambda e, s=s, kc=kc, bank=bank, r_ap=r_ap, ncols=ncols: e.matmul(
                        bank[0:ncols, 0:TG], lhsT=wb[0:kp, s, kc, 0:ncols], rhs=r_ap, start=(kc == 0), stop=(kc == KC - 1)),
                        reads=[wb.res[s]] + r_res, writes=bank.res)
                evac(fc, tg, bank, ncols)
            if post_fc is not None:
                post_fc(fc)
        sch.release(m)

    def stage_inproj(self, l):
        sch, S, TG = self.sch, self.S, self.TG
        m = sch.mark()
        hT = sch.sb([128, 8, S], BF16, nres=self.NTG)
        self.norm_hT(l, self.xcur, self.xcur_res, "mix_pre_g", hT)
        zt = sch.sb([128, 2, S], F32, nres=2)
        cnt = [0]
        fcs = [fc for fc in range(88 if l == 0 else 89) if not (16 <= fc < 24)]
        pos = {fc: i for i, fc in enumerate(fcs)}

        def wsrc(fc):
            if fc == 88:
                return self.w_vres.rearrange("(kc p) f -> p kc f", p=128), 32
            c0, ncols = zcols(fc)
            return self.w_in[l][:, c0:c0 + ncols].rearrange("(kc p) f -> p kc f", p=128), ncols

        def rhs(kc, tg):
            return hT[:, kc, tg * TG:(tg + 1) * TG], [hT.res[tg]]

        def evac(fc, tg, bank, ncols):
            s = pos[fc] % 2
            cnt[0] += 1
            if 64 <= fc < 88:
                sch.op("act", lambda e: e.activation(out=zt[0:ncols, s, tg * TG:(tg + 1) * TG], in_=bank[0:ncols, 0:TG], func=AF.Sigmoid),
                       reads=bank.res, writes=[zt.res[s]])
            elif cnt[0] % 2:
                sch.op("act", lambda e: e.copy(out=zt[0:ncols, s, tg * TG:(tg + 1) * TG], in_=bank[0:ncols, 0:TG]),
                       reads=bank.res, writes=[zt.res[s]])
            else:
                sch.op("dve", lambda e: e.tensor_copy(out=zt[0:ncols, s, tg * TG:(tg + 1) * TG], in_=bank[0:ncols, 0:TG]),
                       reads=bank.res, writes=[zt.res[s]])

        def post_fc(fc):
            s = pos[fc] % 2
            ncols = wsrc(fc)[1]
            sch.dma("sp", self.zT[fc, 0:ncols, :], zt[0:ncols, s, :], reads=[zt.res[s]], writes=[self.zT_res[fc]])

        self.gemm(fcs, 8, 128, wsrc, rhs, evac, self.PS[0:4], post_fc)
        m2 = sch.mark()
        wv = sch.sb([128, 8, 1024], BF16)
        vs = sch.sb([128, 2, 1024], BF16, nres=2)
        for q in range(4):
            sch.dma("pool", wv[:, :, q * 256:(q + 1) * 256],
                    self.w_in[l][:, 2048 + q * 256:2048 + (q + 1) * 256].rearrange("(kc p) f -> p kc f", p=128), writes=wv.res)
        for tt in range(S // 128):
            s = tt % 2
            tg = (tt * 128) // TG
            for hf in range(2):
                bank = self.PS[4 + (tt * 2 + hf) % 4]
                for kc in range(8):
                    sch.op("pe", lambda e, kc=kc, bank=bank, tt=tt, hf=hf: e.matmul(
                        bank[:, :], lhsT=hT[:, kc, tt * 128:(tt + 1) * 128], rhs=wv[:, kc, hf * 512:(hf + 1) * 512],
                        start=(kc == 0), stop=(kc == 7)), reads=[hT.res[tg]] + wv.res, writes=bank.res)
                if hf == 0:
                    sch.op("act", lambda e, s=s, bank=bank: e.copy(out=vs[:, s, 0:512], in_=bank[:, :]), reads=bank.res, writes=[vs.res[s]])
                else:
                    sch.op("dve", lambda e, s=s, bank=bank: e.tensor_copy(out=vs[:, s, 512:1024], in_=bank[:, :]), reads=bank.res, writes=[vs.res[s]])
            sch.dma("sp", self.vtok[tt * 128:(tt + 1) * 128, :], vs[:, s, :], reads=[vs.res[s]], writes=[self.vtok_res[tt]])
        sch.release(m2)
        sch.release(m)

    def pn_bufs(self, nb=2):
        sch, TG = self.sch, self.TG
        return dict(sq=sch.sb([128, nb, 8, TG], BF16, nres=nb), rs=sch.sb([128, nb, TG], F32, nres=nb),
                    xb=sch.sb([128, nb, 8, TG], F32, nres=nb), t=sch.sb([128, 2, TG], F32, nres=2), n=[0], nb=nb)

    def postnorm_residual(self, l, gname, pn, mo_ap, mo_res, tg, to_out):
        sch, TG = self.sch, self.TG
        s = pn["n"][0] % pn["nb"]
        pn["n"][0] += 1
        sq, rs, xb, tt = pn["sq"], pn["rs"], pn["xb"], pn["t"]
        ts = slice(tg * TG, (tg + 1) * TG)
        sch.dma("sp", xb[:, s], self.xcur[:, :, ts].rearrange("c p s -> p c s"), reads=[self.xcur_res[tg]], writes=[xb.res[s]])
        sch.op("act", lambda e: e.activation(out=sq[:, s], in_=mo_ap, func=AF.Square), reads=mo_res, writes=[sq.res[s]])
        bank = self.PS[6 + s]
        for kc in range(8):
            sch.op("pe", lambda e, kc=kc: e.matmul(bank[:, 0:TG], lhsT=self.ones_bf[:], rhs=sq[:, s, kc, :],
                                                   start=(kc == 0), stop=(kc == 7)),
                   reads=[sq.res[s], self.ones_bf.res[0]], writes=bank.res)
        sch.op("act", lambda e: e.activation(out=rs[:, s], in_=bank[:, 0:TG], func=AF.Ln, scale=1.0 / D, bias=self.eps6[:, 0:1]),
               reads=bank.res + self.eps6.res, writes=[rs.res[s]])
        sch.op("act", lambda e: e.activation(out=rs[:, s], in_=rs[:, s], func=AF.Exp, scale=-0.5), reads=[rs.res[s]], writes=[rs.res[s]])
        for kc in range(8):
            sch.op("dve", lambda e, kc=kc: e.scalar_tensor_tensor(out=tt[:, kc % 2], in0=mo_ap[:, kc, :], scalar=self.V(l, gname, kc),
                                                                 in1=rs[:, s], op0=ALU.mult, op1=ALU.mult),
                   reads=mo_res + [rs.res[s], self.vec[l].res[0]], writes=[tt.res[kc % 2]])
            sch.op("pool", lambda e, kc=kc: e.tensor_tensor(out=xb[:, s, kc, :], in0=xb[:, s, kc, :], in1=tt[:, kc % 2], op=ALU.add),
                   reads=[tt.res[kc % 2], xb.res[s]], writes=[xb.res[s]])
        if to_out:
            sch.dma("sp", self.outT.rearrange("(c p) s -> p c s", p=128)[:, :, ts], xb[:, s], reads=[xb.res[s]], writes=[self.xcur_res[tg]])
        else:
            sch.dma("sp", self.xcur[:, :, ts].rearrange("c p s -> p c s"), xb[:, s], reads=[xb.res[s]], writes=[self.xcur_res[tg]])

    def stage_merge(self, l):
        sch, S, TG = self.sch, self.S, self.TG
        TB = min(S, 1024)
        nb = TB // TG
        m = sch.mark()
        mg = sch.sb([128, 8, TB], F32, nres=8)
        ob = sch.sb([128, 2, 8, TB], BF16, nres=16)
        gt = sch.sb([128, 4, TG], F32, nres=4)
        tmp = sch.sb([128, 4, TG], F32, nres=4)
        pn = self.pn_bufs(1)
        cnt = [0]
        for tb in range(S // TB):
            t0 = tb * TB
            def load_ob(b_, t0=t0):
                for kc in range(8):
                    sch.dma("sp", ob[:, b_ % 2, kc, :], self.obr[b_, kc, :, t0:t0 + TB], reads=[self.obr_res[b_][kc]], writes=[ob.res[(b_ % 2) * 8 + kc]])

            load_ob(0)
            for b in range(3):
                if b < 2:
                    load_ob(b + 1)

                def wsrc(fc, b=b):
                    return self.w_branch[l, b][:, fc * 128:(fc + 1) * 128].rearrange("(kc p) f -> p kc f", p=128), 128

                def rhs(kc, tg, b=b):
                    return ob[:, b % 2, kc, tg * TG:(tg + 1) * TG], [ob.res[(b % 2) * 8 + kc]]

                def evac(fc, tg, bank, ncols, b=b, t0=t0):
                    s = cnt[0] % 4
                    cnt[0] += 1
                    zc = 64 + b * 8 + fc
                    tl = slice(tg * TG, (tg + 1) * TG)
                    sch.dma("sp", gt[:, s], self.zT[zc, :, t0 + tg * TG:t0 + (tg + 1) * TG], reads=[self.zT_res[zc]], writes=[gt.res[s]])
                    if b == 0:
                        sch.op("dve", lambda e: e.tensor_tensor(out=mg[:, fc, tl], in0=bank[:, 0:TG], in1=gt[:, s], op=ALU.mult),
                               reads=bank.res + [gt.res[s]], writes=[mg.res[fc]])
                    else:
                        sch.op("dve", lambda e: e.tensor_tensor(out=tmp[:, s], in0=bank[:, 0:TG], in1=gt[:, s], op=ALU.mult),
                               reads=bank.res + [gt.res[s]], writes=[tmp.res[s]])
                        sch.op("pool", lambda e: e.tensor_tensor(out=mg[:, fc, tl], in0=mg[:, fc, tl], in1=tmp[:, s], op=ALU.add),
                               reads=[tmp.res[s], mg.res[fc]], writes=[mg.res[fc]])

                self.gemm(list(range(8)), 8, 128, wsrc, rhs, evac, self.PS[0:4], ntg=nb)
            for kc in range(8):
                eng = "act" if kc % 2 else "pool"
                if eng == "act":
                    sch.op("act", lambda e, kc=kc: e.copy(out=ob[:, 1, kc, :], in_=mg[:, kc, :]), reads=[mg.res[kc]], writes=[ob.res[8 + kc]])
                else:
                    sch.op("pool", lambda e, kc=kc: e.tensor_copy(out=ob[:, 1, kc, :], in_=mg[:, kc, :]), reads=[mg.res[kc]], writes=[ob.res[8 + kc]])

            def wsrc2(fc):
                return self.w_out[l][:, fc * 128:(fc + 1) * 128].rearrange("(kc p) f -> p kc f", p=128), 128

            def rhs2(kc, tg):
                return ob[:, 1, kc, tg * TG:(tg + 1) * TG], [ob.res[8 + kc]]

            def evac2(fc, tg, bank, ncols):
                cnt[0] += 1
                tl = slice(tg * TG, (tg + 1) * TG)
                if cnt[0] % 2:
                    sch.op("act", lambda e: e.copy(out=mg[:, fc, tl], in_=bank[:, 0:TG]), reads=bank.res, writes=[mg.res[fc]])
                else:
                    sch.op("dve", lambda e: e.tensor_copy(out=mg[:, fc, tl], in_=bank[:, 0:TG]), reads=bank.res, writes=[mg.res[fc]])

            self.gemm(list(range(8)), 8, 128, wsrc2, rhs2, evac2, self.PS[0:4], ntg=nb)
            for tg in range(nb):
                self.postnorm_residual(l, "mix_post_g", pn, mg[:, :, tg * TG:(tg + 1) * TG], list(mg.res), tb * nb + tg, False)
        sch.release(m)

    def stage_ffn(self, l, last):
        sch, S, TG = self.sch, self.S, self.TG
        NJ = D_FF // 128
        m = sch.mark()
        hT = sch.sb([128, 8, S], BF16, nres=self.NTG)
        self.norm_hT(l, self.xcur, self.xcur_res, "ffn_pre_g", hT)
        at = sch.sb([128, 2, S], BF16, nres=2)
        sg = sch.sb([128, S], F32, nres=self.NTG)

        def wsrc(fc):
            j, u = fc // 2, fc % 2
            c0 = u * D_FF + j * 128
            return self.w_ffn_in[l][:, c0:c0 + 128].rearrange("(kc p) f -> p kc f", p=128), 128

        def rhs(kc, tg):
            return hT[:, kc, tg * TG:(tg + 1) * TG], [hT.res[tg]]

        def evac(fc, tg, bank, ncols):
            j, u = fc // 2, fc % 2
            ts = slice(tg * TG, (tg + 1) * TG)
            if u == 0:
                sch.op("act", lambda e: e.activation(out=sg[:, ts], in_=bank[:, 0:TG], func=AF.Silu), reads=bank.res, writes=[sg.res[tg]])
            else:
                sch.op("dve", lambda e: e.tensor_tensor(out=at[:, j % 2, ts], in0=bank[:, 0:TG], in1=sg[:, ts], op=ALU.mult),
                       reads=bank.res + [sg.res[tg]], writes=[at.res[j % 2]])

        def post_fc(fc):
            j, u = fc // 2, fc % 2
            if u == 1:
                sch.dma("sp", self.aT[j], at[:, j % 2, :], reads=[at.res[j % 2]], writes=[self.aT_res[j]])

        self.gemm(list(range(2 * NJ)), 8, 128, wsrc, rhs, evac, self.PS[0:4], post_fc)
        sch.release(m)
        m = sch.mark()
        w2 = sch.sb([128, NJ, 1024], BF16, nres=NJ)
        ab = sch.sb([128, 2, NJ, TG], BF16, nres=2)
        mo = sch.sb([128, 2, 8, TG], F32, nres=2)
        pn = self.pn_bufs(1)
        for kc in range(NJ):
            sch.dma("pool", w2[:, kc, :], self.w_ffn_out[l][kc * 128:(kc + 1) * 128, :], writes=[w2.res[kc]])
        it = 0
        for tg in range(self.NTG + 1):
            s = tg % 2
            if tg < self.NTG:
                ts = slice(tg * TG, (tg + 1) * TG)
                sch.dma("sp", ab[:, s], self.aT[:, :, ts].rearrange("j p s -> p j s"), reads=self.aT_res, writes=[ab.res[s]])
            for fc in range(8):
                if tg < self.NTG:
                    bank = self.PS[it % 4]
                    it += 1
                    for kc in range(NJ):
                        sch.op("pe", lambda e, kc=kc, bank=bank, fc=fc, s=s: e.matmul(
                            bank[:, 0:TG], lhsT=w2[:, kc, fc * 128:(fc + 1) * 128], rhs=ab[:, s, kc, :], start=(kc == 0), stop=(kc == NJ - 1)),
                            reads=[w2.res[kc], ab.res[s]], writes=bank.res)
                    if fc % 2:
                        sch.op("act", lambda e, bank=bank, fc=fc, s=s: e.copy(out=mo[:, s, fc, :], in_=bank[:, 0:TG]), reads=bank.res, writes=[mo.res[s]])
                    else:
                        sch.op("dve", lambda e, bank=bank, fc=fc, s=s: e.tensor_copy(out=mo[:, s, fc, :], in_=bank[:, 0:TG]), reads=bank.res, writes=[mo.res[s]])
                if fc == 1 and tg > 0:
                    self.postnorm_residual(l, "ffn_post_g", pn, mo[:, 1 - s], [mo.res[1 - s]], tg - 1, last)
        sch.release(m)

    def hgrn_consts(self):
        sch = self.sch
        c = {}
        c["mbd"] = sch.sb([128, 128], F32)
        c["cm3"] = sch.sb([128, 4, 128], F32)
        c["eps6"] = self.eps6
        mbd, cm3 = c["mbd"], c["cm3"]
        sch.op("pool", lambda e: e.memset(mbd[:], 1.0), writes=mbd.res)
        sch.op("pool", lambda e: e.affine_select(out=mbd[:], in_=mbd[:], pattern=[[1, 128]], compare_op=ALU.is_ge, fill=0.0,
                                                 base=0, channel_multiplier=-1), reads=mbd.res, writes=mbd.res)
        sch.op("pool", lambda e: e.affine_select(out=mbd[:].rearrange("p (c i) -> p c i", i=32), in_=mbd[:].rearrange("p (c i) -> p c i", i=32),
                                                 pattern=[[-32, 4], [0, 32]], compare_op=ALU.is_ge, fill=0.0, base=0, channel_multiplier=1),
               reads=mbd.res, writes=mbd.res)
        sch.op("pool", lambda e: e.memset(cm3[:], 1.0), writes=cm3.res)
        sch.op("pool", lambda e: e.affine_select(out=cm3[:], in_=cm3[:], pattern=[[-32, 4], [0, 128]], compare_op=ALU.is_ge, fill=0.0,
                                                 base=0, channel_multiplier=1), reads=cm3.res, writes=cm3.res)
        sch.op("pool", lambda e: e.affine_select(out=cm3[:], in_=cm3[:], pattern=[[32, 4], [0, 128]], compare_op=ALU.is_ge, fill=0.0,
                                                 base=31, channel_multiplier=-1), reads=cm3.res, writes=cm3.res)
        return c

    def stage_hgrn(self, l):
        sch, S = self.sch, self.S
        T = min(S, 512)
        NB = S // T
        nt = T // 128
        nch = T // 32
        TG = min(T, 512)
        m = sch.mark()
        c = self.hgrn_consts()
        mbd, cm3 = c["mbd"], c["cm3"]
        mask32 = sch.sb([128, T], F32)
        sch.op("pool", lambda e: e.memset(mask32[:], 1.0), writes=mask32.res)
        sch.op("pool", lambda e: e.memset(mask32[:].rearrange("p (c i) -> p c i", i=32)[:, :, 0:1], 0.0), writes=mask32.res)
        lbv = sch.sb([128, 8], F32)
        oml = sch.sb([128, 8], F32)
        if l == 0:
            sch.op("dve", lambda e: e.memset(lbv[:], 0.0), writes=lbv.res)
        else:
            sch.op("dve", lambda e: e.tensor_tensor(out=lbv[:], in0=self.V(l, "lb1", 0, 8), in1=self.V(l, "lb0", 0, 8), op=ALU.subtract),
                   reads=self.vec[l].res, writes=lbv.res)
            sch.op("act", lambda e: e.activation(out=lbv[:], in_=lbv[:], func=AF.Sigmoid), reads=lbv.res, writes=lbv.res)
        sch.op("dve", lambda e: e.tensor_scalar(out=oml[:], in0=lbv[:], scalar1=-1.0, scalar2=1.0, op0=ALU.mult, op1=ALU.add),
               reads=lbv.res, writes=oml.res)
        NS = 4
        bufs = []
        for i in range(NS):
            bufs.append(dict(
                zq=sch.sb([128, T], F32), zf=sch.sb([128, T], F32), zog=sch.sb([128, T], F32), vt=sch.sb([128, nt, 128], BF16),
                lf=sch.sb([128, T], F32), kk=sch.sb([128, T], F32), b=sch.sb([128, T], F32), eb=sch.sb([128, T], F32),
                tmp=sch.sb([128, T], F32), qt32=sch.sb([128, T], F32), qt=sch.sb([128, T], BF16), kt=sch.sb([128, T], BF16), kh=sch.sb([128, T], BF16),
                oT=sch.sb([128, T], F32), ob=sch.sb([128, T], BF16)))
        pT = sch.sb([128, 4, 128], BF16, nres=4)
        k4 = sch.sb([128, 4, 4, 128], BF16, nres=4)
        u4 = sch.sb([128, 4, 4, 128], F32, nres=4)
        st32 = sch.sb([128, 16, 128], F32, nres=16)
        sqs = [sch.sb([128, TG], BF16) for _ in range(2)]
        rss = [sch.sb([128, TG], F32) for _ in range(2)]
        PS = self.PS
        sidxs = [[0], [0]]

        def prep_load(h, tb, B):
            t0 = tb * T
            zq, zf, zog, vt, lf, kk, b, eb, tmp, qt32, qt, kt, kh, oT, ob = (B[k] for k in (
                "zq", "zf", "zog", "vt", "lf", "kk", "b", "eb", "tmp", "qt32", "qt", "kt", "kh", "oT", "ob"))
            sch.dma("sp", zq[:], self.zT[h, :, t0:t0 + T], reads=[self.zT_res[h]], writes=zq.res)
            sch.dma("sp", zf[:], self.zT[8 + h, :, t0:t0 + T], reads=[self.zT_res[8 + h]], writes=zf.res)
            sch.dma("sp", zog[:], self.zT[24 + h, :, t0:t0 + T], reads=[self.zT_res[24 + h]], writes=zog.res)
            sch.dma("sp", vt[:], self.vtok[t0:t0 + T, h * 128:(h + 1) * 128].rearrange("(n p) f -> p n f", p=128),
                    reads=self.vtok_res[t0 // 128:(t0 + T) // 128], writes=vt.res)

        def prep(h, tb, B):
            zq, zf, zog, vt, lf, kk, b, eb, tmp, qt32, qt, kt, kh, oT, ob = (B[k] for k in (
                "zq", "zf", "zog", "vt", "lf", "kk", "b", "eb", "tmp", "qt32", "qt", "kt", "kh", "oT", "ob"))
            sch.op("act", lambda e, zf=zf: e.activation(out=zf[:], in_=zf[:], func=AF.Sigmoid), reads=zf.res, writes=zf.res)
            sch.op("act", lambda e, zog=zog: e.activation(out=zog[:], in_=zog[:], func=AF.Silu), reads=zog.res, writes=zog.res)
            sch.op("dve", lambda e, zf=zf, h=h: e.tensor_scalar(out=zf[:], in0=zf[:], scalar1=oml[:, h:h + 1], scalar2=lbv[:, h:h + 1],
                                                              op0=ALU.mult, op1=ALU.add), reads=zf.res + oml.res + lbv.res, writes=zf.res)
            sch.op("act", lambda e, zf=zf, lf=lf: e.activation(out=lf[:], in_=zf[:], func=AF.Ln), reads=zf.res, writes=lf.res)
            sch.op("pool", lambda e, zf=zf, kk=kk: e.tensor_scalar(out=kk[:], in0=zf[:], scalar1=-1.0, scalar2=1.0, op0=ALU.mult, op1=ALU.add),
                   reads=zf.res, writes=kk.res)
            sch.op("dve", lambda e, b=b, lf=lf: e.tensor_tensor_scan(out=b[:], data0=mask32[:], data1=lf[:], initial=0.0,
                                                                    op0=ALU.mult, op1=ALU.add), reads=lf.res + mask32.res, writes=b.res)
            sch.op("act", lambda e, b=b, eb=eb: e.activation(out=eb[:], in_=b[:], func=AF.Exp), reads=b.res, writes=eb.res)
            sch.op("dve", lambda e, qt32=qt32, zq=zq, eb=eb: e.tensor_tensor(out=qt32[:], in0=zq[:], in1=eb[:], op=ALU.mult),
                   reads=zq.res + eb.res, writes=qt32.res)
            sch.op("pool", lambda e, qt32=qt32, qt=qt: e.tensor_copy(out=qt[:], in_=qt32[:]), reads=qt32.res, writes=qt.res)
            sch.op("act", lambda e, b=b, tmp=tmp: e.activation(out=tmp[:], in_=b[:], func=AF.Exp, scale=-1.0), reads=b.res, writes=tmp.res)
            sch.op("dve", lambda e, kt=kt, kk=kk, tmp=tmp: e.tensor_tensor(out=kt[:], in0=kk[:], in1=tmp[:], op=ALU.mult),
                   reads=kk.res + tmp.res, writes=kt.res)

        def tiles(h, tb, B, X):
            sidx = sidxs[X]
            sq, rs = sqs[X], rss[X]
            t0 = tb * T
            zq, zf, zog, vt, lf, kk, b, eb, tmp, qt32, qt, kt, kh, oT, ob = (B[k] for k in (
                "zq", "zf", "zog", "vt", "lf", "kk", "b", "eb", "tmp", "qt32", "qt", "kt", "kh", "oT", "ob"))
            if tb == 0:
                cur = 8 * X + sidx[0] % 8
                sch.op("pool", lambda e, cur=cur: e.memset(st32[:, cur], 0.0), writes=[st32.res[cur]])
            eb3 = eb[:].rearrange("p (c i) -> p c i", i=32)

            def phA(n):
                tl = slice(n * 128, (n + 1) * 128)
                s2 = 2 * X + n % 2
                pa, pb_, pu, po = PS[4 * X + 0], PS[4 * X + 1], PS[4 * X + 2], PS[4 * X + 3]
                sch.op("pe", lambda e, pa=pa, kt=kt, qt=qt, tl=tl: e.matmul(pa[:, 0:128], lhsT=kt[:, tl], rhs=qt[:, tl], start=True, stop=True),
                       reads=kt.res + qt.res, writes=pa.res)
                sch.op("dve", lambda e, pa=pa, s2=s2: e.tensor_tensor(out=pT[:, s2], in0=pa[:, 0:128], in1=mbd[:], op=ALU.mult),
                       reads=pa.res + mbd.res, writes=[pT.res[s2]])
                pbb = pb_[:].bitcast(BF16)
                sch.op("pe", lambda e, pbb=pbb, kt=kt, tl=tl: e.transpose(out=pbb[:, 0:128], in_=kt[:, tl], identity=self.ident_bf[:]),
                       reads=kt.res + self.ident_bf.res, writes=pb_.res)
                sch.op("dve", lambda e, pbb=pbb, s2=s2: e.tensor_tensor(out=k4[:, s2], in0=pbb[:, 0:128].unsqueeze(1).broadcast_to([128, 4, 128]),
                                                                       in1=cm3[:], op=ALU.mult), reads=pb_.res + cm3.res, writes=[k4.res[s2]])
                for cc in range(4):
                    sch.op("pe", lambda e, pu=pu, s2=s2, cc=cc, vt=vt, n=n: e.matmul(pu[:, cc * 128:(cc + 1) * 128], lhsT=k4[:, s2, cc, :],
                                                                                 rhs=vt[:, n, :], start=True, stop=True),
                           reads=[k4.res[s2]] + vt.res, writes=pu.res)
                for cc in range(4):
                    sch.op("act", lambda e, pu=pu, s2=s2, cc=cc, n=n: e.activation(out=u4[:, s2, cc, :], in_=pu[:, cc * 128:(cc + 1) * 128], func=AF.Copy,
                                                                               scale=eb3[:, n * 4 + cc, 31:32]),
                           reads=pu.res + eb.res, writes=[u4.res[s2]])

            def phB(n):
                tl = slice(n * 128, (n + 1) * 128)
                s2 = 2 * X + n % 2
                po = PS[4 * X + 3]
                sch.op("pe", lambda e, po=po, vt=vt, n=n, s2=s2: e.matmul(po[:, 0:128], lhsT=vt[:, n, :], rhs=pT[:, s2], start=True, stop=False),
                       reads=vt.res + [pT.res[s2]], writes=po.res)
                for cc in range(4):
                    cur = 8 * X + sidx[0] % 8
                    nxt = 8 * X + (sidx[0] + 1) % 8
                    sidx[0] += 1
                    ch = n * 4 + cc
                    cs = slice(n * 128 + cc * 32, n * 128 + (cc + 1) * 32)
                    sch.op("pe", lambda e, po=po, cur=cur, qt32=qt32, cs=cs, cc=cc: e.matmul(po[:, cc * 32:(cc + 1) * 32], lhsT=st32[:, cur], rhs=qt32[:, cs],
                                                                                    start=False, stop=(cc == 3)),
                           reads=[st32.res[cur]] + qt32.res, writes=po.res)
                    sch.op("dve", lambda e, cur=cur, nxt=nxt, eb3=eb3, ch=ch, s2=s2, cc=cc: e.scalar_tensor_tensor(
                        out=st32[:, nxt], in0=st32[:, cur], scalar=eb3[:, ch, 31:32], in1=u4[:, s2, cc, :], op0=ALU.mult, op1=ALU.add),
                        reads=[st32.res[cur], u4.res[s2]] + eb.res, writes=[st32.res[nxt]])
                sch.op("act", lambda e, po=po, oT=oT, tl=tl: e.copy(out=oT[:, tl], in_=po[:, 0:128]), reads=po.res, writes=oT.res)

            phA(0)
            for n in range(nt):
                if n + 1 < nt:
                    phA(n + 1)
                phB(n)
                yield
            for g in range(T // TG):
                gs = slice(g * TG, (g + 1) * TG)
                bank = PS[4 * X + g % 2]
                sch.op("act", lambda e, oT=oT, gs=gs: e.activation(out=sq[:], in_=oT[:, gs], func=AF.Square), reads=oT.res, writes=sq.res)
                sch.op("pe", lambda e, bank=bank: e.matmul(bank[:, 0:TG], lhsT=self.ones_bf[:], rhs=sq[:], start=True, stop=True),
                       reads=sq.res + self.ones_bf.res, writes=bank.res)
                sch.op("act", lambda e, bank=bank: e.activation(out=rs[:], in_=bank[:, 0:TG], func=AF.Ln, scale=1.0 / 128, bias=self.eps6[:, 0:1]),
                       reads=bank.res + self.eps6.res, writes=rs.res)
                sch.op("act", lambda e: e.activation(out=rs[:], in_=rs[:], func=AF.Exp, scale=-0.5), reads=rs.res, writes=rs.res)
                sch.op("dve", lambda e, oT=oT, gs=gs: e.scalar_tensor_tensor(out=oT[:, gs], in0=oT[:, gs], scalar=self.V(l, "onorm_g"), in1=rs[:],
                                                                           op0=ALU.mult, op1=ALU.mult), reads=oT.res + rs.res + self.vec[l].res, writes=oT.res)
                sch.op("dve", lambda e, oT=oT, ob=ob, zog=zog, gs=gs: e.tensor_tensor(out=ob[:, gs], in0=oT[:, gs], in1=zog[:, gs], op=ALU.mult),
                       reads=oT.res + zog.res, writes=ob.res)
            sch.dma("sp", self.obr[0, h, :, t0:t0 + T], ob[:], reads=ob.res, writes=[self.obr_res[0][h]])


        def stream(X):
            items = [(h, tb) for h in range(4 * X, 4 * X + 4) for tb in range(NB)]
            prep_load(items[0][0], items[0][1], bufs[2 * X])
            prep(items[0][0], items[0][1], bufs[2 * X])
            for i, (h, tb) in enumerate(items):
                nxt_ = items[i + 1] if i + 1 < len(items) else None
                if nxt_ is not None:
                    prep_load(nxt_[0], nxt_[1], bufs[2 * X + (i + 1) % 2])
                first = True
                for _ in tiles(h, tb, bufs[2 * X + i % 2], X):
                    if first and nxt_ is not None:
                        prep(nxt_[0], nxt_[1], bufs[2 * X + (i + 1) % 2])
                    first = False
                    yield

        gens = [stream(0), stream(1)]
        for _ in range(2):
            next(gens[0])
        while gens:
            for g_ in list(gens):
                try:
                    next(g_)
                except StopIteration:
                    gens.remove(g_)
        sch.release(m)

    def stage_rope_tables(self):
        sch, S, nc = self.sch, self.S, self.nc
        self.cs = nc.dram_tensor("cs_tab", [2, 64, S], F32, kind=("ExternalOutput" if "cs_tab" in self.debug else "Internal")).ap()
        self.cs_res = [Res(), Res()]
        m = sch.mark()
        pi = sch.sb([64, S], I32)
        ang = sch.sb([64, S], F32)
        u = sch.sb([64, S], F32)
        ki = sch.sb([64, S], I32)
        kf = sch.sb([64, S], F32)
        idx = sch.sb([64, 1], I32)
        invf = sch.sb([64, 1], F32)
        zero = sch.sb([64, 1], F32)
        sch.op("dve", lambda e: e.memset(zero[:], 0.0), writes=zero.res)
        sch.dma("sp", pi[:], self.pos.partition_broadcast(64), writes=pi.res)
        sch.op("pool", lambda e: e.iota(idx[0:32, :], pattern=[[0, 1]], base=0, channel_multiplier=1), writes=idx.res)
        sch.op("pool", lambda e: e.iota(idx[32:64, :], pattern=[[0, 1]], base=0, channel_multiplier=1), writes=idx.res)
        sch.op("dve", lambda e: e.tensor_copy(out=invf[:], in_=idx[:]), reads=idx.res, writes=invf.res)
        sch.op("act", lambda e: e.activation(out=invf[:], in_=invf[:], func=AF.Exp, scale=-(2.0 / 64) * float(np.log(10000.0))),
               reads=invf.res, writes=invf.res)
        sch.op("dve", lambda e: e.tensor_copy(out=ang[:], in_=pi[:]), reads=pi.res, writes=ang.res)
        sch.op("dve", lambda e: e.tensor_scalar(out=ang[:], in0=ang[:], scalar1=invf[:, 0:1], scalar2=1.0 / (2 * np.pi), op0=ALU.mult, op1=ALU.mult),
               reads=ang.res + invf.res, writes=ang.res)
        for which, off in ((0, 0.75), (1, 0.5)):
            sch.op("dve", lambda e, off=off: e.tensor_scalar(out=u[:], in0=ang[:], scalar1=off, scalar2=None, op0=ALU.add), reads=ang.res, writes=u.res)
            sch.op("dve", lambda e: e.tensor_copy(out=ki[:], in_=u[:]), reads=u.res, writes=ki.res)
            sch.op("dve", lambda e: e.tensor_copy(out=kf[:], in_=ki[:]), reads=ki.res, writes=kf.res)
            sch.op("dve", lambda e: e.tensor_tensor(out=u[:], in0=u[:], in1=kf[:], op=ALU.subtract), reads=u.res + kf.res, writes=u.res)
            sch.op("dve", lambda e: e.tensor_scalar(out=kf[:], in0=u[:], scalar1=0.0, scalar2=None, op0=ALU.is_lt), reads=u.res, writes=kf.res)
            sch.op("dve", lambda e: e.tensor_tensor(out=u[:], in0=u[:], in1=kf[:], op=ALU.add), reads=u.res + kf.res, writes=u.res)
            sch.op("dve", lambda e: e.tensor_scalar(out=u[:], in0=u[:], scalar1=-0.5, scalar2=2 * np.pi, op0=ALU.add, op1=ALU.mult), reads=u.res, writes=u.res)
            sch.op("dve", lambda e: e.tensor_scalar(out=u[:], in0=u[:], scalar1=3.14159, scalar2=-3.14159, op0=ALU.min, op1=ALU.max), reads=u.res, writes=u.res)
            sch.op("act", lambda e: e.activation(out=u[:], in_=u[:], func=AF.Sin, bias=zero[:, 0:1]), reads=u.res + zero.res, writes=u.res)
            sch.dma("sp", self.cs[which], u[:], reads=u.res, writes=[self.cs_res[which]])
        sch.release(m)

    def stage_mla(self, l):
        sch, S, TG = self.sch, self.S, self.TG
        NTG = self.NTG
        nt = S // 128
        PS = self.PS
        scale = float((128 + 64) ** -0.5)
        m = sch.mark()
        cqn = sch.sb([128, 3, S], BF16, nres=NTG)
        ckvn = sch.sb([128, 2, S], BF16, nres=NTG)
        cos2 = sch.sb([64, S], F32)
        sin2 = sch.sb([64, S], F32)
        krT = sch.sb([64, S], BF16, nres=NTG)
        tri = sch.sb([128, 128], BF16)
        trif = sch.sb([128, 128], F32)
        rt = sch.sb([64, 64], F32)
        sch.dma("sp", cos2[:], self.cs[0], reads=[self.cs_res[0]], writes=cos2.res)
        sch.dma("sp", sin2[:], self.cs[1], reads=[self.cs_res[1]], writes=sin2.res)
        sch.op("pool", lambda e: e.memset(trif[:], 1.0), writes=trif.res)
        sch.op("pool", lambda e: e.affine_select(out=trif[:], in_=trif[:], pattern=[[1, 128]], compare_op=ALU.is_ge, fill=0.0, base=0,
                                                 channel_multiplier=-1), reads=trif.res, writes=trif.res)
        sch.op("pool", lambda e: e.tensor_copy(out=tri[:], in_=trif[:]), reads=trif.res, writes=tri.res)
        rt2 = sch.sb([64, 64], F32)
        sch.op("pool", lambda e: e.memset(rt[:], -1.0), writes=rt.res)
        sch.op("pool", lambda e: e.affine_select(out=rt[:], in_=rt[:], pattern=[[-1, 64]], compare_op=ALU.is_equal, fill=0.0, base=-32,
                                                 channel_multiplier=1), reads=rt.res, writes=rt.res)
        sch.op("pool", lambda e: e.memset(rt2[:], 1.0), writes=rt2.res)
        sch.op("pool", lambda e: e.affine_select(out=rt2[:], in_=rt2[:], pattern=[[-1, 64]], compare_op=ALU.is_equal, fill=0.0, base=32,
                                                 channel_multiplier=1), reads=rt2.res, writes=rt2.res)
        sch.op("pool", lambda e: e.tensor_tensor(out=rt[:], in0=rt[:], in1=rt2[:], op=ALU.add), reads=rt.res + rt2.res, writes=rt.res)
        m1 = sch.mark()
        zb = sch.sb([128, 2, 3, TG], F32, nres=2)
        sq = sch.sb([128, 2, 3, TG], BF16, nres=2)
        rs = sch.sb([128, 2, TG], F32, nres=2)
        zk = sch.sb([64, 2, TG], F32, nres=2)
        t1 = sch.sb([64, 2, TG], F32, nres=2)
        t2 = sch.sb([64, 2, TG], F32, nres=2)
        it = 0
        for tg in range(NTG):
            ts = slice(tg * TG, (tg + 1) * TG)
            for (c0, ncn, dst, gname, width) in ((58, 3, cqn, "q_norm_g", 384), (61, 2, ckvn, "kv_norm_g", 256)):
                s = it % 2
                it += 1
                sch.dma("sp", zb[:, s, 0:ncn], self.zT[c0:c0 + ncn, :, ts].rearrange("c p s -> p c s"),
                        reads=self.zT_res[c0:c0 + ncn], writes=[zb.res[s]])
                sch.op("act", lambda e, s=s, ncn=ncn: e.activation(out=sq[:, s, 0:ncn], in_=zb[:, s, 0:ncn], func=AF.Square),
                       reads=[zb.res[s]], writes=[sq.res[s]])
                bank = PS[s]
                for kc in range(ncn):
                    sch.op("pe", lambda e, s=s, kc=kc, bank=bank, ncn=ncn: e.matmul(bank[:, 0:TG], lhsT=self.ones_bf[:], rhs=sq[:, s, kc, :],
                                                                                 start=(kc == 0), stop=(kc == ncn - 1)),
                           reads=[sq.res[s]] + self.ones_bf.res, writes=bank.res)
                sch.op("act", lambda e, s=s, bank=bank, width=width: e.activation(out=rs[:, s], in_=bank[:, 0:TG], func=AF.Ln, scale=1.0 / width,
                                                                               bias=self.eps6[:, 0:1]), reads=bank.res + self.eps6.res, writes=[rs.res[s]])
                sch.op("act", lambda e, s=s: e.activation(out=rs[:, s], in_=rs[:, s], func=AF.Exp, scale=-0.5), reads=[rs.res[s]], writes=[rs.res[s]])
                for kc in range(ncn):
                    sch.op("dve", lambda e, s=s, kc=kc, dst=dst, gname=gname, ts=ts: e.scalar_tensor_tensor(
                        out=dst[:, kc, ts], in0=zb[:, s, kc, :], scalar=self.V(l, gname, kc), in1=rs[:, s], op0=ALU.mult, op1=ALU.mult),
                        reads=[zb.res[s], rs.res[s]] + self.vec[l].res, writes=[dst.res[tg]])
            s = tg % 2
            sch.dma("sp", zk[:, s], self.zT[63, 0:64, ts], reads=[self.zT_res[63]], writes=[zk.res[s]])
            bank = PS[2 + s]
            sch.op("pe", lambda e, s=s, bank=bank: e.matmul(bank[0:64, 0:TG], lhsT=rt[:], rhs=zk[:, s], start=True, stop=True),
                   reads=[zk.res[s]] + rt.res, writes=bank.res)
            sch.op("dve", lambda e, s=s, ts=ts: e.tensor_tensor(out=t1[:, s], in0=zk[:, s], in1=cos2[:, ts], op=ALU.mult),
                   reads=[zk.res[s]] + cos2.res, writes=[t1.res[s]])
            sch.op("dve", lambda e, s=s, ts=ts, bank=bank: e.tensor_tensor(out=t2[:, s], in0=bank[0:64, 0:TG], in1=sin2[:, ts], op=ALU.mult),
                   reads=bank.res + sin2.res, writes=[t2.res[s]])
            sch.op("pool", lambda e, s=s, ts=ts: e.tensor_tensor(out=krT[:, ts], in0=t1[:, s], in1=t2[:, s], op=ALU.add),
                   reads=[t1.res[s], t2.res[s]], writes=[krT.res[tg]])
        sch.release(m1)
        wq = sch.sb([128, 3, 192], BF16)
        wqr = sch.sb([128, 3, 64], BF16)
        wkv = sch.sb([128, 2, 256], BF16)
        kT = sch.sb([128, S], BF16, nres=NTG)
        vh = sch.sb([128, nt, 128], BF16, nres=nt)
        qn = sch.sb([128, S], BF16, nres=NTG)
        qr = sch.sb([64, S], BF16, nres=NTG)
        oT = sch.sb([128, S], BF16)
        pT = sch.sb([128, 4, 512], BF16, nres=4)
        rec = sch.sb([128, 2, 512], F32, nres=2)
        acc = sch.sb([128, 2, 512], F32, nres=2)
        accb = sch.sb([128, 2, 512], BF16, nres=2)
        t1h = sch.sb([64, 2, TG], F32, nres=2)
        t2h = sch.sb([64, 2, TG], F32, nres=2)
        pit = 0
        for h in range(8):
            sch.dma("pool", wq[:], self.w_uq[l][:, h * 192:(h + 1) * 192].rearrange("(kc p) f -> p kc f", p=128), writes=wq.res)
            sch.dma("pool", wkv[:], self.w_ukv[l][:, h * 256:(h + 1) * 256].rearrange("(kc p) f -> p kc f", p=128), writes=wkv.res)
            sch.op("pool", lambda e: e.tensor_scalar(out=wqr[:, :, 0:32], in0=wq[:, :, 160:192], scalar1=-1.0, scalar2=None, op0=ALU.mult),
                   reads=wq.res, writes=wqr.res)
            sch.op("pool", lambda e: e.tensor_copy(out=wqr[:, :, 32:64], in_=wq[:, :, 128:160]), reads=wq.res, writes=wqr.res)
            for tg in range(NTG):
                ts = slice(tg * TG, (tg + 1) * TG)
                s = tg % 2
                bk, bq, br, br2 = PS[0], PS[1], PS[2], PS[3]
                for kc in range(2):
                    sch.op("pe", lambda e, kc=kc, ts=ts: e.matmul(bk[:, 0:TG], lhsT=wkv[:, kc, 0:128], rhs=ckvn[:, kc, ts], start=(kc == 0), stop=(kc == 1)),
                           reads=wkv.res + [ckvn.res[tg]], writes=bk.res)
                sch.op("act", lambda e, ts=ts: e.copy(out=kT[:, ts], in_=bk[:, 0:TG]), reads=bk.res, writes=[kT.res[tg]])
                for kc in range(3):
                    sch.op("pe", lambda e, kc=kc, ts=ts: e.matmul(bq[:, 0:TG], lhsT=wq[:, kc, 0:128], rhs=cqn[:, kc, ts], start=(kc == 0), stop=(kc == 2)),
                           reads=wq.res + [cqn.res[tg]], writes=bq.res)
                sch.op("dve", lambda e, ts=ts: e.tensor_copy(out=qn[:, ts], in_=bq[:, 0:TG]), reads=bq.res, writes=[qn.res[tg]])
                for kc in range(3):
                    sch.op("pe", lambda e, kc=kc, ts=ts: e.matmul(br[0:64, 0:TG], lhsT=wq[:, kc, 128:192], rhs=cqn[:, kc, ts], start=(kc == 0), stop=(kc == 2)),
                           reads=wq.res + [cqn.res[tg]], writes=br.res)
                for kc in range(3):
                    sch.op("pe", lambda e, kc=kc, ts=ts: e.matmul(br2[0:64, 0:TG], lhsT=wqr[:, kc, :], rhs=cqn[:, kc, ts], start=(kc == 0), stop=(kc == 2)),
                           reads=wqr.res + [cqn.res[tg]], writes=br2.res)
                sch.op("dve", lambda e, s=s, ts=ts: e.tensor_tensor(out=t1h[:, s], in0=br[0:64, 0:TG], in1=cos2[:, ts], op=ALU.mult),
                       reads=br.res + cos2.res, writes=[t1h.res[s]])
                sch.op("dve", lambda e, s=s, ts=ts: e.tensor_tensor(out=t2h[:, s], in0=br2[0:64, 0:TG], in1=sin2[:, ts], op=ALU.mult),
                       reads=br2.res + sin2.res, writes=[t2h.res[s]])
                sch.op("pool", lambda e, s=s, ts=ts: e.tensor_tensor(out=qr[:, ts], in0=t1h[:, s], in1=t2h[:, s], op=ALU.add),
                       reads=[t1h.res[s], t2h.res[s]], writes=[qr.res[tg]])
            for g in range((nt + 3) // 4):
                bank = PS[4 + g % 2]
                na = min(4, nt - g * 4)
                for a in range(na):
                    tt = g * 4 + a
                    tg = (tt * 128) // TG
                    for kc in range(2):
                        sch.op("pe", lambda e, kc=kc, tt=tt, a=a, bank=bank: e.matmul(bank[:, a * 128:(a + 1) * 128], lhsT=ckvn[:, kc, tt * 128:(tt + 1) * 128],
                                                                                   rhs=wkv[:, kc, 128:256], start=(kc == 0), stop=(kc == 1)),
                               reads=wkv.res + [ckvn.res[tg]], writes=bank.res)
                sch.op("act", lambda e, g=g, na=na, bank=bank: e.copy(out=vh[:, g * 4:g * 4 + na, :], in_=bank[:, 0:na * 128].rearrange("p (a v) -> p a v", v=128)),
                       reads=bank.res, writes=vh.res[g * 4:g * 4 + na])
            GQ = min(4, nt)
            for G in range(nt // GQ):
                W = GQ * 128
                q0 = G * W
                qtg = q0 // TG
                po, pd = PS[4 + 2 * (G % 2)], PS[5 + 2 * (G % 2)]
                nkb = G * GQ + GQ
                slots = {}

                def scores(j, G=G, W=W, q0=q0, qtg=qtg):
                    nonlocal pit
                    a = max(0, j - G * GQ)
                    c0 = a * 128
                    s = pit % 4
                    pit += 1
                    slots[j] = (s, c0)
                    bank = PS[s % 4]
                    ks = slice(j * 128, (j + 1) * 128)
                    ktg = (j * 128) // TG
                    qs = slice(q0 + c0, q0 + W)
                    sch.op("pe", lambda e: e.matmul(bank[:, c0:W], lhsT=kT[:, ks], rhs=qn[:, qs], start=True, stop=False),
                           reads=[kT.res[ktg], qn.res[qtg]], writes=bank.res)
                    sch.op("pe", lambda e: e.matmul(bank[:, c0:W], lhsT=krT[:, ks], rhs=qr[:, qs], start=False, stop=True),
                           reads=[krT.res[ktg], qr.res[qtg]], writes=bank.res)
                    sch.op("act", lambda e: e.activation(out=pT[:, s, c0:W], in_=bank[:, c0:W], func=AF.Exp, scale=scale),
                           reads=bank.res, writes=[pT.res[s]])
                    if j >= G * GQ:
                        sch.op("pool", lambda e: e.tensor_tensor(out=pT[:, s, c0:c0 + 128], in0=pT[:, s, c0:c0 + 128], in1=tri[:], op=ALU.mult),
                               reads=[pT.res[s]] + tri.res, writes=[pT.res[s]])

                g2 = G % 2

                def pv(j, W=W, po=po, nkb=nkb, g2=g2):
                    s, c0 = slots[j]
                    sch.op("pe", lambda e: e.matmul(po[:, c0:W], lhsT=vh[:, j, :], rhs=pT[:, s, c0:W], start=(j == 0), stop=(j == nkb - 1)),
                           reads=[vh.res[j], pT.res[s]], writes=po.res)
                    if j == 0:
                        sch.op("dve", lambda e: e.tensor_copy(out=acc[:, g2, 0:W], in_=pT[:, s, 0:W]), reads=[pT.res[s]], writes=[acc.res[g2]])
                    else:
                        sch.op("dve", lambda e: e.tensor_tensor(out=acc[:, g2, c0:W], in0=acc[:, g2, c0:W], in1=pT[:, s, c0:W], op=ALU.add),
                               reads=[pT.res[s], acc.res[g2]], writes=[acc.res[g2]])

                scores(0)
                if nkb > 1:
                    scores(1)
                for j in range(nkb):
                    if j + 2 < nkb:
                        scores(j + 2)
                    pv(j)
                s2 = G % 2
                sch.op("pool", lambda e, s2=s2, W=W: e.tensor_copy(out=accb[:, s2, 0:W], in_=acc[:, s2, 0:W]), reads=[acc.res[s2]], writes=[accb.res[s2]])
                sch.op("pe", lambda e, s2=s2, pd=pd, W=W: e.matmul(pd[:, 0:W], lhsT=self.ones_bf[:], rhs=accb[:, s2, 0:W], start=True, stop=True),
                       reads=self.ones_bf.res + [accb.res[s2]], writes=pd.res)
                sch.op("dve", lambda e, s2=s2, pd=pd, W=W: e.reciprocal(out=rec[:, s2, 0:W], in_=pd[:, 0:W]), reads=pd.res, writes=[rec.res[s2]])
                sch.op("dve", lambda e, s2=s2, po=po, W=W, q0=q0: e.tensor_tensor(out=oT[:, q0:q0 + W], in0=po[:, 0:W], in1=rec[:, s2, 0:W], op=ALU.mult),
                       reads=po.res + [rec.res[s2]], writes=oT.res)
            sch.dma("sp", self.obr[2, h], oT[:], reads=oT.res, writes=[self.obr_res[2][h]])
        sch.release(m)

    def stage_rwkv(self, l):
        sch, S = self.sch, self.S
        T = min(S, 512)
        NBK = S // T
        NC = T // 64
        NQ = NC // 4
        PS = self.PS
        C0 = 0.6065306597126334
        m = sch.mark()
        blk = sch.sb([128, 128], BF16)
        blkf = sch.sb([128, 128], F32)
        mus = sch.sb([128, 128], F32)
        mui = sch.sb([128, 128], F32)
        mls = sch.sb([128, 128], F32)
        mask64 = sch.sb([128, T], F32)
        tiny = sch.sb([128, 1], F32)
        eps_ln = sch.sb([128, 1], F32)
        sch.op("pool", lambda e: e.memset(tiny[:], 1e-12), writes=tiny.res)
        sch.op("pool", lambda e: e.memset(eps_ln[:], 64e-5), writes=eps_ln.res)
        sch.op("pool", lambda e: e.memset(blkf[:], 0.0), writes=blkf.res)
        sch.op("pool", lambda e: e.memset(blkf[0:64, 0:64], 1.0), writes=blkf.res)
        sch.op("pool", lambda e: e.memset(blkf[64:128, 64:128], 1.0), writes=blkf.res)
        sch.op("pool", lambda e: e.tensor_copy(out=blk[:], in_=blkf[:]), reads=blkf.res, writes=blk.res)
        for (mt, op_, base, cm, pat) in ((mus, ALU.is_gt, 0, -1, 1), (mui, ALU.is_ge, 0, -1, 1), (mls, ALU.is_gt, 0, 1, -1)):
            sch.op("pool", lambda e, mt=mt: e.memset(mt[:], 1.0), writes=mt.res)
            sch.op("pool", lambda e, mt=mt, op_=op_, base=base, cm=cm, pat=pat: e.affine_select(
                out=mt[:], in_=mt[:], pattern=[[pat, 128]], compare_op=op_, fill=0.0, base=base, channel_multiplier=cm),
                reads=mt.res, writes=mt.res)
        sch.op("pool", lambda e: e.memset(mask64[:], 1.0), writes=mask64.res)
        sch.op("pool", lambda e: e.memset(mask64[:].rearrange("p (c i) -> p c i", i=64)[:, :, 0:1], 0.0), writes=mask64.res)

        def shift(dst_ap, zl, mu_ap, np_, eng_r, dt):
            sch.op("dve", lambda e: e.tensor_tensor(out=dt[0:np_, 0:T], in0=zl[0:np_, 0:T], in1=zl[0:np_, 1:T + 1], op=ALU.subtract),
                   reads=zl.res, writes=dt.res)
            sch.op("dve", lambda e: e.scalar_tensor_tensor(out=dst_ap, in0=dt[0:np_, 0:T], scalar=mu_ap, in1=zl[0:np_, 1:T + 1], op0=ALU.mult, op1=ALU.add),
                   reads=zl.res + dt.res + self.vec[l].res, writes=eng_r)

        def load_prev(zl, zc, t0, np_=128, p0=0):
            if t0 == 0:
                sch.op("pool", lambda e: e.memset(zl[p0:p0 + np_, 0:1], 0.0), writes=zl.res)
                sch.dma("sp", zl[p0:p0 + np_, 1:T + 1], self.zT[zc, p0:p0 + np_, 0:T], reads=[self.zT_res[zc]], writes=zl.res)
            else:
                sch.dma("sp", zl[p0:p0 + np_, 0:T + 1], self.zT[zc, p0:p0 + np_, t0 - 1:t0 + T], reads=[self.zT_res[zc]], writes=zl.res)

        tw = sch.sb([128, S], BF16)
        sgg = sch.sb([128, S], BF16)
        zvv = sch.sb([32, S], BF16)
        m0 = sch.mark()
        zl0 = [sch.sb([128, T + 1], F32) for _ in range(2)]
        tmp0 = [sch.sb([128, T], F32) for _ in range(2)]
        dt0 = sch.sb([128, T], F32)
        for tb in range(NBK):
            t0 = tb * T
            zl, tmp = zl0[tb % 2], tmp0[tb % 2]
            load_prev(zl, 56, t0)
            shift(tmp[:, :], zl, self.V(l, "mu", 24), 128, tmp.res, dt0)
            sch.op("act", lambda e, tmp=tmp, t0=t0: e.activation(out=tw[0:64, t0:t0 + T], in_=tmp[0:64, :], func=AF.Tanh), reads=tmp.res, writes=tw.res)
            sch.op("pool", lambda e, tmp=tmp, t0=t0: e.tensor_copy(out=tw[64:128, t0:t0 + T], in_=tmp[64:128, :]), reads=tmp.res, writes=tw.res)
        for tb in range(NBK):
            t0 = tb * T
            zl, tmp = zl0[tb % 2], tmp0[tb % 2]
            load_prev(zl, 57, t0)
            shift(tmp[:, :], zl, self.V(l, "mu", 25), 128, tmp.res, dt0)
            sch.op("act", lambda e, tmp=tmp, t0=t0: e.activation(out=sgg[:, t0:t0 + T], in_=tmp[:, :], func=AF.Sigmoid), reads=tmp.res, writes=sgg.res)
        if l > 0:
            for tb in range(NBK):
                t0 = tb * T
                zl, tmp = zl0[tb % 2], tmp0[tb % 2]
                load_prev(zl, 88, t0, 32)
                shift(tmp[0:32, :], zl, self.V(l, "vres_mu")[0:32, :], 32, tmp.res, dt0)
                sch.op("act", lambda e, tmp=tmp, t0=t0: e.copy(out=zvv[:, t0:t0 + T], in_=tmp[0:32, :]), reads=tmp.res, writes=zvv.res)
        sch.release(m0)
        f32n = ["zr", "zk", "zv"]
        zls = {n: sch.sb([128, T + 1], F32) for n in f32n}
        A = {n: sch.sb([128, T], F32) for n in ("rs", "ks", "v", "sig", "a", "kkn", "kmod", "b", "cw", "e2", "t1", "t2", "vf")}
        Bb = {n: sch.sb([128, T], BF16) for n in ("kk2", "rkb")}
        P2 = [{n: sch.sb([128, NC, 128], BF16) for n in ("a", "r", "b", "k", "B", "K", "V")} for _ in range(2)]
        for Pq in P2:
            for n in Pq:
                sch.op("pool", lambda e, n=n, Pq=Pq: e.memset(Pq[n][:], 0.0), writes=Pq[n].res)
        E1 = [sch.sb([128, T], F32) for _ in range(2)]
        GB3 = [dict(g=sch.sb([128, T], F32), bonus=sch.sb([128, T], F32)) for _ in range(3)]
        Q = {n: sch.sb([128, 2, 4, 128], BF16, nres=2) for n in ("Ta", "TB", "TK", "TV", "NT", "Nn", "Aak", "Arb", "Ark", "nA0", "nA1", "nB0", "nB1",
                                                               "PT0", "PT1")}
        Q["Ap"], Q["X"], Q["Vp"] = Q["nA1"], Q["nB0"], Q["nB1"]
        Sbd = sch.sb([128, 2, 128], BF16, nres=2)
        wup = sch.sb([128, 128], BF16)
        gup = sch.sb([128, 128], BF16)
        vup = sch.sb([32, 128], BF16)
        omka = sch.sb([128, 1], F32)
        bankc = [0, 0]

        def nb():
            bankc[0] += 1
            return PS[bankc[0] % 6]

        def nbp():
            bankc[1] += 1
            return PS[6 + bankc[1] % 2]

        evc = [0]

        def evac_copy(out_ap, in_ap, reads, writes):
            sch.op("act", lambda e: e.copy(out=out_ap, in_=in_ap), reads=reads, writes=writes)

        def v4(bank, bf=False):
            if bf:
                return bank[:].bitcast(BF16)[:, 0:512].rearrange("p (a q) -> p a q", q=128)
            return bank[:, :].rearrange("p (a q) -> p a q", q=128)

        def bc4(t):
            return t[:].unsqueeze(1).broadcast_to([128, 4, 128])

        D2 = [dict(Yp=sch.sb([128, NC, 128], F32), Gs=sch.sb([128, NC, 128], F32), MT=sch.sb([128, NC, 128], BF16), RT=sch.sb([128, NC, 128], BF16),
                   yT=sch.sb([128, T], F32), ob=sch.sb([128, T], BF16)) for _ in range(2)]
        PT_ = dict(t1=sch.sb([128, T], F32), t2=sch.sb([128, T], F32), ybf=Bb["kk2"], ysq=Bb["rkb"])
        sidx = [0]
        pending = None
        itc = [0]

        def quad_gen(qi, Dd, e1tile, Pp):
            e13 = e1tile[:].rearrange("p (c i) -> p c i", i=64)
            s = qi % 2
            cs = slice(qi * 4, qi * 4 + 4)

            def mm4(bank, L, Rr, lres, rres, start=True, stop=True, bf=False, tr=False):
                for a in range(4):
                    la = L(a)
                    if tr:
                        ov = bank[:].bitcast(BF16)[:, a * 128:(a + 1) * 128]
                        sch.op("pe", lambda e, ov=ov, la=la: e.transpose(out=ov, in_=la, identity=self.ident_bf[:]), reads=lres + self.ident_bf.res, writes=bank.res)
                    else:
                        ra = Rr(a)
                        sch.op("pe", lambda e, a=a, la=la, ra=ra: e.matmul(bank[:, a * 128:(a + 1) * 128], lhsT=la, rhs=ra, start=start, stop=stop),
                               reads=lres + rres, writes=bank.res)

            def pq(n):
                return lambda a, n=n, qi=qi: Pp[n][:, qi * 4 + a, :]

            def qq(n):
                return lambda a, n=n, s=s: Q[n][:, s, a, :]

            for src, dst in (("a", "Ta"), ("B", "TB"), ("K", "TK"), ("V", "TV")):
                bank = nb()
                mm4(bank, pq(src), None, Pp[src].res, [], tr=True)
                evac_copy(Q[dst][:, s], v4(bank, True), bank.res, [Q[dst].res[s]])
                yield
            for Ln, Rn, mk, dst in (("b", "a", mus, "NT"), ("a", "b", mls, "Nn"), ("k", "a", mus, "Aak"), ("b", "r", mui, "Arb"), ("k", "r", mui, "Ark")):
                bank = nb()
                mm4(bank, pq(Ln), pq(Rn), Pp[Ln].res, Pp[Rn].res)
                sch.op("dve", lambda e, s=s, cs=cs, qi=qi, bank=bank, dst=dst, mk=mk: e.tensor_tensor(out=Q[dst][:, s], in0=v4(bank), in1=bc4(mk), op=ALU.mult),
                       reads=bank.res + mk.res, writes=[Q[dst].res[s]])
            sch.op("pool", lambda e, s=s, cs=cs, qi=qi: e.tensor_tensor(out=Q["PT0"][:, s], in0=Q["NT"][:, s], in1=bc4(self.ident_bf), op=ALU.add),
                   reads=[Q["NT"].res[s]] + self.ident_bf.res, writes=[Q["PT0"].res[s]])
            curT, cur = "NT", "Nn"
            for lv in range(5):
                nA, nB = f"nA{lv % 2}", f"nB{lv % 2}"
                pin, pout = f"PT{lv % 2}", f"PT{(lv + 1) % 2}"
                bank = nb()
                mm4(bank, qq(curT), qq(cur), [Q[curT].res[s]], [Q[cur].res[s]])
                evac_copy(Q[nA][:, s], v4(bank), bank.res, [Q[nA].res[s]])
                yield
                if lv < 4:
                    bank = nb()
                    mm4(bank, qq(cur), qq(curT), [Q[cur].res[s]], [Q[curT].res[s]])
                    evac_copy(Q[nB][:, s], v4(bank), bank.res, [Q[nB].res[s]])
                    yield
                bank = nb()
                for a in range(4):
                    sch.op("pe", lambda e, a=a, bank=bank, pin=pin: e.matmul(bank[:, a * 128:(a + 1) * 128], lhsT=self.ident_bf[:], rhs=Q[pin][:, s, a, :], start=True, stop=False),
                           reads=self.ident_bf.res + [Q[pin].res[s]], writes=bank.res)
                    sch.op("pe", lambda e, a=a, bank=bank, pin=pin, nA=nA: e.matmul(bank[:, a * 128:(a + 1) * 128], lhsT=Q[nA][:, s, a, :], rhs=Q[pin][:, s, a, :], start=False, stop=True),
                           reads=[Q[nA].res[s], Q[pin].res[s]], writes=bank.res)
                evac_copy(Q[pout][:, s], v4(bank), bank.res, [Q[pout].res[s]])
                yield
                curT, cur = nB, nA
            TT = "PT1"
            bank = nb()
            mm4(bank, qq(TT), qq("Ta"), [Q[TT].res[s]], [Q["Ta"].res[s]])
            evac_copy(Q["Ap"][:, s], v4(bank), bank.res, [Q["Ap"].res[s]])
            yield
            bank = nb()
            mm4(bank, qq("Aak"), qq("TV"), [Q["Aak"].res[s]], [Q["TV"].res[s]])
            evac_copy(Q["X"][:, s], v4(bank), bank.res, [Q["X"].res[s]])
            yield
            bank = nb()
            mm4(bank, qq(TT), qq("X"), [Q[TT].res[s]], [Q["X"].res[s]])
            evac_copy(Q["Vp"][:, s], v4(bank), bank.res, [Q["Vp"].res[s]])
            yield
            bank = nb()
            for a in range(4):
                sch.op("pe", lambda e, s=s, cs=cs, qi=qi, a=a, bank=bank: e.matmul(bank[:, a * 128:(a + 1) * 128], lhsT=Q["Vp"][:, s, a, :], rhs=Q["Arb"][:, s, a, :], start=True, stop=False),
                       reads=[Q["Vp"].res[s], Q["Arb"].res[s]], writes=bank.res)
                sch.op("pe", lambda e, s=s, cs=cs, qi=qi, a=a, bank=bank: e.matmul(bank[:, a * 128:(a + 1) * 128], lhsT=Q["TV"][:, s, a, :], rhs=Q["Ark"][:, s, a, :], start=False, stop=True),
                       reads=[Q["TV"].res[s], Q["Ark"].res[s]], writes=bank.res)
            evac_copy(Dd["Yp"][:, cs, :], v4(bank), bank.res, Dd["Yp"].res)
            yield
            bank = nb()
            for a in range(4):
                sch.op("pe", lambda e, s=s, cs=cs, qi=qi, a=a, bank=bank: e.matmul(bank[:, a * 128:(a + 1) * 128], lhsT=Q["TB"][:, s, a, :], rhs=Q["Vp"][:, s, a, :], start=True, stop=False),
                       reads=[Q["TB"].res[s], Q["Vp"].res[s]], writes=bank.res)
                sch.op("pe", lambda e, s=s, cs=cs, qi=qi, a=a, bank=bank: e.matmul(bank[:, a * 128:(a + 1) * 128], lhsT=Q["TK"][:, s, a, :], rhs=Q["TV"][:, s, a, :], start=False, stop=True),
                       reads=[Q["TK"].res[s], Q["TV"].res[s]], writes=bank.res)
            evac_copy(Dd["Gs"][:, cs, :], v4(bank), bank.res, Dd["Gs"].res)
            yield
            bank = nb()
            mm4(bank, qq("Ap"), qq("TB"), [Q["Ap"].res[s]], [Q["TB"].res[s]])
            for a in range(4):
                c = qi * 4 + a
                sch.op("dve", lambda e, s=s, cs=cs, qi=qi, a=a, c=c, bank=bank, e13=e13: e.scalar_tensor_tensor(out=Dd["MT"][:, c, :], in0=self.ident_f[:], scalar=e13[:, c, 63:64],
                                                                                           in1=bank[:, a * 128:(a + 1) * 128], op0=ALU.mult, op1=ALU.add),
                       reads=bank.res + self.ident_f.res + e1tile.res, writes=Dd["MT"].res)
            bank = nb()
            for a in range(4):
                sch.op("pe", lambda e, a=a, bank=bank: e.matmul(bank[:, a * 128:(a + 1) * 128], lhsT=self.ident_bf[:], rhs=Pp["r"][:, qi * 4 + a, :], start=True, stop=False),
                       reads=self.ident_bf.res + Pp["r"].res, writes=bank.res)
                sch.op("pe", lambda e, a=a, bank=bank: e.matmul(bank[:, a * 128:(a + 1) * 128], lhsT=Q["Ap"][:, s, a, :], rhs=Q["Arb"][:, s, a, :], start=False, stop=True),
                       reads=[Q["Ap"].res[s], Q["Arb"].res[s]], writes=bank.res)
            evac_copy(Dd["RT"][:, cs, :], v4(bank), bank.res, Dd["RT"].res)

        def seq_gen(Dd, reset):
            if reset:
                cur0 = sidx[0] % 2
                sch.op("pool", lambda e: e.memset(Sbd[:, cur0], 0.0), writes=[Sbd.res[cur0]])
            for c in range(NC):
                cur = sidx[0] % 2
                nxt = (sidx[0] + 1) % 2
                sidx[0] += 1
                by, bs = nb(), nb()
                sch.op("pe", lambda e, by=by, cur=cur, c=c: e.matmul(by[:, 0:128], lhsT=Sbd[:, cur], rhs=Dd["RT"][:, c, :], start=True, stop=True),
                       reads=[Sbd.res[cur]] + Dd["RT"].res, writes=by.res)
                sch.op("pe", lambda e, bs=bs, cur=cur, c=c: e.matmul(bs[:, 0:128], lhsT=Dd["MT"][:, c, :], rhs=Sbd[:, cur], start=True, stop=True),
                       reads=[Sbd.res[cur]] + Dd["MT"].res, writes=bs.res)
                sch.op("dve", lambda e, bs=bs, nxt=nxt, c=c: e.tensor_tensor(out=Sbd[:, nxt], in0=bs[:, 0:128], in1=Dd["Gs"][:, c, :], op=ALU.add),
                       reads=bs.res + Dd["Gs"].res, writes=[Sbd.res[nxt]])
                for hh in range(2):
                    ps_ = slice(hh * 64, hh * 64 + 64)
                    sch.op("dve", lambda e, by=by, ps_=ps_, c=c, hh=hh: e.tensor_tensor(out=Dd["yT"][ps_, c * 64:(c + 1) * 64], in0=by[ps_, hh * 64:hh * 64 + 64],
                                                                                  in1=Dd["Yp"][ps_, c, hh * 64:hh * 64 + 64], op=ALU.add),
                           reads=by.res + Dd["Yp"].res, writes=Dd["yT"].res)
                yield

        def post(Dd, j, tsl, gb):
            sch.op("act", lambda e: e.copy(out=PT_["ybf"][:], in_=Dd["yT"][:]), reads=Dd["yT"].res, writes=PT_["ybf"].res)
            sch.op("act", lambda e: e.activation(out=PT_["ysq"][:], in_=Dd["yT"][:], func=AF.Square), reads=Dd["yT"].res, writes=PT_["ysq"].res)
            bm, bq = nb(), nb()
            sch.op("pe", lambda e, bm=bm: e.matmul(bm[:, 0:T], lhsT=blk[:], rhs=PT_["ybf"][:], start=True, stop=True), reads=blk.res + PT_["ybf"].res, writes=bm.res)
            sch.op("pe", lambda e, bq=bq: e.matmul(bq[:, 0:T], lhsT=blk[:], rhs=PT_["ysq"][:], start=True, stop=True), reads=blk.res + PT_["ysq"].res, writes=bq.res)
            sch.op("act", lambda e, bm=bm: e.activation(out=PT_["t1"][:], in_=bm[:, 0:T], func=AF.Square, scale=1.0 / 64), reads=bm.res, writes=PT_["t1"].res)
            sch.op("dve", lambda e, bq=bq: e.scalar_tensor_tensor(out=PT_["t1"][:], in0=bq[:, 0:T], scalar=1.0 / 64, in1=PT_["t1"][:], op0=ALU.mult, op1=ALU.subtract),
                   reads=bq.res + PT_["t1"].res, writes=PT_["t1"].res)
            sch.op("act", lambda e: e.activation(out=PT_["t1"][:], in_=PT_["t1"][:], func=AF.Ln, bias=eps_ln[:, 0:1]), reads=PT_["t1"].res + eps_ln.res, writes=PT_["t1"].res)
            sch.op("act", lambda e: e.activation(out=PT_["t1"][:], in_=PT_["t1"][:], func=AF.Exp, scale=-0.5), reads=PT_["t1"].res, writes=PT_["t1"].res)
            sch.op("dve", lambda e, bm=bm: e.scalar_tensor_tensor(out=PT_["t2"][:], in0=bm[:, 0:T], scalar=-1.0 / 64, in1=Dd["yT"][:], op0=ALU.mult, op1=ALU.add),
                   reads=bm.res + Dd["yT"].res, writes=PT_["t2"].res)
            sch.op("dve", lambda e, j=j: e.scalar_tensor_tensor(out=PT_["t2"][:], in0=PT_["t2"][:], scalar=self.V(l, "lnx_g", j), in1=PT_["t1"][:], op0=ALU.mult, op1=ALU.mult),
                   reads=PT_["t2"].res + PT_["t1"].res + self.vec[l].res, writes=PT_["t2"].res)
            sch.op("dve", lambda e, j=j: e.scalar_tensor_tensor(out=PT_["t2"][:], in0=PT_["t2"][:], scalar=self.V(l, "lnx_b", j), in1=gb["bonus"][:], op0=ALU.add, op1=ALU.add),
                   reads=PT_["t2"].res + gb["bonus"].res + self.vec[l].res, writes=PT_["t2"].res)
            sch.op("dve", lambda e: e.tensor_tensor(out=Dd["ob"][:], in0=PT_["t2"][:], in1=gb["g"][:], op=ALU.mult), reads=PT_["t2"].res + gb["g"].res, writes=Dd["ob"].res)
            sch.dma("sp", self.obr[1, j, :, tsl], Dd["ob"][:], reads=Dd["ob"].res, writes=[self.obr_res[1][j]])

        def prep_gen(j, tb, Dd, Pp, e1t, gb):
            t0 = tb * T
            tsl = slice(t0, t0 + T)
            if tb == 0:
                fs = slice(j * 128, (j + 1) * 128)
                sch.dma("pool", wup[0:64, :], self.w_up[l][:, fs], writes=wup.res)
                sch.dma("pool", wup[64:128, :], self.a_up[l][:, fs], writes=wup.res)
                sch.dma("pool", gup[:], self.g_up[l][:, fs], writes=gup.res)
                if l > 0:
                    sch.dma("pool", vup[:], self.v_up[:, fs], writes=vup.res)
                sch.op("dve", lambda e: e.tensor_scalar(out=omka[:], in0=self.V(l, "k_a", j), scalar1=-1.0, scalar2=1.0, op0=ALU.mult, op1=ALU.add),
                       reads=self.vec[l].res, writes=omka.res)
            for n, zc, dst in (("zr", 32 + j, "rs"), ("zk", 40 + j, "ks"), ("zv", 48 + j, "v")):
                load_prev(zls[n], zc, t0)
                shift(A[dst][:, :], zls[n], self.V(l, "mu", zc - 32), 128, A[dst].res, A["t1"])
            b0, b1, b2, b3 = nbp(), nbp(), nbp(), nbp()
            sch.op("pe", lambda e, b0=b0, tsl=tsl: e.matmul(b0[:, 0:T], lhsT=wup[0:64, :], rhs=tw[0:64, tsl], start=True, stop=True),
                   reads=wup.res + tw.res, writes=b0.res)
            sch.op("act", lambda e, b0=b0, j=j: e.activation(out=A["sig"][:], in_=b0[:, 0:T], func=AF.Sigmoid, bias=self.V(l, "w0", j)),
                   reads=b0.res + self.vec[l].res, writes=A["sig"].res)
            yield
            sch.op("pe", lambda e, b1=b1, tsl=tsl: e.matmul(b1[:, 0:T], lhsT=wup[64:128, :], rhs=tw[64:128, tsl], start=True, stop=True),
                   reads=wup.res + tw.res, writes=b1.res)
            sch.op("act", lambda e, b1=b1, j=j: e.activation(out=A["a"][:], in_=b1[:, 0:T], func=AF.Sigmoid, bias=self.V(l, "a0", j)),
                   reads=b1.res + self.vec[l].res, writes=A["a"].res)
            yield
            sch.op("pe", lambda e, b2=b2, tsl=tsl: e.matmul(b2[:, 0:T], lhsT=gup[:], rhs=sgg[:, tsl], start=True, stop=True),
                   reads=gup.res + sgg.res, writes=b2.res)
            sch.op("act", lambda e, b2=b2, Dd=Dd: e.copy(out=gb["g"][:], in_=b2[:, 0:T]), reads=b2.res, writes=gb["g"].res)
            yield
            if l == 0:
                sch.dma("sp", self.vfirst[j, :, tsl], A["v"][:], reads=A["v"].res, writes=[self.vfirst_res[j]])
            else:
                sch.dma("sp", A["vf"][:], self.vfirst[j, :, tsl], reads=[self.vfirst_res[j]], writes=A["vf"].res)
                sch.op("pe", lambda e, b3=b3, tsl=tsl: e.matmul(b3[:, 0:T], lhsT=vup[0:32, :], rhs=zvv[0:32, tsl], start=True, stop=True),
                       reads=vup.res + zvv.res, writes=b3.res)
                sch.op("act", lambda e, b3=b3, j=j: e.activation(out=A["t1"][:], in_=b3[:, 0:T], func=AF.Sigmoid, bias=self.V(l, "v0", j)),
                       reads=b3.res + self.vec[l].res, writes=A["t1"].res)
                sch.op("pool", lambda e: e.tensor_tensor(out=A["vf"][:], in0=A["vf"][:], in1=A["v"][:], op=ALU.subtract),
                       reads=A["vf"].res + A["v"].res, writes=A["vf"].res)
                sch.op("dve", lambda e: e.tensor_tensor(out=A["vf"][:], in0=A["vf"][:], in1=A["t1"][:], op=ALU.mult),
                       reads=A["vf"].res + A["t1"].res, writes=A["vf"].res)
                sch.op("pool", lambda e: e.tensor_tensor(out=A["v"][:], in0=A["v"][:], in1=A["vf"][:], op=ALU.add),
                       reads=A["vf"].res + A["v"].res, writes=A["v"].res)
            sch.op("act", lambda e, j=j: e.activation(out=Bb["kk2"][:], in_=A["ks"][:], func=AF.Square, scale=self.V(l, "k_k", j)),
                   reads=A["ks"].res + self.vec[l].res, writes=Bb["kk2"].res)
            b4 = nbp()
            sch.op("pe", lambda e, b4=b4: e.matmul(b4[:, 0:T], lhsT=blk[:], rhs=Bb["kk2"][:], start=True, stop=True), reads=blk.res + Bb["kk2"].res, writes=b4.res)
            yield
            sch.op("act", lambda e, b4=b4: e.activation(out=A["t2"][:], in_=b4[:, 0:T], func=AF.Ln, bias=tiny[:, 0:1]), reads=b4.res + tiny.res, writes=A["t2"].res)
            sch.op("act", lambda e: e.activation(out=A["t2"][:], in_=A["t2"][:], func=AF.Exp, scale=-0.5), reads=A["t2"].res, writes=A["t2"].res)
            yield
            sch.op("dve", lambda e, j=j: e.scalar_tensor_tensor(out=A["kkn"][:], in0=A["ks"][:], scalar=self.V(l, "k_k", j), in1=A["t2"][:], op0=ALU.mult, op1=ALU.mult),
                   reads=A["ks"].res + A["t2"].res + self.vec[l].res, writes=A["kkn"].res)
            sch.op("dve", lambda e, j=j: e.tensor_scalar(out=A["t2"][:], in0=A["a"][:], scalar1=self.V(l, "k_a", j), scalar2=omka[:, 0:1], op0=ALU.mult, op1=ALU.add),
                   reads=A["a"].res + omka.res + self.vec[l].res, writes=A["t2"].res)
            yield
            sch.op("dve", lambda e: e.tensor_tensor(out=A["kmod"][:], in0=A["ks"][:], in1=A["t2"][:], op=ALU.mult), reads=A["ks"].res + A["t2"].res, writes=A["kmod"].res)
            sch.op("pool", lambda e: e.tensor_tensor(out=A["b"][:], in0=A["kkn"][:], in1=A["a"][:], op=ALU.mult), reads=A["kkn"].res + A["a"].res, writes=A["b"].res)
            yield
            sch.op("pool", lambda e: e.tensor_tensor(out=A["t2"][:], in0=A["rs"][:], in1=A["kmod"][:], op=ALU.mult), reads=A["rs"].res + A["kmod"].res, writes=A["t2"].res)
            sch.op("dve", lambda e, j=j: e.tensor_scalar(out=Bb["rkb"][:], in0=A["t2"][:], scalar1=self.V(l, "r_k", j), scalar2=None, op0=ALU.mult),
                   reads=A["t2"].res + self.vec[l].res, writes=Bb["rkb"].res)
            yield
            b5 = nbp()
            sch.op("pe", lambda e, b5=b5: e.matmul(b5[:, 0:T], lhsT=blk[:], rhs=Bb["rkb"][:], start=True, stop=True), reads=blk.res + Bb["rkb"].res, writes=b5.res)
            sch.op("dve", lambda e, b5=b5, Dd=Dd: e.tensor_tensor(out=gb["bonus"][:], in0=b5[:, 0:T], in1=A["v"][:], op=ALU.mult), reads=b5.res + A["v"].res, writes=gb["bonus"].res)
            yield
            sch.op("dve", lambda e: e.tensor_tensor_scan(out=A["cw"][:], data0=mask64[:], data1=A["sig"][:], initial=0.0, op0=ALU.mult, op1=ALU.add),
                   reads=A["sig"].res + mask64.res, writes=A["cw"].res)
            sch.op("act", lambda e: e.activation(out=e1t[:], in_=A["cw"][:], func=AF.Exp, scale=-C0), reads=A["cw"].res, writes=e1t.res)
            yield
            sch.op("act", lambda e: e.activation(out=A["e2"][:], in_=A["cw"][:], func=AF.Exp, scale=C0), reads=A["cw"].res, writes=A["e2"].res)
            sch.op("pool", lambda e: e.tensor_tensor(out=A["t1"][:], in0=A["cw"][:], in1=A["sig"][:], op=ALU.subtract), reads=A["cw"].res + A["sig"].res, writes=A["t1"].res)
            yield
            sch.op("act", lambda e: e.activation(out=A["t1"][:], in_=A["t1"][:], func=AF.Exp, scale=-C0), reads=A["t1"].res, writes=A["t1"].res)
            cw3 = A["cw"][:].rearrange("p (c i) -> p c i", i=64)
            sch.op("dve", lambda e, cw3=cw3: e.tensor_tensor(out=A["t2"][:].rearrange("p (c i) -> p c i", i=64), in0=cw3[:, :, 63:64].broadcast_to([128, NC, 64]),
                                                            in1=cw3, op=ALU.subtract), reads=A["cw"].res, writes=A["t2"].res)
            yield
            sch.op("act", lambda e: e.activation(out=A["t2"][:], in_=A["t2"][:], func=AF.Exp, scale=-C0), reads=A["t2"].res, writes=A["t2"].res)

            def padw(dst, fn, reads):
                for hh in range(2):
                    ps_ = slice(hh * 64, hh * 64 + 64)
                    o_ap = Pp[dst][ps_, :, hh * 64:hh * 64 + 64]
                    sch.op("dve" if hh == 0 else "pool", lambda e, o_ap=o_ap, ps_=ps_: fn(e, o_ap, ps_), reads=reads, writes=Pp[dst].res)

            def v3(n, ps_):
                return A[n][ps_, :].rearrange("p (c i) -> p c i", i=64)

            padw("r", lambda e, o, ps_: e.tensor_tensor(out=o, in0=v3("rs", ps_), in1=e1t[ps_, :].rearrange("p (c i) -> p c i", i=64), op=ALU.mult), A["rs"].res + e1t.res)
            yield
            for hh in range(2):
                ps_ = slice(hh * 64, hh * 64 + 64)
                sch.op("dve", lambda e, ps_=ps_, hh=hh: e.scalar_tensor_tensor(out=Pp["a"][ps_, :, hh * 64:hh * 64 + 64], in0=v3("kkn", ps_), scalar=-1.0,
                                                                            in1=v3("t1", ps_), op0=ALU.mult, op1=ALU.mult),
                       reads=A["kkn"].res + A["t1"].res, writes=Pp["a"].res)
            padw("b", lambda e, o, ps_: e.tensor_tensor(out=o, in0=v3("b", ps_), in1=v3("e2", ps_), op=ALU.mult), A["b"].res + A["e2"].res)
            padw("k", lambda e, o, ps_: e.tensor_tensor(out=o, in0=v3("kmod", ps_), in1=v3("e2", ps_), op=ALU.mult), A["kmod"].res + A["e2"].res)
            yield
            padw("B", lambda e, o, ps_: e.tensor_tensor(out=o, in0=v3("b", ps_), in1=v3("t2", ps_), op=ALU.mult), A["b"].res + A["t2"].res)
            padw("K", lambda e, o, ps_: e.tensor_tensor(out=o, in0=v3("kmod", ps_), in1=v3("t2", ps_), op=ALU.mult), A["kmod"].res + A["t2"].res)
            yield
            padw("V", lambda e, o, ps_: e.tensor_copy(out=o, in_=v3("v", ps_)), A["v"].res)
            e13 = e1t[:].rearrange("p (c i) -> p c i", i=64)

        items = [(j, tb) for j in range(8) for tb in range(NBK)]
        for g_ in prep_gen(items[0][0], items[0][1], D2[0], P2[0], E1[0], GB3[0]):
            pass
        for it_, (j, tb) in enumerate(items):
            if True:
                t0 = tb * T
                tsl = slice(t0, t0 + T)
                Dd = D2[it_ % 2]
                gens = [quad_gen(qi, Dd, E1[it_ % 2], P2[it_ % 2]) for qi in range(NQ)]
                if pending is not None:
                    gens.append(seq_gen(pending[0], pending[3]))
                if it_ + 1 < len(items):
                    jn, tbn = items[it_ + 1]
                    gens.append(prep_gen(jn, tbn, D2[(it_ + 1) % 2], P2[(it_ + 1) % 2], E1[(it_ + 1) % 2], GB3[(it_ + 1) % 3]))
                while gens:
                    for g_ in list(gens):
                        try:
                            next(g_)
                        except StopIteration:
                            gens.remove(g_)
                if pending is not None:
                    post(pending[0], pending[1], pending[2], pending[4])
                pending = (Dd, j, tsl, tb == 0, GB3[it_ % 3])
        for g_ in seq_gen(pending[0], pending[3]):
            pass
        post(pending[0], pending[1], pending[2], pending[4])
        sch.release(m)

    def load_x(self):
        sch, TG = self.sch, self.TG
        for tg in range(self.NTG):
            ts = slice(tg * TG, (tg + 1) * TG)
            sch.dma("sp", self.xcur[:, :, ts], self.xT.rearrange("(c p) s -> c p s", p=128)[:, :, ts], writes=[self.xcur_res[tg]])

    def finish(self):
        sch = self.sch
        sch.barrier()
        fo = [d for d in sch.dma_last if d is not None]
        sch.emit(fo)
        return self.nc


def make_in_maps(inp, n_cores=8):
    f = lambda a: np.ascontiguousarray(np.asarray(a, np.float32))
    vecs = np.stack([pack_vecs(inp, l) for l in range(DEPTH)])
    shared = {
        "vecs": vecs, "w_in": f(inp["w_in"]), "w_vres": f(inp["w_vres_down"][0]),
        "w_up": f(inp["rwkv_w_up"]), "a_up": f(inp["rwkv_a_up"]), "g_up": f(inp["rwkv_g_up"]),
        "v_up": f(inp["rwkv_v_up"][0]), "w_uq": f(inp["mla_w_uq"]), "w_ukv": f(inp["mla_w_ukv"]),
        "w_branch": f(inp["w_branch"]), "w_out": f(inp["w_out"]), "w_ffn_in": f(inp["w_ffn_in"]),
        "w_ffn_out": f(inp["w_ffn_out"]),
    }
    x = np.asarray(inp["x"], np.float32)
    pos = np.asarray(inp["positions"], np.int32)
    maps = []
    for b in range(n_cores):
        m = dict(shared)
        m["xT"] = np.ascontiguousarray(x[b].T)
        m["pos"] = np.ascontiguousarray(pos[b])
        maps.append(m)
    return maps


def build_program(S=S_LEN, nlayers=DEPTH, debug=()):
    B = Builder(S, nlayers, debug)
    B.load_x()
    B.stage_rope_tables()
    for l in range(nlayers):
        B.stage_inproj(l)
        B.stage_hgrn(l)
        B.stage_rwkv(l)
        B.stage_mla(l)
        B.stage_merge(l)
        B.stage_ffn(l, l == nlayers - 1)
    return B.finish(), B


def kernel(**inputs):
    x = np.asarray(inputs["x"])
    nb, S, _ = x.shape
    nc, _ = build_program(S, DEPTH)
    in_maps = make_in_maps(inputs, nb)
    res = run_bass_kernel_spmd(nc, in_maps, core_ids=list(range(nb)))
    out = np.stack([np.ascontiguousarray(np.asarray(r["outT"]).T) for r in res.results]).astype(np.float32)
    return out
```

```python
import numpy as np
import concourse.bass as bass
import concourse.mybir as mybir
from concourse.bass_utils import run_bass_kernel_spmd

F32 = mybir.dt.float32
BF16 = mybir.dt.bfloat16
I32 = mybir.dt.int32
ALU = mybir.AluOpType
AF = mybir.ActivationFunctionType
AX = mybir.AxisListType

S_LEN = 4096
D = 1024
NT = S_LEN // 128
DEPTH = 2
D_FF = 2816
IN_W = 11200


class Res:
    __slots__ = ("w", "r")

    def __init__(self):
        self.w = None
        self.r = []


class Op:
    __slots__ = ("eng", "fn", "deps", "isdma", "sem", "val", "inc", "waits")

    def __init__(self, eng, fn, isdma):
        self.eng = eng
        self.fn = fn
        self.deps = set()
        self.isdma = isdma
        self.sem = None
        self.val = 0
        self.inc = False
        self.waits = []


class Tile:
    def __init__(self, t, nres):
        self.t = t
        self.res = [Res() for _ in range(nres)]

    def __getitem__(self, k):
        return self.t[k]


class Reg:
    def __init__(self, ap, off, n):
        self.ap, self.off, self.n = ap, off, n
        self.res = [Res()]

    def __getitem__(self, k):
        p, c = k
        a = 0 if c.start is None else c.start
        b = self.n if c.stop is None else c.stop
        return self.ap[p, self.off + a:self.off + b]


N_DMA_SEM = 24
N_HW_SEM = 16
COMPUTE = ("pe", "dve", "act", "pool")


class Sched:
    def __init__(self, nc):
        self.nc = nc
        self.ops = []
        self.base = set()
        self.last = {}
        self.dma_last = [None] * N_DMA_SEM
        self.dma_rr = 0
        self.dma_rr_sw = 0
        self.sb_top = 16640
        self.sb_peak = 0
        self.uid = 0

    def sb(self, shape, dtype, nres=1, name=None):
        self.uid += 1
        esz = {F32: 4, BF16: 2, I32: 4}[dtype]
        nb = int(np.prod(shape[1:])) * esz
        nb = (nb + 63) // 64 * 64
        off = self.sb_top
        self.sb_top += nb
        self.sb_peak = max(self.sb_peak, self.sb_top)
        assert self.sb_top <= 192 * 1024, ("SBUF overflow", self.sb_top)
        t = self.nc.alloc_sbuf_tensor_at(name or f"sb{self.uid}", list(shape), dtype, offset=off)
        return Tile(t, nres)

    def mark(self):
        return self.sb_top

    def release(self, mark):
        self.barrier()
        self.sb_top = mark

    def _rec(self, o, reads, writes):
        deps = set(self.base)
        for r in reads:
            if r.w is not None:
                deps.add(r.w)
        for w in writes:
            if w.w is not None:
                deps.add(w.w)
            deps.update(w.r)
        for r in reads:
            if o.isdma:
                r.r.append(o)
            else:
                r.r = [x for x in r.r if x.isdma or x.eng != o.eng]
                r.r.append(o)
        for w in writes:
            w.w = o
            w.r = []
        deps.discard(o)
        o.deps = deps
        self.ops.append(o)
        if not o.isdma:
            self.last[o.eng] = o
        return o

    def op(self, eng, fn, reads=(), writes=()):
        return self._rec(Op(eng, fn, False), reads, writes)

    def dma(self, q, out, in_, reads=(), writes=()):
        o = Op(q, lambda e: e.dma_start(out=out, in_=in_), True)
        if q == "pool":
            s = N_HW_SEM + self.dma_rr_sw
            self.dma_rr_sw = (self.dma_rr_sw + 1) % (N_DMA_SEM - N_HW_SEM)
        else:
            s = self.dma_rr
            self.dma_rr = (s + 1) % N_HW_SEM
        o.sem = s
        o = self._rec(o, reads, writes)
        if self.dma_last[s] is not None:
            o.deps.add(self.dma_last[s])
        self.dma_last[s] = o
        return o

    def barrier(self):
        b = set(self.last.values())
        b.update(d for d in self.dma_last if d is not None)
        self.base = b

    def emit(self, final_ops):
        nc = self.nc
        for o in self.ops:
            for d in o.deps:
                if d.eng == "pe" and o.eng == "pe" and not d.isdma and not o.isdma:
                    continue
                d.inc = True
        for o in final_ops:
            o.inc = True
        cnt = {e: 0 for e in COMPUTE}
        dcnt = [0] * N_DMA_SEM
        for o in self.ops:
            if o.isdma:
                dcnt[o.sem] += 16
                o.val = dcnt[o.sem]
                o.inc = True
            elif o.inc:
                cnt[o.eng] += 1
                o.val = cnt[o.eng]
        streams = {e: [] for e in ("pe", "dve", "act", "pool", "sp")}
        for o in self.ops:
            streams[o.eng].append(o)
        for e, lst in streams.items():
            have = {}
            for o in lst:
                need = {}
                for d in o.deps:
                    if d.eng == "pe" and o.eng == "pe" and not d.isdma and not o.isdma:
                        continue
                    key = ("d", d.sem) if d.isdma else ("c", d.eng)
                    if have.get(key, 0) < d.val and need.get(key, 0) < d.val:
                        need[key] = d.val
                for k, v in need.items():
                    have[k] = v
                o.waits = list(need.items())
        self.sem_max = dict(cnt)
        import contextlib
        with contextlib.ExitStack() as es:
            csem = {e: es.enter_context(nc.semaphore(f"s_{e}")) for e in COMPUTE}
            dsem = [es.enter_context(nc.semaphore(f"s_d{i}")) for i in range(N_DMA_SEM)]
            fin = es.enter_context(nc.semaphore("s_fin"))
            block = es.enter_context(nc.Block())

            def run(e, name):
                for o in streams[name]:
                    for (kind, k), v in o.waits:
                        e.wait_ge(csem[k] if kind == "c" else dsem[k], v)
                    ins = o.fn(e)
                    if o.isdma:
                        ins.then_inc(dsem[o.sem], 16)
                    elif o.inc:
                        ins.then_inc(csem[o.eng], 1)
                if name == "sp":
                    for o in final_ops:
                        if o.isdma:
                            e.wait_ge(dsem[o.sem], o.val)
                        else:
                            e.wait_ge(csem[o.eng], o.val)

            @block.tensor
            def _(e):
                run(e, "pe")

            @block.vector
            def _(e):
                run(e, "dve")

            @block.scalar
            def _(e):
                run(e, "act")

            @block.gpsimd
            def _(e):
                run(e, "pool")

            @block.sync
            def _(e):
                run(e, "sp")


VEC_SPEC = [
    ("mix_pre_g", 1024), ("mix_post_g", 1024), ("ffn_pre_g", 1024), ("ffn_post_g", 1024),
    ("lb0", 1024), ("lb1", 1024), ("onorm_g", 128), ("mu", 3328), ("vres_mu", 32),
    ("w0", 1024), ("a0", 1024), ("v0", 1024), ("k_k", 1024), ("k_a", 1024), ("r_k", 1024),
    ("lnx_g", 1024), ("lnx_b", 1024), ("q_norm_g", 384), ("kv_norm_g", 256),
]
VOFF = {}
_o = 0
for _n, _f in VEC_SPEC:
    VOFF[_n] = _o
    _o += (_f + 127) // 128
NV = _o


def pack_vecs(inp, l):
    lv = max(l - 1, 0)
    src = {
        "mix_pre_g": inp["mix_pre_g"][l], "mix_post_g": inp["mix_post_g"][l],
        "ffn_pre_g": inp["ffn_pre_g"][l], "ffn_post_g": inp["ffn_post_g"][l],
        "lb0": inp["hgrn_lb_logits"][0], "lb1": inp["hgrn_lb_logits"][1],
        "onorm_g": inp["hgrn_onorm_g"][l], "mu": inp["rwkv_mu"][l], "vres_mu": inp["rwkv_vres_mu"][lv],
        "w0": inp["rwkv_w0"][l], "a0": inp["rwkv_a0"][l], "v0": inp["rwkv_v0"][lv],
        "k_k": inp["rwkv_k_k"][l], "k_a": inp["rwkv_k_a"][l], "r_k": inp["rwkv_r_k"][l],
        "lnx_g": inp["rwkv_lnx_g"][l], "lnx_b": inp["rwkv_lnx_b"][l],
        "q_norm_g": inp["mla_q_norm_g"][l], "kv_norm_g": inp["mla_kv_norm_g"][l],
    }
    out = np.zeros((128, NV), np.float32)
    for n, f in VEC_SPEC:
        v = np.asarray(src[n], np.float32).reshape(-1)
        nch = (f + 127) // 128
        pad = np.zeros(nch * 128, np.float32)
        pad[:f] = v
        out[:, VOFF[n]:VOFF[n] + nch] = pad.reshape(nch, 128).T
    return out


N_ZC = 89


def zcols(fc):
    if fc < 63:
        return fc * 128, 128
    if fc == 63:
        return 8064, 64
    return 8128 + (fc - 64) * 128, 128


class Builder:
    def __init__(self, S_LEN=S_LEN, nlayers=DEPTH, debug=()):
        self.S = S_LEN
        self.TG = min(512, S_LEN)
        self.NTG = S_LEN // self.TG
        self.L = nlayers
        self.debug = set(debug)
        nc = bass.Bass("TRN2", target_bir_lowering=False)
        self.nc = nc
        self.sch = Sched(nc)
        S = S_LEN

        def di(name, shape, dt=F32):
            return nc.dram_tensor(name, list(shape), dt, kind="ExternalInput").ap()

        self.xT = di("xT", [D, S])
        self.pos = di("pos", [S], I32)
        self.vecs = di("vecs", [DEPTH, 128, NV])
        self.w_in = di("w_in", [DEPTH, D, IN_W])
        self.w_vres = di("w_vres", [D, 32])
        self.w_up = di("w_up", [DEPTH, 64, D])
        self.a_up = di("a_up", [DEPTH, 64, D])
        self.g_up = di("g_up", [DEPTH, 128, D])
        self.v_up = di("v_up", [32, D])
        self.w_uq = di("w_uq", [DEPTH, 384, 1536])
        self.w_ukv = di("w_ukv", [DEPTH, 256, 2048])
        self.w_branch = di("w_branch", [DEPTH, 3, D, D])
        self.w_out = di("w_out", [DEPTH, D, D])
        self.w_ffn_in = di("w_ffn_in", [DEPTH, D, 2 * D_FF])
        self.w_ffn_out = di("w_ffn_out", [DEPTH, D_FF, D])
        self.outT = nc.dram_tensor("outT", [D, S], F32, kind="ExternalOutput").ap()
        self.dbg = {}

        def dscr(name, shape, dt=F32):
            kind = "ExternalOutput" if name in self.debug else "Internal"
            t = nc.dram_tensor(name, list(shape), dt, kind=kind).ap()
            return t

        self.zT = dscr("zT", [N_ZC, 128, S])
        self.zT_res = [Res() for _ in range(N_ZC)]
        self.xcur = dscr("xcur", [8, 128, S])
        self.xcur_res = [Res() for _ in range(self.NTG)]
        self.vtok = dscr("vtok", [S, D], BF16)
        self.vtok_res = [Res() for _ in range(S // 128)]
        self.obr = dscr("obr", [3, 8, 128, S], BF16)
        self.obr_res = [[Res() for _ in range(8)] for _ in range(3)]
        self.aT = dscr("aT", [D_FF // 128, 128, S], BF16)
        self.aT_res = [Res() for _ in range(D_FF // 128)]
        self.vfirst = dscr("vfirst", [8, 128, S])
        self.vfirst_res = [Res() for _ in range(8)]
        self.final_ops = []

        sch = self.sch
        self.PS = [Tile(nc.alloc_psum_tensor(f"psb{i}", [128, 512], F32), 1) for i in range(8)]
        self.ones_bf = sch.sb([128, 128], BF16)
        self.ident_bf = sch.sb([128, 128], BF16)
        self.ident_f = sch.sb([128, 128], F32)
        self.eps6 = sch.sb([128, 1], F32)
        self.vec = [sch.sb([128, NV], F32) for _ in range(DEPTH)]
        sch.op("dve", lambda e: e.memset(self.ones_bf[:], 1.0), writes=self.ones_bf.res)
        sch.op("dve", lambda e: e.memset(self.eps6[:], 1e-6), writes=self.eps6.res)
        sch.op("pool", lambda e: e.memset(self.ident_f[:], 1.0), writes=self.ident_f.res)
        sch.op("pool", lambda e: e.affine_select(out=self.ident_f[:], in_=self.ident_f[:], pattern=[[-1, 128]],
                                                 compare_op=ALU.is_equal, fill=0.0, base=0, channel_multiplier=1),
               reads=self.ident_f.res, writes=self.ident_f.res)
        sch.op("dve", lambda e: e.tensor_copy(out=self.ident_bf[:], in_=self.ident_f[:]),
               reads=self.ident_f.res, writes=self.ident_bf.res)
        for l in range(DEPTH):
            sch.dma("sp", self.vec[l][:], self.vecs[l], writes=self.vec[l].res)

    def V(self, l, name, c=0, n=1):
        o = VOFF[name] + c
        return self.vec[l][:, o:o + n]

    def norm_hT(self, l, src, src_res, gname, hT, deferred=False):
        sch, TG = self.sch, self.TG
        m = sch.mark()
        xb = sch.sb([128, 2, 8, TG], F32, nres=2)
        sq = sch.sb([128, 2, 8, TG], BF16, nres=2)
        rs = sch.sb([128, 2, TG], F32, nres=2)

        def step(tg):
            s = tg % 2
            ts = slice(tg * TG, (tg + 1) * TG)
            sch.dma("sp", xb[:, s], src[:, :, ts].rearrange("c p s -> p c s"), reads=[src_res[tg]], writes=[xb.res[s]])
            sch.op("act", lambda e, s=s: e.activation(out=sq[:, s], in_=xb[:, s], func=AF.Square),
                   reads=[xb.res[s]], writes=[sq.res[s]])
            bank = self.PS[4 + tg % 2]
            for kc in range(8):
                sch.op("pe", lambda e, s=s, kc=kc, bank=bank: e.matmul(bank[:, 0:TG], lhsT=self.ones_bf[:], rhs=sq[:, s, kc, :],
                                                                     start=(kc == 0), stop=(kc == 7)),
                       reads=[sq.res[s], self.ones_bf.res[0]], writes=bank.res)
            sch.op("act", lambda e, s=s, bank=bank: e.activation(out=rs[:, s], in_=bank[:, 0:TG], func=AF.Ln,
                                                                scale=1.0 / D, bias=self.eps6[:, 0:1]),
                   reads=bank.res + self.eps6.res, writes=[rs.res[s]])
            sch.op("act", lambda e, s=s: e.activation(out=rs[:, s], in_=rs[:, s], func=AF.Exp, scale=-0.5), reads=[rs.res[s]], writes=[rs.res[s]])
            for kc in range(8):
                sch.op("dve", lambda e, s=s, kc=kc, ts=ts: e.scalar_tensor_tensor(
                    out=hT[:, kc, ts], in0=xb[:, s, kc, :], scalar=self.V(l, gname, kc), in1=rs[:, s],
                    op0=ALU.mult, op1=ALU.mult),
                    reads=[xb.res[s], rs.res[s], self.vec[l].res[0]], writes=[hT.res[tg]])
        if deferred:
            return step
        for tg in range(self.NTG):
            step(tg)
        sch.release(m)

    def gemm(self, fcs, KC, kp, wsrc, rhs, evac, banks, post_fc=None, ntg=None, pre_tg=None):
        sch, TG = self.sch, self.TG
        ntg = self.NTG if ntg is None else ntg
        m = sch.mark()
        wb = sch.sb([128, 2, KC, 128], BF16, nres=2)
        it = 0
        for i, fc in enumerate(fcs):
            s = i % 2
            ap, ncols = wsrc(fc)
            sch.dma("pool", wb[0:kp, s, :, 0:ncols], ap, writes=[wb.res[s]])
            for tg in range(ntg):
                if i == 0 and pre_tg is not None:
                    pre_tg(tg)
                bank = banks[it % len(banks)]
                it += 1
                for kc in range(KC):
                    r_ap, r_res = rhs(kc, tg)
                    sch.op("pe", lambda e, s=s, kc=kc, bank=bank, r_ap=r_ap, ncols=ncols: e.matmul(
                        bank[0:ncols, 0:TG], lhsT=wb[0:kp, s, kc, 0:ncols], rhs=r_ap, start=(kc == 0), stop=(kc == KC - 1)),
                        reads=[wb.res[s]] + r_res, writes=bank.res)
                evac(fc, tg, bank, ncols)
            if post_fc is not None:
                post_fc(fc)
        sch.release(m)

    def stage_inproj(self, l):
        sch, S, TG = self.sch, self.S, self.TG
        m = sch.mark()
        hT = sch.sb([128, 8, S], BF16, nres=self.NTG)
        nstep = self.norm_hT(l, self.xcur, self.xcur_res, "mix_pre_g", hT, deferred=True)
        wv = sch.sb([128, 8, 1024], BF16)
        vs = sch.sb([128, 2, 1024], BF16, nres=2)
        for q in range(4):
            sch.dma("pool", wv[:, :, q * 256:(q + 1) * 256],
                    self.w_in[l][:, 2048 + q * 256:2048 + (q + 1) * 256].rearrange("(kc p) f -> p kc f", p=128), writes=wv.res)
        zt = sch.sb([128, 2, S], F32, nres=2)
        cnt = [0]
        fcs = [fc for fc in range(88 if l == 0 else 89) if not (16 <= fc < 24)]
        pos = {fc: i for i, fc in enumerate(fcs)}

        def wsrc(fc):
            if fc == 88:
                return self.w_vres.rearrange("(kc p) f -> p kc f", p=128), 32
            c0, ncols = zcols(fc)
            return self.w_in[l][:, c0:c0 + ncols].rearrange("(kc p) f -> p kc f", p=128), ncols

        def rhs(kc, tg):
            return hT[:, kc, tg * TG:(tg + 1) * TG], [hT.res[tg]]

        def evac(fc, tg, bank, ncols):
            s = pos[fc] % 2
            cnt[0] += 1
            if 64 <= fc < 88:
                sch.op("act", lambda e: e.activation(out=zt[0:ncols, s, tg * TG:(tg + 1) * TG], in_=bank[0:ncols, 0:TG], func=AF.Sigmoid),
                       reads=bank.res, writes=[zt.res[s]])
            elif cnt[0] % 2:
                sch.op("act", lambda e: e.copy(out=zt[0:ncols, s, tg * TG:(tg + 1) * TG], in_=bank[0:ncols, 0:TG]),
                       reads=bank.res, writes=[zt.res[s]])
            else:
                sch.op("dve", lambda e: e.tensor_copy(out=zt[0:ncols, s, tg * TG:(tg + 1) * TG], in_=bank[0:ncols, 0:TG]),
                       reads=bank.res, writes=[zt.res[s]])

        def post_fc(fc):
            s = pos[fc] % 2
            ncols = wsrc(fc)[1]
            sch.dma("sp", self.zT[fc, 0:ncols, :], zt[0:ncols, s, :], reads=[zt.res[s]], writes=[self.zT_res[fc]])

        self.gemm(fcs, 8, 128, wsrc, rhs, evac, self.PS[0:4], post_fc, pre_tg=nstep)
        m2 = sch.mark()
        for tt in range(S // 128):
            s = tt % 2
            tg = (tt * 128) // TG
            for hf in range(2):
                bank = self.PS[4 + (tt * 2 + hf) % 4]
                for kc in range(8):
                    sch.op("pe", lambda e, kc=kc, bank=bank, tt=tt, hf=hf: e.matmul(
                        bank[:, :], lhsT=hT[:, kc, tt * 128:(tt + 1) * 128], rhs=wv[:, kc, hf * 512:(hf + 1) * 512],
                        start=(kc == 0), stop=(kc == 7)), reads=[hT.res[tg]] + wv.res, writes=bank.res)
                if hf == 0:
                    sch.op("act", lambda e, s=s, bank=bank: e.copy(out=vs[:, s, 0:512], in_=bank[:, :]), reads=bank.res, writes=[vs.res[s]])
                else:
                    sch.op("dve", lambda e, s=s, bank=bank: e.tensor_copy(out=vs[:, s, 512:1024], in_=bank[:, :]), reads=bank.res, writes=[vs.res[s]])
            sch.dma("sp", self.vtok[tt * 128:(tt + 1) * 128, :], vs[:, s, :], reads=[vs.res[s]], writes=[self.vtok_res[tt]])
        sch.release(m2)
        sch.release(m)

    def pn_bufs(self, nb=2):
        sch, TG = self.sch, self.TG
        return dict(sq=sch.sb([128, nb, 8, TG], BF16, nres=nb), rs=sch.sb([128, nb, TG], F32, nres=nb),
                    xb=sch.sb([128, nb, 8, TG], F32, nres=nb), t=sch.sb([128, 2, TG], F32, nres=2), n=[0], nb=nb)

    def postnorm_residual(self, l, gname, pn, mo_ap, mo_res, tg, to_out):
        sch, TG = self.sch, self.TG
        s = pn["n"][0] % pn["nb"]
        pn["n"][0] += 1
        sq, rs, xb, tt = pn["sq"], pn["rs"], pn["xb"], pn["t"]
        ts = slice(tg * TG, (tg + 1) * TG)
        sch.dma("sp", xb[:, s], self.xcur[:, :, ts].rearrange("c p s -> p c s"), reads=[self.xcur_res[tg]], writes=[xb.res[s]])
        sch.op("act", lambda e: e.activation(out=sq[:, s], in_=mo_ap, func=AF.Square), reads=mo_res, writes=[sq.res[s]])
        bank = self.PS[6 + s]
        for kc in range(8):
            sch.op("pe", lambda e, kc=kc: e.matmul(bank[:, 0:TG], lhsT=self.ones_bf[:], rhs=sq[:, s, kc, :],
                                                   start=(kc == 0), stop=(kc == 7)),
                   reads=[sq.res[s], self.ones_bf.res[0]], writes=bank.res)
        sch.op("act", lambda e: e.activation(out=rs[:, s], in_=bank[:, 0:TG], func=AF.Ln, scale=1.0 / D, bias=self.eps6[:, 0:1]),
               reads=bank.res + self.eps6.res, writes=[rs.res[s]])
        sch.op("act", lambda e: e.activation(out=rs[:, s], in_=rs[:, s], func=AF.Exp, scale=-0.5), reads=[rs.res[s]], writes=[rs.res[s]])
        for kc in range(8):
            sch.op("dve", lambda e, kc=kc: e.scalar_tensor_tensor(out=tt[:, kc % 2], in0=mo_ap[:, kc, :], scalar=self.V(l, gname, kc),
                                                                 in1=rs[:, s], op0=ALU.mult, op1=ALU.mult),
                   reads=mo_res + [rs.res[s], self.vec[l].res[0]], writes=[tt.res[kc % 2]])
            sch.op("pool", lambda e, kc=kc: e.tensor_tensor(out=xb[:, s, kc, :], in0=xb[:, s, kc, :], in1=tt[:, kc % 2], op=ALU.add),
                   reads=[tt.res[kc % 2], xb.res[s]], writes=[xb.res[s]])
        if to_out:
            sch.dma("sp", self.outT.rearrange("(c p) s -> p c s", p=128)[:, :, ts], xb[:, s], reads=[xb.res[s]], writes=[self.xcur_res[tg]])
        else:
            sch.dma("sp", self.xcur[:, :, ts].rearrange("c p s -> p c s"), xb[:, s], reads=[xb.res[s]], writes=[self.xcur_res[tg]])

    def stage_merge(self, l):
        sch, S, TG = self.sch, self.S, self.TG
        TB = min(S, 2048)
        nb = TB // TG
        m = sch.mark()
        mg = sch.sb([128, 8, TB], F32, nres=8)
        ob = sch.sb([128, 8, TB], BF16, nres=8)
        gt = sch.sb([128, 4, TG], F32, nres=4)
        tmp = sch.sb([128, 4, TG], F32, nres=4)
        pn = self.pn_bufs(1)
        cnt = [0]
        for tb in range(S // TB):
            t0 = tb * TB
            for b in range(3):
                for kc in range(8):
                    sch.dma("sp", ob[:, kc, :], self.obr[b, kc, :, t0:t0 + TB], reads=[self.obr_res[b][kc]], writes=[ob.res[kc]])

                def wsrc(fc, b=b):
                    return self.w_branch[l, b][:, fc * 128:(fc + 1) * 128].rearrange("(kc p) f -> p kc f", p=128), 128

                def rhs(kc, tg):
                    return ob[:, kc, tg * TG:(tg + 1) * TG], [ob.res[kc]]

                def evac(fc, tg, bank, ncols, b=b, t0=t0):
                    s = cnt[0] % 4
                    cnt[0] += 1
                    zc = 64 + b * 8 + fc
                    tl = slice(tg * TG, (tg + 1) * TG)
                    sch.dma("sp", gt[:, s], self.zT[zc, :, t0 + tg * TG:t0 + (tg + 1) * TG], reads=[self.zT_res[zc]], writes=[gt.res[s]])
                    if b == 0:
                        sch.op("dve", lambda e: e.tensor_tensor(out=mg[:, fc, tl], in0=bank[:, 0:TG], in1=gt[:, s], op=ALU.mult),
                               reads=bank.res + [gt.res[s]], writes=[mg.res[fc]])
                    else:
                        sch.op("dve", lambda e: e.tensor_tensor(out=tmp[:, s], in0=bank[:, 0:TG], in1=gt[:, s], op=ALU.mult),
                               reads=bank.res + [gt.res[s]], writes=[tmp.res[s]])
                        sch.op("pool", lambda e: e.tensor_tensor(out=mg[:, fc, tl], in0=mg[:, fc, tl], in1=tmp[:, s], op=ALU.add),
                               reads=[tmp.res[s], mg.res[fc]], writes=[mg.res[fc]])

                self.gemm(list(range(8)), 8, 128, wsrc, rhs, evac, self.PS[0:4], ntg=nb)
            for kc in range(8):
                eng = "act" if kc % 2 else "pool"
                if eng == "act":
                    sch.op("act", lambda e, kc=kc: e.copy(out=ob[:, kc, :], in_=mg[:, kc, :]), reads=[mg.res[kc]], writes=[ob.res[kc]])
                else:
                    sch.op("pool", lambda e, kc=kc: e.tensor_copy(out=ob[:, kc, :], in_=mg[:, kc, :]), reads=[mg.res[kc]], writes=[ob.res[kc]])

            def wsrc2(fc):
                return self.w_out[l][:, fc * 128:(fc + 1) * 128].rearrange("(kc p) f -> p kc f", p=128), 128

            def rhs2(kc, tg):
                return ob[:, kc, tg * TG:(tg + 1) * TG], [ob.res[kc]]

            def evac2(fc, tg, bank, ncols):
                cnt[0] += 1
                tl = slice(tg * TG, (tg + 1) * TG)
                if cnt[0] % 2:
                    sch.op("act", lambda e: e.copy(out=mg[:, fc, tl], in_=bank[:, 0:TG]), reads=bank.res, writes=[mg.res[fc]])
                else:
                    sch.op("dve", lambda e: e.tensor_copy(out=mg[:, fc, tl], in_=bank[:, 0:TG]), reads=bank.res, writes=[mg.res[fc]])

            self.gemm(list(range(8)), 8, 128, wsrc2, rhs2, evac2, self.PS[0:4], ntg=nb)
            for tg in range(nb):
                self.postnorm_residual(l, "mix_post_g", pn, mg[:, :, tg * TG:(tg + 1) * TG], list(mg.res), tb * nb + tg, False)
        sch.release(m)

    def stage_ffn(self, l, last):
        sch, S, TG = self.sch, self.S, self.TG
        NJ = D_FF // 128
        m = sch.mark()
        hT = sch.sb([128, 8, S], BF16, nres=self.NTG)
        nstep = self.norm_hT(l, self.xcur, self.xcur_res, "ffn_pre_g", hT, deferred=True)
        at = sch.sb([128, 2, S], BF16, nres=2)
        sg = sch.sb([128, S], F32, nres=self.NTG)

        def wsrc(fc):
            j, u = fc // 2, fc % 2
            c0 = u * D_FF + j * 128
            return self.w_ffn_in[l][:, c0:c0 + 128].rearrange("(kc p) f -> p kc f", p=128), 128

        def rhs(kc, tg):
            return hT[:, kc, tg * TG:(tg + 1) * TG], [hT.res[tg]]

        def evac(fc, tg, bank, ncols):
            j, u = fc // 2, fc % 2
            ts = slice(tg * TG, (tg + 1) * TG)
            if u == 0:
                sch.op("act", lambda e: e.activation(out=sg[:, ts], in_=bank[:, 0:TG], func=AF.Silu), reads=bank.res, writes=[sg.res[tg]])
            else:
                sch.op("dve", lambda e: e.tensor_tensor(out=at[:, j % 2, ts], in0=bank[:, 0:TG], in1=sg[:, ts], op=ALU.mult),
                       reads=bank.res + [sg.res[tg]], writes=[at.res[j % 2]])

        def post_fc(fc):
            j, u = fc // 2, fc % 2
            if u == 1:
                sch.dma("sp", self.aT[j], at[:, j % 2, :], reads=[at.res[j % 2]], writes=[self.aT_res[j]])

        self.gemm(list(range(2 * NJ)), 8, 128, wsrc, rhs, evac, self.PS[0:4], post_fc, pre_tg=nstep)
        sch.release(m)
        m = sch.mark()
        w2 = sch.sb([128, NJ, 1024], BF16, nres=NJ)
        ab = sch.sb([128, 2, NJ, TG], BF16, nres=2)
        mo = sch.sb([128, 2, 8, TG], F32, nres=2)
        pn = self.pn_bufs(1)
        for kc in range(NJ):
            sch.dma("pool", w2[:, kc, :], self.w_ffn_out[l][kc * 128:(kc + 1) * 128, :], writes=[w2.res[kc]])
        it = 0
        for tg in range(self.NTG + 1):
            s = tg % 2
            if tg < self.NTG:
                ts = slice(tg * TG, (tg + 1) * TG)
                sch.dma("sp", ab[:, s], self.aT[:, :, ts].rearrange("j p s -> p j s"), reads=self.aT_res, writes=[ab.res[s]])
            for fc in range(8):
                if tg < self.NTG:
                    bank = self.PS[it % 4]
                    it += 1
                    for kc in range(NJ):
                        sch.op("pe", lambda e, kc=kc, bank=bank, fc=fc, s=s: e.matmul(
                            bank[:, 0:TG], lhsT=w2[:, kc, fc * 128:(fc + 1) * 128], rhs=ab[:, s, kc, :], start=(kc == 0), stop=(kc == NJ - 1)),
                            reads=[w2.res[kc], ab.res[s]], writes=bank.res)
                    if fc % 2:
                        sch.op("act", lambda e, bank=bank, fc=fc, s=s: e.copy(out=mo[:, s, fc, :], in_=bank[:, 0:TG]), reads=bank.res, writes=[mo.res[s]])
                    else:
                        sch.op("dve", lambda e, bank=bank, fc=fc, s=s: e.tensor_copy(out=mo[:, s, fc, :], in_=bank[:, 0:TG]), reads=bank.res, writes=[mo.res[s]])
                if fc == 1 and tg > 0:
                    self.postnorm_residual(l, "ffn_post_g", pn, mo[:, 1 - s], [mo.res[1 - s]], tg - 1, last)
        sch.release(m)

    def hgrn_consts(self):
        sch = self.sch
        c = {}
        c["mbd"] = sch.sb([128, 128], F32)
        c["cm3"] = sch.sb([128, 4, 128], F32)
        c["eps6"] = self.eps6
        mbd, cm3 = c["mbd"], c["cm3"]
        sch.op("pool", lambda e: e.memset(mbd[:], 1.0), writes=mbd.res)
        sch.op("pool", lambda e: e.affine_select(out=mbd[:], in_=mbd[:], pattern=[[1, 128]], compare_op=ALU.is_ge, fill=0.0,
                                                 base=0, channel_multiplier=-1), reads=mbd.res, writes=mbd.res)
        sch.op("pool", lambda e: e.affine_select(out=mbd[:].rearrange("p (c i) -> p c i", i=32), in_=mbd[:].rearrange("p (c i) -> p c i", i=32),
                                                 pattern=[[-32, 4], [0, 32]], compare_op=ALU.is_ge, fill=0.0, base=0, channel_multiplier=1),
               reads=mbd.res, writes=mbd.res)
        sch.op("pool", lambda e: e.memset(cm3[:], 1.0), writes=cm3.res)
        sch.op("pool", lambda e: e.affine_select(out=cm3[:], in_=cm3[:], pattern=[[-32, 4], [0, 128]], compare_op=ALU.is_ge, fill=0.0,
                                                 base=0, channel_multiplier=1), reads=cm3.res, writes=cm3.res)
        sch.op("pool", lambda e: e.affine_select(out=cm3[:], in_=cm3[:], pattern=[[32, 4], [0, 128]], compare_op=ALU.is_ge, fill=0.0,
                                                 base=31, channel_multiplier=-1), reads=cm3.res, writes=cm3.res)
        return c

    def stage_hgrn(self, l):
        sch, S = self.sch, self.S
        T = min(S, 512)
        NB = S // T
        nt = T // 128
        nch = T // 32
        TG = min(T, 512)
        m = sch.mark()
        c = self.hgrn_consts()
        mbd, cm3 = c["mbd"], c["cm3"]
        mask32 = sch.sb([128, T], F32)
        sch.op("pool", lambda e: e.memset(mask32[:], 1.0), writes=mask32.res)
        sch.op("pool", lambda e: e.memset(mask32[:].rearrange("p (c i) -> p c i", i=32)[:, :, 0:1], 0.0), writes=mask32.res)
        lbv = sch.sb([128, 8], F32)
        oml = sch.sb([128, 8], F32)
        if l == 0:
            sch.op("dve", lambda e: e.memset(lbv[:], 0.0), writes=lbv.res)
        else:
            sch.op("dve", lambda e: e.tensor_tensor(out=lbv[:], in0=self.V(l, "lb1", 0, 8), in1=self.V(l, "lb0", 0, 8), op=ALU.subtract),
                   reads=self.vec[l].res, writes=lbv.res)
            sch.op("act", lambda e: e.activation(out=lbv[:], in_=lbv[:], func=AF.Sigmoid), reads=lbv.res, writes=lbv.res)
        sch.op("dve", lambda e: e.tensor_scalar(out=oml[:], in0=lbv[:], scalar1=-1.0, scalar2=1.0, op0=ALU.mult, op1=ALU.add),
               reads=lbv.res, writes=oml.res)
        NS = 4
        bufs = []
        for i in range(NS):
            bufs.append(dict(
                zq=sch.sb([128, T], F32), zf=sch.sb([128, T], F32), zog=sch.sb([128, T], F32), vt=sch.sb([128, nt, 128], BF16),
                lf=sch.sb([128, T], F32), kk=sch.sb([128, T], F32), b=sch.sb([128, T], F32), eb=sch.sb([128, T], F32),
                tmp=sch.sb([128, T], F32), qt32=sch.sb([128, T], F32), qt=sch.sb([128, T], BF16), kt=sch.sb([128, T], BF16), kh=sch.sb([128, T], BF16),
                oT=sch.sb([128, T], F32), ob=sch.sb([128, T], BF16)))
        pT = sch.sb([128, 4, 128], BF16, nres=4)
        k4 = sch.sb([128, 4, 4, 128], BF16, nres=4)
        u4 = sch.sb([128, 4, 4, 128], F32, nres=4)
        st32 = sch.sb([128, 16, 128], F32, nres=16)
        sqs = [sch.sb([128, TG], BF16) for _ in range(2)]
        rss = [sch.sb([128, TG], F32) for _ in range(2)]
        PS = self.PS
        sidxs = [[0], [0]]

        def prep_load(h, tb, B):
            t0 = tb * T
            zq, zf, zog, vt, lf, kk, b, eb, tmp, qt32, qt, kt, kh, oT, ob = (B[k] for k in (
                "zq", "zf", "zog", "vt", "lf", "kk", "b", "eb", "tmp", "qt32", "qt", "kt", "kh", "oT", "ob"))
            sch.dma("sp", zq[:], self.zT[h, :, t0:t0 + T], reads=[self.zT_res[h]], writes=zq.res)
            sch.dma("sp", zf[:], self.zT[8 + h, :, t0:t0 + T], reads=[self.zT_res[8 + h]], writes=zf.res)
            sch.dma("sp", zog[:], self.zT[24 + h, :, t0:t0 + T], reads=[self.zT_res[24 + h]], writes=zog.res)
            sch.dma("sp", vt[:], self.vtok[t0:t0 + T, h * 128:(h + 1) * 128].rearrange("(n p) f -> p n f", p=128),
                    reads=self.vtok_res[t0 // 128:(t0 + T) // 128], writes=vt.res)

        def prep(h, tb, B):
            zq, zf, zog, vt, lf, kk, b, eb, tmp, qt32, qt, kt, kh, oT, ob = (B[k] for k in (
                "zq", "zf", "zog", "vt", "lf", "kk", "b", "eb", "tmp", "qt32", "qt", "kt", "kh", "oT", "ob"))
            sch.op("act", lambda e, zf=zf: e.activation(out=zf[:], in_=zf[:], func=AF.Sigmoid), reads=zf.res, writes=zf.res)
            sch.op("act", lambda e, zog=zog: e.activation(out=zog[:], in_=zog[:], func=AF.Silu), reads=zog.res, writes=zog.res)
            sch.op("dve", lambda e, zf=zf, h=h: e.tensor_scalar(out=zf[:], in0=zf[:], scalar1=oml[:, h:h + 1], scalar2=lbv[:, h:h + 1],
                                                              op0=ALU.mult, op1=ALU.add), reads=zf.res + oml.res + lbv.res, writes=zf.res)
            sch.op("act", lambda e, zf=zf, lf=lf: e.activation(out=lf[:], in_=zf[:], func=AF.Ln), reads=zf.res, writes=lf.res)
            sch.op("pool", lambda e, zf=zf, kk=kk: e.tensor_scalar(out=kk[:], in0=zf[:], scalar1=-1.0, scalar2=1.0, op0=ALU.mult, op1=ALU.add),
                   reads=zf.res, writes=kk.res)
            sch.op("dve", lambda e, b=b, lf=lf: e.tensor_tensor_scan(out=b[:], data0=mask32[:], data1=lf[:], initial=0.0,
                                                                    op0=ALU.mult, op1=ALU.add), reads=lf.res + mask32.res, writes=b.res)
            sch.op("act", lambda e, b=b, eb=eb: e.activation(out=eb[:], in_=b[:], func=AF.Exp), reads=b.res, writes=eb.res)
            sch.op("dve", lambda e, qt32=qt32, zq=zq, eb=eb: e.tensor_tensor(out=qt32[:], in0=zq[:], in1=eb[:], op=ALU.mult),
                   reads=zq.res + eb.res, writes=qt32.res)
            sch.op("pool", lambda e, qt32=qt32, qt=qt: e.tensor_copy(out=qt[:], in_=qt32[:]), reads=qt32.res, writes=qt.res)
            sch.op("act", lambda e, b=b, tmp=tmp: e.activation(out=tmp[:], in_=b[:], func=AF.Exp, scale=-1.0), reads=b.res, writes=tmp.res)
            sch.op("dve", lambda e, kt=kt, kk=kk, tmp=tmp: e.tensor_tensor(out=kt[:], in0=kk[:], in1=tmp[:], op=ALU.mult),
                   reads=kk.res + tmp.res, writes=kt.res)

        def tiles(h, tb, B, X):
            sidx = sidxs[X]
            sq, rs = sqs[X], rss[X]
            t0 = tb * T
            zq, zf, zog, vt, lf, kk, b, eb, tmp, qt32, qt, kt, kh, oT, ob = (B[k] for k in (
                "zq", "zf", "zog", "vt", "lf", "kk", "b", "eb", "tmp", "qt32", "qt", "kt", "kh", "oT", "ob"))
            if tb == 0:
                cur = 8 * X + sidx[0] % 8
                sch.op("pool", lambda e, cur=cur: e.memset(st32[:, cur], 0.0), writes=[st32.res[cur]])
            eb3 = eb[:].rearrange("p (c i) -> p c i", i=32)

            def phA(n):
                tl = slice(n * 128, (n + 1) * 128)
                s2 = 2 * X + n % 2
                pa, pb_, pu, po = PS[4 * X + 0], PS[4 * X + 1], PS[4 * X + 2], PS[4 * X + 3]
                sch.op("pe", lambda e, pa=pa, kt=kt, qt=qt, tl=tl: e.matmul(pa[:, 0:128], lhsT=kt[:, tl], rhs=qt[:, tl], start=True, stop=True),
                       reads=kt.res + qt.res, writes=pa.res)
                sch.op("dve", lambda e, pa=pa, s2=s2: e.tensor_tensor(out=pT[:, s2], in0=pa[:, 0:128], in1=mbd[:], op=ALU.mult),
                       reads=pa.res + mbd.res, writes=[pT.res[s2]])
                pbb = pb_[:].bitcast(BF16)
                sch.op("pe", lambda e, pbb=pbb, kt=kt, tl=tl: e.transpose(out=pbb[:, 0:128], in_=kt[:, tl], identity=self.ident_bf[:]),
                       reads=kt.res + self.ident_bf.res, writes=pb_.res)
                sch.op("dve", lambda e, pbb=pbb, s2=s2: e.tensor_tensor(out=k4[:, s2], in0=pbb[:, 0:128].unsqueeze(1).broadcast_to([128, 4, 128]),
                                                                       in1=cm3[:], op=ALU.mult), reads=pb_.res + cm3.res, writes=[k4.res[s2]])
                for cc in range(4):
                    sch.op("pe", lambda e, pu=pu, s2=s2, cc=cc, vt=vt, n=n: e.matmul(pu[:, cc * 128:(cc + 1) * 128], lhsT=k4[:, s2, cc, :],
                                                                                 rhs=vt[:, n, :], start=True, stop=True),
                           reads=[k4.res[s2]] + vt.res, writes=pu.res)
                for cc in range(4):
                    sch.op("act", lambda e, pu=pu, s2=s2, cc=cc, n=n: e.activation(out=u4[:, s2, cc, :], in_=pu[:, cc * 128:(cc + 1) * 128], func=AF.Copy,
                                                                               scale=eb3[:, n * 4 + cc, 31:32]),
                           reads=pu.res + eb.res, writes=[u4.res[s2]])

            def phB(n):
                tl = slice(n * 128, (n + 1) * 128)
                s2 = 2 * X + n % 2
                po = PS[4 * X + 3]
                sch.op("pe", lambda e, po=po, vt=vt, n=n, s2=s2: e.matmul(po[:, 0:128], lhsT=vt[:, n, :], rhs=pT[:, s2], start=True, stop=False),
                       reads=vt.res + [pT.res[s2]], writes=po.res)
                for cc in range(4):
                    cur = 8 * X + sidx[0] % 8
                    nxt = 8 * X + (sidx[0] + 1) % 8
                    sidx[0] += 1
                    ch = n * 4 + cc
                    cs = slice(n * 128 + cc * 32, n * 128 + (cc + 1) * 32)
                    sch.op("pe", lambda e, po=po, cur=cur, qt32=qt32, cs=cs, cc=cc: e.matmul(po[:, cc * 32:(cc + 1) * 32], lhsT=st32[:, cur], rhs=qt32[:, cs],
                                                                                    start=False, stop=(cc == 3)),
                           reads=[st32.res[cur]] + qt32.res, writes=po.res)
                    sch.op("dve", lambda e, cur=cur, nxt=nxt, eb3=eb3, ch=ch, s2=s2, cc=cc: e.scalar_tensor_tensor(
                        out=st32[:, nxt], in0=st32[:, cur], scalar=eb3[:, ch, 31:32], in1=u4[:, s2, cc, :], op0=ALU.mult, op1=ALU.add),
                        reads=[st32.res[cur], u4.res[s2]] + eb.res, writes=[st32.res[nxt]])
                sch.op("act", lambda e, po=po, oT=oT, tl=tl: e.copy(out=oT[:, tl], in_=po[:, 0:128]), reads=po.res, writes=oT.res)

            phA(0)
            for n in range(nt):
                if n + 1 < nt:
                    phA(n + 1)
                phB(n)
                yield
            for g in range(T // TG):
                gs = slice(g * TG, (g + 1) * TG)
                bank = PS[4 * X + g % 2]
                sch.op("act", lambda e, oT=oT, gs=gs: e.activation(out=sq[:], in_=oT[:, gs], func=AF.Square), reads=oT.res, writes=sq.res)
                sch.op("pe", lambda e, bank=bank: e.matmul(bank[:, 0:TG], lhsT=self.ones_bf[:], rhs=sq[:], start=True, stop=True),
                       reads=sq.res + self.ones_bf.res, writes=bank.res)
                sch.op("act", lambda e, bank=bank: e.activation(out=rs[:], in_=bank[:, 0:TG], func=AF.Ln, scale=1.0 / 128, bias=self.eps6[:, 0:1]),
                       reads=bank.res + self.eps6.res, writes=rs.res)
                sch.op("act", lambda e: e.activation(out=rs[:], in_=rs[:], func=AF.Exp, scale=-0.5), reads=rs.res, writes=rs.res)
                sch.op("dve", lambda e, oT=oT, gs=gs: e.scalar_tensor_tensor(out=oT[:, gs], in0=oT[:, gs], scalar=self.V(l, "onorm_g"), in1=rs[:],
                                                                           op0=ALU.mult, op1=ALU.mult), reads=oT.res + rs.res + self.vec[l].res, writes=oT.res)
                sch.op("dve", lambda e, oT=oT, ob=ob, zog=zog, gs=gs: e.tensor_tensor(out=ob[:, gs], in0=oT[:, gs], in1=zog[:, gs], op=ALU.mult),
                       reads=oT.res + zog.res, writes=ob.res)
            sch.dma("sp", self.obr[0, h, :, t0:t0 + T], ob[:], reads=ob.res, writes=[self.obr_res[0][h]])


        def stream(X):
            items = [(h, tb) for h in range(4 * X, 4 * X + 4) for tb in range(NB)]
            prep_load(items[0][0], items[0][1], bufs[2 * X])
            prep(items[0][0], items[0][1], bufs[2 * X])
            for i, (h, tb) in enumerate(items):
                nxt_ = items[i + 1] if i + 1 < len(items) else None
                if nxt_ is not None:
                    prep_load(nxt_[0], nxt_[1], bufs[2 * X + (i + 1) % 2])
                first = True
                for _ in tiles(h, tb, bufs[2 * X + i % 2], X):
                    if first and nxt_ is not None:
                        prep(nxt_[0], nxt_[1], bufs[2 * X + (i + 1) % 2])
                    first = False
                    yield

        gens = [stream(0), stream(1)]
        for _ in range(2):
            next(gens[0])
        while gens:
            for g_ in list(gens):
                try:
                    next(g_)
                except StopIteration:
                    gens.remove(g_)
        sch.release(m)

    def stage_rope_tables(self):
        sch, S, nc = self.sch, self.S, self.nc
        self.cs = nc.dram_tensor("cs_tab", [2, 64, S], F32, kind=("ExternalOutput" if "cs_tab" in self.debug else "Internal")).ap()
        self.cs_res = [Res(), Res()]
        m = sch.mark()
        pi = sch.sb([64, S], I32)
        ang = sch.sb([64, S], F32)
        u = sch.sb([64, S], F32)
        ki = sch.sb([64, S], I32)
        kf = sch.sb([64, S], F32)
        idx = sch.sb([64, 1], I32)
        invf = sch.sb([64, 1], F32)
        zero = sch.sb([64, 1], F32)
        sch.op("dve", lambda e: e.memset(zero[:], 0.0), writes=zero.res)
        sch.dma("sp", pi[:], self.pos.partition_broadcast(64), writes=pi.res)
        sch.op("pool", lambda e: e.iota(idx[0:32, :], pattern=[[0, 1]], base=0, channel_multiplier=1), writes=idx.res)
        sch.op("pool", lambda e: e.iota(idx[32:64, :], pattern=[[0, 1]], base=0, channel_multiplier=1), writes=idx.res)
        sch.op("dve", lambda e: e.tensor_copy(out=invf[:], in_=idx[:]), reads=idx.res, writes=invf.res)
        sch.op("act", lambda e: e.activation(out=invf[:], in_=invf[:], func=AF.Exp, scale=-(2.0 / 64) * float(np.log(10000.0))),
               reads=invf.res, writes=invf.res)
        sch.op("dve", lambda e: e.tensor_copy(out=ang[:], in_=pi[:]), reads=pi.res, writes=ang.res)
        sch.op("dve", lambda e: e.tensor_scalar(out=ang[:], in0=ang[:], scalar1=invf[:, 0:1], scalar2=1.0 / (2 * np.pi), op0=ALU.mult, op1=ALU.mult),
               reads=ang.res + invf.res, writes=ang.res)
        for which, off in ((0, 0.75), (1, 0.5)):
            sch.op("dve", lambda e, off=off: e.tensor_scalar(out=u[:], in0=ang[:], scalar1=off, scalar2=None, op0=ALU.add), reads=ang.res, writes=u.res)
            sch.op("dve", lambda e: e.tensor_copy(out=ki[:], in_=u[:]), reads=u.res, writes=ki.res)
            sch.op("dve", lambda e: e.tensor_copy(out=kf[:], in_=ki[:]), reads=ki.res, writes=kf.res)
            sch.op("dve", lambda e: e.tensor_tensor(out=u[:], in0=u[:], in1=kf[:], op=ALU.subtract), reads=u.res + kf.res, writes=u.res)
            sch.op("dve", lambda e: e.tensor_scalar(out=kf[:], in0=u[:], scalar1=0.0, scalar2=None, op0=ALU.is_lt), reads=u.res, writes=kf.res)
            sch.op("dve", lambda e: e.tensor_tensor(out=u[:], in0=u[:], in1=kf[:], op=ALU.add), reads=u.res + kf.res, writes=u.res)
            sch.op("dve", lambda e: e.tensor_scalar(out=u[:], in0=u[:], scalar1=-0.5, scalar2=2 * np.pi, op0=ALU.add, op1=ALU.mult), reads=u.res, writes=u.res)
            sch.op("dve", lambda e: e.tensor_scalar(out=u[:], in0=u[:], scalar1=3.14159, scalar2=-3.14159, op0=ALU.min, op1=ALU.max), reads=u.res, writes=u.res)
            sch.op("act", lambda e: e.activation(out=u[:], in_=u[:], func=AF.Sin, bias=zero[:, 0:1]), reads=u.res + zero.res, writes=u.res)
            sch.dma("sp", self.cs[which], u[:], reads=u.res, writes=[self.cs_res[which]])
        sch.release(m)

    def stage_mla(self, l):
        sch, S, TG = self.sch, self.S, self.TG
        NTG = self.NTG
        nt = S // 128
        PS = self.PS
        scale = float((128 + 64) ** -0.5)
        m = sch.mark()
        cqn = sch.sb([128, 3, S], BF16, nres=NTG)
        ckvn = sch.sb([128, 2, S], BF16, nres=NTG)
        cos2 = sch.sb([64, S], F32)
        sin2 = sch.sb([64, S], F32)
        krT = sch.sb([64, S], BF16, nres=NTG)
        tri = sch.sb([128, 128], BF16)
        trif = sch.sb([128, 128], F32)
        rt = sch.sb([64, 64], F32)
        sch.dma("sp", cos2[:], self.cs[0], reads=[self.cs_res[0]], writes=cos2.res)
        sch.dma("sp", sin2[:], self.cs[1], reads=[self.cs_res[1]], writes=sin2.res)
        sch.op("pool", lambda e: e.memset(trif[:], 1.0), writes=trif.res)
        sch.op("pool", lambda e: e.affine_select(out=trif[:], in_=trif[:], pattern=[[1, 128]], compare_op=ALU.is_ge, fill=0.0, base=0,
                                                 channel_multiplier=-1), reads=trif.res, writes=trif.res)
        sch.op("pool", lambda e: e.tensor_copy(out=tri[:], in_=trif[:]), reads=trif.res, writes=tri.res)
        rt2 = sch.sb([64, 64], F32)
        sch.op("pool", lambda e: e.memset(rt[:], -1.0), writes=rt.res)
        sch.op("pool", lambda e: e.affine_select(out=rt[:], in_=rt[:], pattern=[[-1, 64]], compare_op=ALU.is_equal, fill=0.0, base=-32,
                                                 channel_multiplier=1), reads=rt.res, writes=rt.res)
        sch.op("pool", lambda e: e.memset(rt2[:], 1.0), writes=rt2.res)
        sch.op("pool", lambda e: e.affine_select(out=rt2[:], in_=rt2[:], pattern=[[-1, 64]], compare_op=ALU.is_equal, fill=0.0, base=32,
                                                 channel_multiplier=1), reads=rt2.res, writes=rt2.res)
        sch.op("pool", lambda e: e.tensor_tensor(out=rt[:], in0=rt[:], in1=rt2[:], op=ALU.add), reads=rt.res + rt2.res, writes=rt.res)
        m1 = sch.mark()
        zb = sch.sb([128, 2, 3, TG], F32, nres=2)
        sq = sch.sb([128, 2, 3, TG], BF16, nres=2)
        rs = sch.sb([128, 2, TG], F32, nres=2)
        zk = sch.sb([64, 2, TG], F32, nres=2)
        t1 = sch.sb([64, 2, TG], F32, nres=2)
        t2 = sch.sb([64, 2, TG], F32, nres=2)
        it = 0
        for tg in range(NTG):
            ts = slice(tg * TG, (tg + 1) * TG)
            for (c0, ncn, dst, gname, width) in ((58, 3, cqn, "q_norm_g", 384), (61, 2, ckvn, "kv_norm_g", 256)):
                s = it % 2
                it += 1
                sch.dma("sp", zb[:, s, 0:ncn], self.zT[c0:c0 + ncn, :, ts].rearrange("c p s -> p c s"),
                        reads=self.zT_res[c0:c0 + ncn], writes=[zb.res[s]])
                sch.op("act", lambda e, s=s, ncn=ncn: e.activation(out=sq[:, s, 0:ncn], in_=zb[:, s, 0:ncn], func=AF.Square),
                       reads=[zb.res[s]], writes=[sq.res[s]])
                bank = PS[s]
                for kc in range(ncn):
                    sch.op("pe", lambda e, s=s, kc=kc, bank=bank, ncn=ncn: e.matmul(bank[:, 0:TG], lhsT=self.ones_bf[:], rhs=sq[:, s, kc, :],
                                                                                 start=(kc == 0), stop=(kc == ncn - 1)),
                           reads=[sq.res[s]] + self.ones_bf.res, writes=bank.res)
                sch.op("act", lambda e, s=s, bank=bank, width=width: e.activation(out=rs[:, s], in_=bank[:, 0:TG], func=AF.Ln, scale=1.0 / width,
                                                                               bias=self.eps6[:, 0:1]), reads=bank.res + self.eps6.res, writes=[rs.res[s]])
                sch.op("act", lambda e, s=s: e.activation(out=rs[:, s], in_=rs[:, s], func=AF.Exp, scale=-0.5), reads=[rs.res[s]], writes=[rs.res[s]])
                for kc in range(ncn):
                    sch.op("dve", lambda e, s=s, kc=kc, dst=dst, gname=gname, ts=ts: e.scalar_tensor_tensor(
                        out=dst[:, kc, ts], in0=zb[:, s, kc, :], scalar=self.V(l, gname, kc), in1=rs[:, s], op0=ALU.mult, op1=ALU.mult),
                        reads=[zb.res[s], rs.res[s]] + self.vec[l].res, writes=[dst.res[tg]])
            s = tg % 2
            sch.dma("sp", zk[:, s], self.zT[63, 0:64, ts], reads=[self.zT_res[63]], writes=[zk.res[s]])
            bank = PS[2 + s]
            sch.op("pe", lambda e, s=s, bank=bank: e.matmul(bank[0:64, 0:TG], lhsT=rt[:], rhs=zk[:, s], start=True, stop=True),
                   reads=[zk.res[s]] + rt.res, writes=bank.res)
            sch.op("dve", lambda e, s=s, ts=ts: e.tensor_tensor(out=t1[:, s], in0=zk[:, s], in1=cos2[:, ts], op=ALU.mult),
                   reads=[zk.res[s]] + cos2.res, writes=[t1.res[s]])
            sch.op("dve", lambda e, s=s, ts=ts, bank=bank: e.tensor_tensor(out=t2[:, s], in0=bank[0:64, 0:TG], in1=sin2[:, ts], op=ALU.mult),
                   reads=bank.res + sin2.res, writes=[t2.res[s]])
            sch.op("pool", lambda e, s=s, ts=ts: e.tensor_tensor(out=krT[:, ts], in0=t1[:, s], in1=t2[:, s], op=ALU.add),
                   reads=[t1.res[s], t2.res[s]], writes=[krT.res[tg]])
        sch.release(m1)
        wq = sch.sb([128, 3, 192], BF16)
        wqr = sch.sb([128, 3, 64], BF16)
        wkv = sch.sb([128, 2, 256], BF16)
        kT = sch.sb([128, S], BF16, nres=NTG)
        vh = sch.sb([128, nt, 128], BF16, nres=nt)
        qn = sch.sb([128, S], BF16, nres=NTG)
        qr = sch.sb([64, S], BF16, nres=NTG)
        oT = sch.sb([128, S], BF16)
        pT = sch.sb([128, 4, 512], BF16, nres=4)
        rec = sch.sb([128, 2, 512], F32, nres=2)
        t1h = sch.sb([64, 2, TG], F32, nres=2)
        t2h = sch.sb([64, 2, TG], F32, nres=2)
        pit = 0
        for h in range(8):
            sch.dma("pool", wq[:], self.w_uq[l][:, h * 192:(h + 1) * 192].rearrange("(kc p) f -> p kc f", p=128), writes=wq.res)
            sch.dma("pool", wkv[:], self.w_ukv[l][:, h * 256:(h + 1) * 256].rearrange("(kc p) f -> p kc f", p=128), writes=wkv.res)
            sch.op("pool", lambda e: e.tensor_scalar(out=wqr[:, :, 0:32], in0=wq[:, :, 160:192], scalar1=-1.0, scalar2=None, op0=ALU.mult),
                   reads=wq.res, writes=wqr.res)
            sch.op("pool", lambda e: e.tensor_copy(out=wqr[:, :, 32:64], in_=wq[:, :, 128:160]), reads=wq.res, writes=wqr.res)
            for tg in range(NTG):
                ts = slice(tg * TG, (tg + 1) * TG)
                s = tg % 2
                bk, bq, br, br2 = PS[0], PS[1], PS[2], PS[3]
                for kc in range(2):
                    sch.op("pe", lambda e, kc=kc, ts=ts: e.matmul(bk[:, 0:TG], lhsT=wkv[:, kc, 0:128], rhs=ckvn[:, kc, ts], start=(kc == 0), stop=(kc == 1)),
                           reads=wkv.res + [ckvn.res[tg]], writes=bk.res)
                sch.op("act", lambda e, ts=ts: e.copy(out=kT[:, ts], in_=bk[:, 0:TG]), reads=bk.res, writes=[kT.res[tg]])
                for kc in range(3):
                    sch.op("pe", lambda e, kc=kc, ts=ts: e.matmul(bq[:, 0:TG], lhsT=wq[:, kc, 0:128], rhs=cqn[:, kc, ts], start=(kc == 0), stop=(kc == 2)),
                           reads=wq.res + [cqn.res[tg]], writes=bq.res)
                sch.op("dve", lambda e, ts=ts: e.tensor_copy(out=qn[:, ts], in_=bq[:, 0:TG]), reads=bq.res, writes=[qn.res[tg]])
                for kc in range(3):
                    sch.op("pe", lambda e, kc=kc, ts=ts: e.matmul(br[0:64, 0:TG], lhsT=wq[:, kc, 128:192], rhs=cqn[:, kc, ts], start=(kc == 0), stop=(kc == 2)),
                           reads=wq.res + [cqn.res[tg]], writes=br.res)
                for kc in range(3):
                    sch.op("pe", lambda e, kc=kc, ts=ts: e.matmul(br2[0:64, 0:TG], lhsT=wqr[:, kc, :], rhs=cqn[:, kc, ts], start=(kc == 0), stop=(kc == 2)),
                           reads=wqr.res + [cqn.res[tg]], writes=br2.res)
                sch.op("dve", lambda e, s=s, ts=ts: e.tensor_tensor(out=t1h[:, s], in0=br[0:64, 0:TG], in1=cos2[:, ts], op=ALU.mult),
                       reads=br.res + cos2.res, writes=[t1h.res[s]])
                sch.op("dve", lambda e, s=s, ts=ts: e.tensor_tensor(out=t2h[:, s], in0=br2[0:64, 0:TG], in1=sin2[:, ts], op=ALU.mult),
                       reads=br2.res + sin2.res, writes=[t2h.res[s]])
                sch.op("pool", lambda e, s=s, ts=ts: e.tensor_tensor(out=qr[:, ts], in0=t1h[:, s], in1=t2h[:, s], op=ALU.add),
                       reads=[t1h.res[s], t2h.res[s]], writes=[qr.res[tg]])
            for g in range((nt + 3) // 4):
                bank = PS[4 + g % 2]
                na = min(4, nt - g * 4)
                for a in range(na):
                    tt = g * 4 + a
                    tg = (tt * 128) // TG
                    for kc in range(2):
                        sch.op("pe", lambda e, kc=kc, tt=tt, a=a, bank=bank: e.matmul(bank[:, a * 128:(a + 1) * 128], lhsT=ckvn[:, kc, tt * 128:(tt + 1) * 128],
                                                                                   rhs=wkv[:, kc, 128:256], start=(kc == 0), stop=(kc == 1)),
                               reads=wkv.res + [ckvn.res[tg]], writes=bank.res)
                sch.op("act", lambda e, g=g, na=na, bank=bank: e.copy(out=vh[:, g * 4:g * 4 + na, :], in_=bank[:, 0:na * 128].rearrange("p (a v) -> p a v", v=128)),
                       reads=bank.res, writes=vh.res[g * 4:g * 4 + na])
            GQ = min(4, nt)
            for G in range(nt // GQ):
                W = GQ * 128
                q0 = G * W
                qtg = q0 // TG
                po, pd = PS[4 + 2 * (G % 2)], PS[5 + 2 * (G % 2)]
                nkb = G * GQ + GQ
                slots = {}

                def scores(j, G=G, W=W, q0=q0, qtg=qtg):
                    nonlocal pit
                    a = max(0, j - G * GQ)
                    c0 = a * 128
                    s = pit % 4
                    pit += 1
                    slots[j] = (s, c0)
                    bank = PS[s % 4]
                    ks = slice(j * 128, (j + 1) * 128)
                    ktg = (j * 128) // TG
                    qs = slice(q0 + c0, q0 + W)
                    sch.op("pe", lambda e: e.matmul(bank[:, c0:W], lhsT=kT[:, ks], rhs=qn[:, qs], start=True, stop=False),
                           reads=[kT.res[ktg], qn.res[qtg]], writes=bank.res)
                    sch.op("pe", lambda e: e.matmul(bank[:, c0:W], lhsT=krT[:, ks], rhs=qr[:, qs], start=False, stop=True),
                           reads=[krT.res[ktg], qr.res[qtg]], writes=bank.res)
                    sch.op("act", lambda e: e.activation(out=pT[:, s, c0:W], in_=bank[:, c0:W], func=AF.Exp, scale=scale),
                           reads=bank.res, writes=[pT.res[s]])
                    if j >= G * GQ:
                        sch.op("pool", lambda e: e.tensor_tensor(out=pT[:, s, c0:c0 + 128], in0=pT[:, s, c0:c0 + 128], in1=tri[:], op=ALU.mult),
                               reads=[pT.res[s]] + tri.res, writes=[pT.res[s]])

                def pv(j, W=W, po=po, pd=pd, nkb=nkb):
                    s, c0 = slots[j]
                    sch.op("pe", lambda e: e.matmul(po[:, c0:W], lhsT=vh[:, j, :], rhs=pT[:, s, c0:W], start=(j == 0), stop=(j == nkb - 1)),
                           reads=[vh.res[j], pT.res[s]], writes=po.res)
                    sch.op("pe", lambda e: e.matmul(pd[:, c0:W], lhsT=self.ones_bf[:], rhs=pT[:, s, c0:W], start=(j == 0), stop=(j == nkb - 1)),
                           reads=self.ones_bf.res + [pT.res[s]], writes=pd.res)

                scores(0)
                if nkb > 1:
                    scores(1)
                for j in range(nkb):
                    if j + 2 < nkb:
                        scores(j + 2)
                    pv(j)
                s2 = G % 2
                sch.op("dve", lambda e, s2=s2, pd=pd, W=W: e.reciprocal(out=rec[:, s2, 0:W], in_=pd[:, 0:W]), reads=pd.res, writes=[rec.res[s2]])
                sch.op("dve", lambda e, s2=s2, po=po, W=W, q0=q0: e.tensor_tensor(out=oT[:, q0:q0 + W], in0=po[:, 0:W], in1=rec[:, s2, 0:W], op=ALU.mult),
                       reads=po.res + [rec.res[s2]], writes=oT.res)
            sch.dma("sp", self.obr[2, h], oT[:], reads=oT.res, writes=[self.obr_res[2][h]])
        sch.release(m)

    def stage_rwkv(self, l):
        sch, S = self.sch, self.S
        T = min(S, 512)
        NBK = S // T
        NC = T // 64
        NQ = NC // 4
        PS = self.PS
        C0 = 0.6065306597126334
        m = sch.mark()
        blk = sch.sb([128, 128], BF16)
        blkf = sch.sb([128, 128], F32)
        mus = sch.sb([128, 128], F32)
        mui = sch.sb([128, 128], F32)
        mls = sch.sb([128, 128], F32)
        mask64 = sch.sb([128, T], F32)
        tiny = sch.sb([128, 1], F32)
        eps_ln = sch.sb([128, 1], F32)
        sch.op("pool", lambda e: e.memset(tiny[:], 1e-12), writes=tiny.res)
        sch.op("pool", lambda e: e.memset(eps_ln[:], 64e-5), writes=eps_ln.res)
        sch.op("pool", lambda e: e.memset(blkf[:], 0.0), writes=blkf.res)
        sch.op("pool", lambda e: e.memset(blkf[0:64, 0:64], 1.0), writes=blkf.res)
        sch.op("pool", lambda e: e.memset(blkf[64:128, 64:128], 1.0), writes=blkf.res)
        sch.op("pool", lambda e: e.tensor_copy(out=blk[:], in_=blkf[:]), reads=blkf.res, writes=blk.res)
        for (mt, op_, base, cm, pat) in ((mus, ALU.is_gt, 0, -1, 1), (mui, ALU.is_ge, 0, -1, 1), (mls, ALU.is_gt, 0, 1, -1)):
            sch.op("pool", lambda e, mt=mt: e.memset(mt[:], 1.0), writes=mt.res)
            sch.op("pool", lambda e, mt=mt, op_=op_, base=base, cm=cm, pat=pat: e.affine_select(
                out=mt[:], in_=mt[:], pattern=[[pat, 128]], compare_op=op_, fill=0.0, base=base, channel_multiplier=cm),
                reads=mt.res, writes=mt.res)
        sch.op("pool", lambda e: e.memset(mask64[:], 1.0), writes=mask64.res)
        sch.op("pool", lambda e: e.memset(mask64[:].rearrange("p (c i) -> p c i", i=64)[:, :, 0:1], 0.0), writes=mask64.res)

        def shift(dst_ap, zl, mu_ap, np_, eng_r, dt):
            sch.op("dve", lambda e: e.tensor_tensor(out=dt[0:np_, 0:T], in0=zl[0:np_, 0:T], in1=zl[0:np_, 1:T + 1], op=ALU.subtract),
                   reads=zl.res, writes=dt.res)
            sch.op("dve", lambda e: e.scalar_tensor_tensor(out=dst_ap, in0=dt[0:np_, 0:T], scalar=mu_ap, in1=zl[0:np_, 1:T + 1], op0=ALU.mult, op1=ALU.add),
                   reads=zl.res + dt.res + self.vec[l].res, writes=eng_r)

        def load_prev(zl, zc, t0, np_=128, p0=0):
            if t0 == 0:
                sch.op("pool", lambda e: e.memset(zl[p0:p0 + np_, 0:1], 0.0), writes=zl.res)
                sch.dma("sp", zl[p0:p0 + np_, 1:T + 1], self.zT[zc, p0:p0 + np_, 0:T], reads=[self.zT_res[zc]], writes=zl.res)
            else:
                sch.dma("sp", zl[p0:p0 + np_, 0:T + 1], self.zT[zc, p0:p0 + np_, t0 - 1:t0 + T], reads=[self.zT_res[zc]], writes=zl.res)

        tw = sch.sb([128, S], BF16)
        sgg = sch.sb([128, S], BF16)
        zvv = sch.sb([32, S], BF16)
        m0 = sch.mark()
        zl0 = [sch.sb([128, T + 1], F32) for _ in range(2)]
        tmp0 = [sch.sb([128, T], F32) for _ in range(2)]
        dt0 = sch.sb([128, T], F32)
        for tb in range(NBK):
            t0 = tb * T
            zl, tmp = zl0[tb % 2], tmp0[tb % 2]
            load_prev(zl, 56, t0)
            shift(tmp[:, :], zl, self.V(l, "mu", 24), 128, tmp.res, dt0)
            sch.op("act", lambda e, tmp=tmp, t0=t0: e.activation(out=tw[0:64, t0:t0 + T], in_=tmp[0:64, :], func=AF.Tanh), reads=tmp.res, writes=tw.res)
            sch.op("pool", lambda e, tmp=tmp, t0=t0: e.tensor_copy(out=tw[64:128, t0:t0 + T], in_=tmp[64:128, :]), reads=tmp.res, writes=tw.res)
        for tb in range(NBK):
            t0 = tb * T
            zl, tmp = zl0[tb % 2], tmp0[tb % 2]
            load_prev(zl, 57, t0)
            shift(tmp[:, :], zl, self.V(l, "mu", 25), 128, tmp.res, dt0)
            sch.op("act", lambda e, tmp=tmp, t0=t0: e.activation(out=sgg[:, t0:t0 + T], in_=tmp[:, :], func=AF.Sigmoid), reads=tmp.res, writes=sgg.res)
        if l > 0:
            for tb in range(NBK):
                t0 = tb * T
                zl, tmp = zl0[tb % 2], tmp0[tb % 2]
                load_prev(zl, 88, t0, 32)
                shift(tmp[0:32, :], zl, self.V(l, "vres_mu")[0:32, :], 32, tmp.res, dt0)
                sch.op("act", lambda e, tmp=tmp, t0=t0: e.copy(out=zvv[:, t0:t0 + T], in_=tmp[0:32, :]), reads=tmp.res, writes=zvv.res)
        sch.release(m0)
        f32n = ["zr", "zk", "zv"]
        zls = {n: sch.sb([128, T + 1], F32) for n in f32n}
        A = {n: sch.sb([128, T], F32) for n in ("rs", "ks", "v", "sig", "a", "kkn", "kmod", "b", "cw", "e2", "t1", "t2", "vf")}
        Bb = {n: sch.sb([128, T], BF16) for n in ("kk2", "rkb")}
        P2 = [{n: sch.sb([128, NC, 128], BF16) for n in ("a", "r", "b", "k", "B", "K", "V")} for _ in range(2)]
        for Pq in P2:
            for n in Pq:
                sch.op("pool", lambda e, n=n, Pq=Pq: e.memset(Pq[n][:], 0.0), writes=Pq[n].res)
        E1 = [sch.sb([128, T], F32) for _ in range(2)]
        GB3 = [dict(g=sch.sb([128, T], F32), bonus=sch.sb([128, T], F32)) for _ in range(3)]
        Q = {n: sch.sb([128, 2, 4, 128], BF16, nres=2) for n in ("Ta", "TB", "TK", "TV", "NT", "Nn", "Aak", "Arb", "Ark", "nA0", "nA1", "nB0", "nB1",
                                                               "PT0", "PT1")}
        Q["Ap"], Q["X"], Q["Vp"] = Q["nA1"], Q["nB0"], Q["nB1"]
        Sbd = sch.sb([128, 2, 128], BF16, nres=2)
        wup = sch.sb([128, 128], BF16)
        gup = sch.sb([128, 128], BF16)
        vup = sch.sb([32, 128], BF16)
        omka = sch.sb([128, 1], F32)
        bankc = [0, 0]

        def nb():
            bankc[0] += 1
            return PS[bankc[0] % 6]

        def nbp():
            bankc[1] += 1
            return PS[6 + bankc[1] % 2]

        evc = [0]

        def evac_copy(out_ap, in_ap, reads, writes):
            sch.op("act", lambda e: e.copy(out=out_ap, in_=in_ap), reads=reads, writes=writes)

        def v4(bank, bf=False):
            if bf:
                return bank[:].bitcast(BF16)[:, 0:512].rearrange("p (a q) -> p a q", q=128)
            return bank[:, :].rearrange("p (a q) -> p a q", q=128)

        def bc4(t):
            return t[:].unsqueeze(1).broadcast_to([128, 4, 128])

        D2 = [dict(Yp=sch.sb([128, NC, 128], F32), Gs=sch.sb([128, NC, 128], F32), MT=sch.sb([128, NC, 128], BF16), RT=sch.sb([128, NC, 128], BF16),
                   yT=sch.sb([128, T], F32), ob=sch.sb([128, T], BF16)) for _ in range(2)]
        PT_ = dict(t1=sch.sb([128, T], F32), t2=sch.sb([128, T], F32), ybf=Bb["kk2"], ysq=Bb["rkb"])
        sidx = [0]
        pending = None
        itc = [0]

        def quad_gen(qi, Dd, e1tile, Pp):
            e13 = e1tile[:].rearrange("p (c i) -> p c i", i=64)
            s = qi % 2
            cs = slice(qi * 4, qi * 4 + 4)

            def mm4(bank, L, Rr, lres, rres, start=True, stop=True, bf=False, tr=False):
                for a in range(4):
                    la = L(a)
                    if tr:
                        ov = bank[:].bitcast(BF16)[:, a * 128:(a + 1) * 128]
                        sch.op("pe", lambda e, ov=ov, la=la: e.transpose(out=ov, in_=la, identity=self.ident_bf[:]), reads=lres + self.ident_bf.res, writes=bank.res)
                    else:
                        ra = Rr(a)
                        sch.op("pe", lambda e, a=a, la=la, ra=ra: e.matmul(bank[:, a * 128:(a + 1) * 128], lhsT=la, rhs=ra, start=start, stop=stop),
                               reads=lres + rres, writes=bank.res)

            def pq(n):
                return lambda a, n=n, qi=qi: Pp[n][:, qi * 4 + a, :]

            def qq(n):
                return lambda a, n=n, s=s: Q[n][:, s, a, :]

            for src, dst in (("a", "Ta"), ("B", "TB"), ("K", "TK"), ("V", "TV")):
                bank = nb()
                mm4(bank, pq(src), None, Pp[src].res, [], tr=True)
                evac_copy(Q[dst][:, s], v4(bank, True), bank.res, [Q[dst].res[s]])
                yield
            for Ln, Rn, mk, dst in (("b", "a", mus, "NT"), ("a", "b", mls, "Nn"), ("k", "a", mus, "Aak"), ("b", "r", mui, "Arb"), ("k", "r", mui, "Ark")):
                bank = nb()
                mm4(bank, pq(Ln), pq(Rn), Pp[Ln].res, Pp[Rn].res)
                sch.op("dve", lambda e, s=s, cs=cs, qi=qi, bank=bank, dst=dst, mk=mk: e.tensor_tensor(out=Q[dst][:, s], in0=v4(bank), in1=bc4(mk), op=ALU.mult),
                       reads=bank.res + mk.res, writes=[Q[dst].res[s]])
            sch.op("pool", lambda e, s=s, cs=cs, qi=qi: e.tensor_tensor(out=Q["PT0"][:, s], in0=Q["NT"][:, s], in1=bc4(self.ident_bf), op=ALU.add),
                   reads=[Q["NT"].res[s]] + self.ident_bf.res, writes=[Q["PT0"].res[s]])
            curT, cur = "NT", "Nn"
            for lv in range(5):
                nA, nB = f"nA{lv % 2}", f"nB{lv % 2}"
                pin, pout = f"PT{lv % 2}", f"PT{(lv + 1) % 2}"
                bank = nb()
                mm4(bank, qq(curT), qq(cur), [Q[curT].res[s]], [Q[cur].res[s]])
                evac_copy(Q[nA][:, s], v4(bank), bank.res, [Q[nA].res[s]])
                yield
                if lv < 4:
                    bank = nb()
                    mm4(bank, qq(cur), qq(curT), [Q[cur].res[s]], [Q[curT].res[s]])
                    evac_copy(Q[nB][:, s], v4(bank), bank.res, [Q[nB].res[s]])
                    yield
                bank = nb()
                for a in range(4):
                    sch.op("pe", lambda e, a=a, bank=bank, pin=pin: e.matmul(bank[:, a * 128:(a + 1) * 128], lhsT=self.ident_bf[:], rhs=Q[pin][:, s, a, :], start=True, stop=False),
                           reads=self.ident_bf.res + [Q[pin].res[s]], writes=bank.res)
                    sch.op("pe", lambda e, a=a, bank=bank, pin=pin, nA=nA: e.matmul(bank[:, a * 128:(a + 1) * 128], lhsT=Q[nA][:, s, a, :], rhs=Q[pin][:, s, a, :], start=False, stop=True),
                           reads=[Q[nA].res[s], Q[pin].res[s]], writes=bank.res)
                evac_copy(Q[pout][:, s], v4(bank), bank.res, [Q[pout].res[s]])
                yield
                curT, cur = nB, nA
            TT = "PT1"
            bank = nb()
            mm4(bank, qq(TT), qq("Ta"), [Q[TT].res[s]], [Q["Ta"].res[s]])
            evac_copy(Q["Ap"][:, s], v4(bank), bank.res, [Q["Ap"].res[s]])
            yield
            bank = nb()
            mm4(bank, qq("Aak"), qq("TV"), [Q["Aak"].res[s]], [Q["TV"].res[s]])
            evac_copy(Q["X"][:, s], v4(bank), bank.res, [Q["X"].res[s]])
            yield
            bank = nb()
            mm4(bank, qq(TT), qq("X"), [Q[TT].res[s]], [Q["X"].res[s]])
            evac_copy(Q["Vp"][:, s], v4(bank), bank.res, [Q["Vp"].res[s]])
            yield
            bank = nb()
            for a in range(4):
                sch.op("pe", lambda e, s=s, cs=cs, qi=qi, a=a, bank=bank: e.matmul(bank[:, a * 128:(a + 1) * 128], lhsT=Q["Vp"][:, s, a, :], rhs=Q["Arb"][:, s, a, :], start=True, stop=False),
                       reads=[Q["Vp"].res[s], Q["Arb"].res[s]], writes=bank.res)
                sch.op("pe", lambda e, s=s, cs=cs, qi=qi, a=a, bank=bank: e.matmul(bank[:, a * 128:(a + 1) * 128], lhsT=Q["TV"][:, s, a, :], rhs=Q["Ark"][:, s, a, :], start=False, stop=True),
                       reads=[Q["TV"].res[s], Q["Ark"].res[s]], writes=bank.res)
            evac_copy(Dd["Yp"][:, cs, :], v4(bank), bank.res, Dd["Yp"].res)
            yield
            bank = nb()
            for a in range(4):
                sch.op("pe", lambda e, s=s, cs=cs, qi=qi, a=a, bank=bank: e.matmul(bank[:, a * 128:(a + 1) * 128], lhsT=Q["TB"][:, s, a, :], rhs=Q["Vp"][:, s, a, :], start=True, stop=False),
                       reads=[Q["TB"].res[s], Q["Vp"].res[s]], writes=bank.res)
                sch.op("pe", lambda e, s=s, cs=cs, qi=qi, a=a, bank=bank: e.matmul(bank[:, a * 128:(a + 1) * 128], lhsT=Q["TK"][:, s, a, :], rhs=Q["TV"][:, s, a, :], start=False, stop=True),
                       reads=[Q["TK"].res[s], Q["TV"].res[s]], writes=bank.res)
            evac_copy(Dd["Gs"][:, cs, :], v4(bank), bank.res, Dd["Gs"].res)
            yield
            bank = nb()
            mm4(bank, qq("Ap"), qq("TB"), [Q["Ap"].res[s]], [Q["TB"].res[s]])
            for a in range(4):
                c = qi * 4 + a
                sch.op("dve", lambda e, s=s, cs=cs, qi=qi, a=a, c=c, bank=bank, e13=e13: e.scalar_tensor_tensor(out=Dd["MT"][:, c, :], in0=self.ident_f[:], scalar=e13[:, c, 63:64],
                                                                                           in1=bank[:, a * 128:(a + 1) * 128], op0=ALU.mult, op1=ALU.add),
                       reads=bank.res + self.ident_f.res + e1tile.res, writes=Dd["MT"].res)
            bank = nb()
            for a in range(4):
                sch.op("pe", lambda e, a=a, bank=bank: e.matmul(bank[:, a * 128:(a + 1) * 128], lhsT=self.ident_bf[:], rhs=Pp["r"][:, qi * 4 + a, :], start=True, stop=False),
                       reads=self.ident_bf.res + Pp["r"].res, writes=bank.res)
                sch.op("pe", lambda e, a=a, bank=bank: e.matmul(bank[:, a * 128:(a + 1) * 128], lhsT=Q["Ap"][:, s, a, :], rhs=Q["Arb"][:, s, a, :], start=False, stop=True),
                       reads=[Q["Ap"].res[s], Q["Arb"].res[s]], writes=bank.res)
            evac_copy(Dd["RT"][:, cs, :], v4(bank), bank.res, Dd["RT"].res)

        def seq_gen(Dd, reset):
            if reset:
                cur0 = sidx[0] % 2
                sch.op("pool", lambda e: e.memset(Sbd[:, cur0], 0.0), writes=[Sbd.res[cur0]])
            for c in range(NC):
                cur = sidx[0] % 2
                nxt = (sidx[0] + 1) % 2
                sidx[0] += 1
                by, bs = nb(), nb()
                sch.op("pe", lambda e, by=by, cur=cur, c=c: e.matmul(by[:, 0:128], lhsT=Sbd[:, cur], rhs=Dd["RT"][:, c, :], start=True, stop=True),
                       reads=[Sbd.res[cur]] + Dd["RT"].res, writes=by.res)
                sch.op("pe", lambda e, bs=bs, cur=cur, c=c: e.matmul(bs[:, 0:128], lhsT=Dd["MT"][:, c, :], rhs=Sbd[:, cur], start=True, stop=True),
                       reads=[Sbd.res[cur]] + Dd["MT"].res, writes=bs.res)
                sch.op("dve", lambda e, bs=bs, nxt=nxt, c=c: e.tensor_tensor(out=Sbd[:, nxt], in0=bs[:, 0:128], in1=Dd["Gs"][:, c, :], op=ALU.add),
                       reads=bs.res + Dd["Gs"].res, writes=[Sbd.res[nxt]])
                for hh in range(2):
                    ps_ = slice(hh * 64, hh * 64 + 64)
                    sch.op("dve", lambda e, by=by, ps_=ps_, c=c, hh=hh: e.tensor_tensor(out=Dd["yT"][ps_, c * 64:(c + 1) * 64], in0=by[ps_, hh * 64:hh * 64 + 64],
                                                                                  in1=Dd["Yp"][ps_, c, hh * 64:hh * 64 + 64], op=ALU.add),
                           reads=by.res + Dd["Yp"].res, writes=Dd["yT"].res)
                yield

        def post(Dd, j, tsl, gb):
            sch.op("act", lambda e: e.copy(out=PT_["ybf"][:], in_=Dd["yT"][:]), reads=Dd["yT"].res, writes=PT_["ybf"].res)
            sch.op("act", lambda e: e.activation(out=PT_["ysq"][:], in_=Dd["yT"][:], func=AF.Square), reads=Dd["yT"].res, writes=PT_["ysq"].res)
            bm, bq = nb(), nb()
            sch.op("pe", lambda e, bm=bm: e.matmul(bm[:, 0:T], lhsT=blk[:], rhs=PT_["ybf"][:], start=True, stop=True), reads=blk.res + PT_["ybf"].res, writes=bm.res)
            sch.op("pe", lambda e, bq=bq: e.matmul(bq[:, 0:T], lhsT=blk[:], rhs=PT_["ysq"][:], start=True, stop=True), reads=blk.res + PT_["ysq"].res, writes=bq.res)
            sch.op("act", lambda e, bm=bm: e.activation(out=PT_["t1"][:], in_=bm[:, 0:T], func=AF.Square, scale=1.0 / 64), reads=bm.res, writes=PT_["t1"].res)
            sch.op("dve", lambda e, bq=bq: e.scalar_tensor_tensor(out=PT_["t1"][:], in0=bq[:, 0:T], scalar=1.0 / 64, in1=PT_["t1"][:], op0=ALU.mult, op1=ALU.subtract),
                   reads=bq.res + PT_["t1"].res, writes=PT_["t1"].res)
            sch.op("act", lambda e: e.activation(out=PT_["t1"][:], in_=PT_["t1"][:], func=AF.Ln, bias=eps_ln[:, 0:1]), reads=PT_["t1"].res + eps_ln.res, writes=PT_["t1"].res)
            sch.op("act", lambda e: e.activation(out=PT_["t1"][:], in_=PT_["t1"][:], func=AF.Exp, scale=-0.5), reads=PT_["t1"].res, writes=PT_["t1"].res)
            sch.op("dve", lambda e, bm=bm: e.scalar_tensor_tensor(out=PT_["t2"][:], in0=bm[:, 0:T], scalar=-1.0 / 64, in1=Dd["yT"][:], op0=ALU.mult, op1=ALU.add),
                   reads=bm.res + Dd["yT"].res, writes=PT_["t2"].res)
            sch.op("dve", lambda e, j=j: e.scalar_tensor_tensor(out=PT_["t2"][:], in0=PT_["t2"][:], scalar=self.V(l, "lnx_g", j), in1=PT_["t1"][:], op0=ALU.mult, op1=ALU.mult),
                   reads=PT_["t2"].res + PT_["t1"].res + self.vec[l].res, writes=PT_["t2"].res)
            sch.op("dve", lambda e, j=j: e.scalar_tensor_tensor(out=PT_["t2"][:], in0=PT_["t2"][:], scalar=self.V(l, "lnx_b", j), in1=gb["bonus"][:], op0=ALU.add, op1=ALU.add),
                   reads=PT_["t2"].res + gb["bonus"].res + self.vec[l].res, writes=PT_["t2"].res)
            sch.op("dve", lambda e: e.tensor_tensor(out=Dd["ob"][:], in0=PT_["t2"][:], in1=gb["g"][:], op=ALU.mult), reads=PT_["t2"].res + gb["g"].res, writes=Dd["ob"].res)
            sch.dma("sp", self.obr[1, j, :, tsl], Dd["ob"][:], reads=Dd["ob"].res, writes=[self.obr_res[1][j]])

        def prep_gen(j, tb, Dd, Pp, e1t, gb):
            t0 = tb * T
            tsl = slice(t0, t0 + T)
            if tb == 0:
                fs = slice(j * 128, (j + 1) * 128)
                sch.dma("pool", wup[0:64, :], self.w_up[l][:, fs], writes=wup.res)
                sch.dma("pool", wup[64:128, :], self.a_up[l][:, fs], writes=wup.res)
                sch.dma("pool", gup[:], self.g_up[l][:, fs], writes=gup.res)
                if l > 0:
                    sch.dma("pool", vup[:], self.v_up[:, fs], writes=vup.res)
                sch.op("dve", lambda e: e.tensor_scalar(out=omka[:], in0=self.V(l, "k_a", j), scalar1=-1.0, scalar2=1.0, op0=ALU.mult, op1=ALU.add),
                       reads=self.vec[l].res, writes=omka.res)
            for n, zc, dst in (("zr", 32 + j, "rs"), ("zk", 40 + j, "ks"), ("zv", 48 + j, "v")):
                load_prev(zls[n], zc, t0)
                shift(A[dst][:, :], zls[n], self.V(l, "mu", zc - 32), 128, A[dst].res, A["t1"])
            b0, b1, b2, b3 = nbp(), nbp(), nbp(), nbp()
            sch.op("pe", lambda e, b0=b0, tsl=tsl: e.matmul(b0[:, 0:T], lhsT=wup[0:64, :], rhs=tw[0:64, tsl], start=True, stop=True),
                   reads=wup.res + tw.res, writes=b0.res)
            sch.op("act", lambda e, b0=b0, j=j: e.activation(out=A["sig"][:], in_=b0[:, 0:T], func=AF.Sigmoid, bias=self.V(l, "w0", j)),
                   reads=b0.res + self.vec[l].res, writes=A["sig"].res)
            yield
            sch.op("pe", lambda e, b1=b1, tsl=tsl: e.matmul(b1[:, 0:T], lhsT=wup[64:128, :], rhs=tw[64:128, tsl], start=True, stop=True),
                   reads=wup.res + tw.res, writes=b1.res)
            sch.op("act", lambda e, b1=b1, j=j: e.activation(out=A["a"][:], in_=b1[:, 0:T], func=AF.Sigmoid, bias=self.V(l, "a0", j)),
                   reads=b1.res + self.vec[l].res, writes=A["a"].res)
            yield
            sch.op("pe", lambda e, b2=b2, tsl=tsl: e.matmul(b2[:, 0:T], lhsT=gup[:], rhs=sgg[:, tsl], start=True, stop=True),
                   reads=gup.res + sgg.res, writes=b2.res)
            sch.op("act", lambda e, b2=b2, Dd=Dd: e.copy(out=gb["g"][:], in_=b2[:, 0:T]), reads=b2.res, writes=gb["g"].res)
            yield
            if l == 0:
                sch.dma("sp", self.vfirst[j, :, tsl], A["v"][:], reads=A["v"].res, writes=[self.vfirst_res[j]])
            else:
                sch.dma("sp", A["vf"][:], self.vfirst[j, :, tsl], reads=[self.vfirst_res[j]], writes=A["vf"].res)
                sch.op("pe", lambda e, b3=b3, tsl=tsl: e.matmul(b3[:, 0:T], lhsT=vup[0:32, :], rhs=zvv[0:32, tsl], start=True, stop=True),
                       reads=vup.res + zvv.res, writes=b3.res)
                sch.op("act", lambda e, b3=b3, j=j: e.activation(out=A["t1"][:], in_=b3[:, 0:T], func=AF.Sigmoid, bias=self.V(l, "v0", j)),
                       reads=b3.res + self.vec[l].res, writes=A["t1"].res)
                sch.op("pool", lambda e: e.tensor_tensor(out=A["vf"][:], in0=A["vf"][:], in1=A["v"][:], op=ALU.subtract),
                       reads=A["vf"].res + A["v"].res, writes=A["vf"].res)
                sch.op("dve", lambda e: e.tensor_tensor(out=A["vf"][:], in0=A["vf"][:], in1=A["t1"][:], op=ALU.mult),
                       reads=A["vf"].res + A["t1"].res, writes=A["vf"].res)
                sch.op("pool", lambda e: e.tensor_tensor(out=A["v"][:], in0=A["v"][:], in1=A["vf"][:], op=ALU.add),
                       reads=A["vf"].res + A["v"].res, writes=A["v"].res)
            sch.op("act", lambda e, j=j: e.activation(out=Bb["kk2"][:], in_=A["ks"][:], func=AF.Square, scale=self.V(l, "k_k", j)),
                   reads=A["ks"].res + self.vec[l].res, writes=Bb["kk2"].res)
            b4 = nbp()
            sch.op("pe", lambda e, b4=b4: e.matmul(b4[:, 0:T], lhsT=blk[:], rhs=Bb["kk2"][:], start=True, stop=True), reads=blk.res + Bb["kk2"].res, writes=b4.res)
            yield
            sch.op("act", lambda e, b4=b4: e.activation(out=A["t2"][:], in_=b4[:, 0:T], func=AF.Ln, bias=tiny[:, 0:1]), reads=b4.res + tiny.res, writes=A["t2"].res)
            sch.op("act", lambda e: e.activation(out=A["t2"][:], in_=A["t2"][:], func=AF.Exp, scale=-0.5), reads=A["t2"].res, writes=A["t2"].res)
            yield
            sch.op("dve", lambda e, j=j: e.scalar_tensor_tensor(out=A["kkn"][:], in0=A["ks"][:], scalar=self.V(l, "k_k", j), in1=A["t2"][:], op0=ALU.mult, op1=ALU.mult),
                   reads=A["ks"].res + A["t2"].res + self.vec[l].res, writes=A["kkn"].res)
            sch.op("dve", lambda e, j=j: e.tensor_scalar(out=A["t2"][:], in0=A["a"][:], scalar1=self.V(l, "k_a", j), scalar2=omka[:, 0:1], op0=ALU.mult, op1=ALU.add),
                   reads=A["a"].res + omka.res + self.vec[l].res, writes=A["t2"].res)
            yield
            sch.op("dve", lambda e: e.tensor_tensor(out=A["kmod"][:], in0=A["ks"][:], in1=A["t2"][:], op=ALU.mult), reads=A["ks"].res + A["t2"].res, writes=A["kmod"].res)
            sch.op("pool", lambda e: e.tensor_tensor(out=A["b"][:], in0=A["kkn"][:], in1=A["a"][:], op=ALU.mult), reads=A["kkn"].res + A["a"].res, writes=A["b"].res)
            yield
            sch.op("pool", lambda e: e.tensor_tensor(out=A["t2"][:], in0=A["rs"][:], in1=A["kmod"][:], op=ALU.mult), reads=A["rs"].res + A["kmod"].res, writes=A["t2"].res)
            sch.op("dve", lambda e, j=j: e.tensor_scalar(out=Bb["rkb"][:], in0=A["t2"][:], scalar1=self.V(l, "r_k", j), scalar2=None, op0=ALU.mult),
                   reads=A["t2"].res + self.vec[l].res, writes=Bb["rkb"].res)
            yield
            b5 = nbp()
            sch.op("pe", lambda e, b5=b5: e.matmul(b5[:, 0:T], lhsT=blk[:], rhs=Bb["rkb"][:], start=True, stop=True), reads=blk.res + Bb["rkb"].res, writes=b5.res)
            sch.op("dve", lambda e, b5=b5, Dd=Dd: e.tensor_tensor(out=gb["bonus"][:], in0=b5[:, 0:T], in1=A["v"][:], op=ALU.mult), reads=b5.res + A["v"].res, writes=gb["bonus"].res)
            yield
            sch.op("dve", lambda e: e.tensor_tensor_scan(out=A["cw"][:], data0=mask64[:], data1=A["sig"][:], initial=0.0, op0=ALU.mult, op1=ALU.add),
                   reads=A["sig"].res + mask64.res, writes=A["cw"].res)
            sch.op("act", lambda e: e.activation(out=e1t[:], in_=A["cw"][:], func=AF.Exp, scale=-C0), reads=A["cw"].res, writes=e1t.res)
            yield
            sch.op("act", lambda e: e.activation(out=A["e2"][:], in_=A["cw"][:], func=AF.Exp, scale=C0), reads=A["cw"].res, writes=A["e2"].res)
            sch.op("pool", lambda e: e.tensor_tensor(out=A["t1"][:], in0=A["cw"][:], in1=A["sig"][:], op=ALU.subtract), reads=A["cw"].res + A["sig"].res, writes=A["t1"].res)
            yield
            sch.op("act", lambda e: e.activation(out=A["t1"][:], in_=A["t1"][:], func=AF.Exp, scale=-C0), reads=A["t1"].res, writes=A["t1"].res)
            cw3 = A["cw"][:].rearrange("p (c i) -> p c i", i=64)
            sch.op("dve", lambda e, cw3=cw3: e.tensor_tensor(out=A["t2"][:].rearrange("p (c i) -> p c i", i=64), in0=cw3[:, :, 63:64].broadcast_to([128, NC, 64]),
                                                            in1=cw3, op=ALU.subtract), reads=A["cw"].res, writes=A["t2"].res)
            yield
            sch.op("act", lambda e: e.activation(out=A["t2"][:], in_=A["t2"][:], func=AF.Exp, scale=-C0), reads=A["t2"].res, writes=A["t2"].res)

            def padw(dst, fn, reads):
                for hh in range(2):
                    ps_ = slice(hh * 64, hh * 64 + 64)
                    o_ap = Pp[dst][ps_, :, hh * 64:hh * 64 + 64]
                    sch.op("dve" if hh == 0 else "pool", lambda e, o_ap=o_ap, ps_=ps_: fn(e, o_ap, ps_), reads=reads, writes=Pp[dst].res)

            def v3(n, ps_):
                return A[n][ps_, :].rearrange("p (c i) -> p c i", i=64)

            padw("r", lambda e, o, ps_: e.tensor_tensor(out=o, in0=v3("rs", ps_), in1=e1t[ps_, :].rearrange("p (c i) -> p c i", i=64), op=ALU.mult), A["rs"].res + e1t.res)
            yield
            for hh in range(2):
                ps_ = slice(hh * 64, hh * 64 + 64)
                sch.op("dve", lambda e, ps_=ps_, hh=hh: e.scalar_tensor_tensor(out=Pp["a"][ps_, :, hh * 64:hh * 64 + 64], in0=v3("kkn", ps_), scalar=-1.0,
                                                                            in1=v3("t1", ps_), op0=ALU.mult, op1=ALU.mult),
                       reads=A["kkn"].res + A["t1"].res, writes=Pp["a"].res)
            padw("b", lambda e, o, ps_: e.tensor_tensor(out=o, in0=v3("b", ps_), in1=v3("e2", ps_), op=ALU.mult), A["b"].res + A["e2"].res)
            padw("k", lambda e, o, ps_: e.tensor_tensor(out=o, in0=v3("kmod", ps_), in1=v3("e2", ps_), op=ALU.mult), A["kmod"].res + A["e2"].res)
            yield
            padw("B", lambda e, o, ps_: e.tensor_tensor(out=o, in0=v3("b", ps_), in1=v3("t2", ps_), op=ALU.mult), A["b"].res + A["t2"].res)
            padw("K", lambda e, o, ps_: e.tensor_tensor(out=o, in0=v3("kmod", ps_), in1=v3("t2", ps_), op=ALU.mult), A["kmod"].res + A["t2"].res)
            yield
            padw("V", lambda e, o, ps_: e.tensor_copy(out=o, in_=v3("v", ps_)), A["v"].res)
            e13 = e1t[:].rearrange("p (c i) -> p c i", i=64)

        items = [(j, tb) for j in range(8) for tb in range(NBK)]
        for g_ in prep_gen(items[0][0], items[0][1], D2[0], P2[0], E1[0], GB3[0]):
            pass
        for it_, (j, tb) in enumerate(items):
            if True:
                t0 = tb * T
                tsl = slice(t0, t0 + T)
                Dd = D2[it_ % 2]
                gens = [quad_gen(qi, Dd, E1[it_ % 2], P2[it_ % 2]) for qi in range(NQ)]
                if pending is not None:
                    gens.append(seq_gen(pending[0], pending[3]))
                if it_ + 1 < len(items):
                    jn, tbn = items[it_ + 1]
                    gens.append(prep_gen(jn, tbn, D2[(it_ + 1) % 2], P2[(it_ + 1) % 2], E1[(it_ + 1) % 2], GB3[(it_ + 1) % 3]))
                while gens:
                    for g_ in list(gens):
                        try:
                            next(g_)
                        except StopIteration:
                            gens.remove(g_)
                if pending is not None:
                    post(pending[0], pending[1], pending[2], pending[4])
                pending = (Dd, j, tsl, tb == 0, GB3[it_ % 3])
        for g_ in seq_gen(pending[0], pending[3]):
            pass
        post(pending[0], pending[1], pending[2], pending[4])
        sch.release(m)

    def load_x(self):
        sch, TG = self.sch, self.TG
        for tg in range(self.NTG):
            ts = slice(tg * TG, (tg + 1) * TG)
            sch.dma("sp", self.xcur[:, :, ts], self.xT.rearrange("(c p) s -> c p s", p=128)[:, :, ts], writes=[self.xcur_res[tg]])

    def finish(self):
        sch = self.sch
        sch.barrier()
        fo = [d for d in sch.dma_last if d is not None]
        sch.emit(fo)
        return self.nc


def make_in_maps(inp, n_cores=8):
    f = lambda a: np.ascontiguousarray(np.asarray(a, np.float32))
    vecs = np.stack([pack_vecs(inp, l) for l in range(DEPTH)])
    shared = {
        "vecs": vecs, "w_in": f(inp["w_in"]), "w_vres": f(inp["w_vres_down"][0]),
        "w_up": f(inp["rwkv_w_up"]), "a_up": f(inp["rwkv_a_up"]), "g_up": f(inp["rwkv_g_up"]),
        "v_up": f(inp["rwkv_v_up"][0]), "w_uq": f(inp["mla_w_uq"]), "w_ukv": f(inp["mla_w_ukv"]),
        "w_branch": f(inp["w_branch"]), "w_out": f(inp["w_out"]), "w_ffn_in": f(inp["w_ffn_in"]),
        "w_ffn_out": f(inp["w_ffn_out"]),
    }
    x = np.asarray(inp["x"], np.float32)
    pos = np.asarray(inp["positions"], np.int32)
    maps = []
    for b in range(n_cores):
        m = dict(shared)
        m["xT"] = np.ascontiguousarray(x[b].T)
        m["pos"] = np.ascontiguousarray(pos[b])
        maps.append(m)
    return maps


def build_program(S=S_LEN, nlayers=DEPTH, debug=()):
    B = Builder(S, nlayers, debug)
    B.load_x()
    B.stage_rope_tables()
    for l in range(nlayers):
        B.stage_inproj(l)
        B.stage_hgrn(l)
        B.stage_rwkv(l)
        B.stage_mla(l)
        B.stage_merge(l)
        B.stage_ffn(l, l == nlayers - 1)
    return B.finish(), B


def kernel(**inputs):
    x = np.asarray(inputs["x"])
    nb, S, _ = x.shape
    nc, _ = build_program(S, DEPTH)
    in_maps = make_in_maps(inputs, nb)
    res = run_bass_kernel_spmd(nc, in_maps, core_ids=list(range(nb)))
    out = np.stack([np.ascontiguousarray(np.asarray(r["outT"]).T) for r in res.results]).astype(np.float32)
    return out
```

```python
import numpy as np
import concourse.bass as bass
import concourse.mybir as mybir
from concourse.bass_utils import run_bass_kernel_spmd

F32 = mybir.dt.float32
BF16 = mybir.dt.bfloat16
I32 = mybir.dt.int32
ALU = mybir.AluOpType
AF = mybir.ActivationFunctionType
AX = mybir.AxisListType

S_LEN = 4096
D = 1024
NT = S_LEN // 128
DEPTH = 2
D_FF = 2816
IN_W = 11200


class Res:
    __slots__ = ("w", "r")

    def __init__(self):
        self.w = None
        self.r = []


class Op:
    __slots__ = ("eng", "fn", "deps", "isdma", "sem", "val", "inc", "waits")

    def __init__(self, eng, fn, isdma):
        self.eng = eng
        self.fn = fn
        self.deps = set()
        self.isdma = isdma
        self.sem = None
        self.val = 0
        self.inc = False
        self.waits = []


class Tile:
    def __init__(self, t, nres):
        self.t = t
        self.res = [Res() for _ in range(nres)]

    def __getitem__(self, k):
        return self.t[k]


class Reg:
    def __init__(self, ap, off, n):
        self.ap, self.off, self.n = ap, off, n
        self.res = [Res()]

    def __getitem__(self, k):
        p, c = k
        a = 0 if c.start is None else c.start
        b = self.n if c.stop is None else c.stop
        return self.ap[p, self.off + a:self.off + b]


N_DMA_SEM = 24
N_HW_SEM = 16
COMPUTE = ("pe", "dve", "act", "pool")


class Sched:
    def __init__(self, nc):
        self.nc = nc
        self.ops = []
        self.base = set()
        self.last = {}
        self.dma_last = [None] * N_DMA_SEM
        self.dma_rr = 0
        self.dma_rr_sw = 0
        self.sb_top = 16640
        self.sb_peak = 0
        self.uid = 0

    def sb(self, shape, dtype, nres=1, name=None):
        self.uid += 1
        esz = {F32: 4, BF16: 2, I32: 4}[dtype]
        nb = int(np.prod(shape[1:])) * esz
        nb = (nb + 63) // 64 * 64
        off = self.sb_top
        self.sb_top += nb
        self.sb_peak = max(self.sb_peak, self.sb_top)
        assert self.sb_top <= 192 * 1024, ("SBUF overflow", self.sb_top)
        t = self.nc.alloc_sbuf_tensor_at(name or f"sb{self.uid}", list(shape), dtype, offset=off)
        return Tile(t, nres)

    def mark(self):
        return self.sb_top

    def release(self, mark):
        self.barrier()
        self.sb_top = mark

    def _rec(self, o, reads, writes):
        deps = set(self.base)
        for r in reads:
            if r.w is not None:
                deps.add(r.w)
        for w in writes:
            if w.w is not None:
                deps.add(w.w)
            deps.update(w.r)
        for r in reads:
            if o.isdma:
                r.r.append(o)
            else:
                r.r = [x for x in r.r if x.isdma or x.eng != o.eng]
                r.r.append(o)
        for w in writes:
            w.w = o
            w.r = []
        deps.discard(o)
        o.deps = deps
        self.ops.append(o)
        if not o.isdma:
            self.last[o.eng] = o
        return o

    def op(self, eng, fn, reads=(), writes=()):
        return self._rec(Op(eng, fn, False), reads, writes)

    def dma(self, q, out, in_, reads=(), writes=()):
        o = Op(q, lambda e: e.dma_start(out=out, in_=in_), True)
        if q == "pool":
            s = N_HW_SEM + self.dma_rr_sw
            self.dma_rr_sw = (self.dma_rr_sw + 1) % (N_DMA_SEM - N_HW_SEM)
        else:
            s = self.dma_rr
            self.dma_rr = (s + 1) % N_HW_SEM
        o.sem = s
        o = self._rec(o, reads, writes)
        if self.dma_last[s] is not None:
            o.deps.add(self.dma_last[s])
        self.dma_last[s] = o
        return o

    def barrier(self):
        b = set(self.last.values())
        b.update(d for d in self.dma_last if d is not None)
        self.base = b

    def emit(self, final_ops):
        nc = self.nc
        for o in self.ops:
            for d in o.deps:
                if d.eng == "pe" and o.eng == "pe" and not d.isdma and not o.isdma:
                    continue
                d.inc = True
        for o in final_ops:
            o.inc = True
        cnt = {e: 0 for e in COMPUTE}
        dcnt = [0] * N_DMA_SEM
        for o in self.ops:
            if o.isdma:
                dcnt[o.sem] += 16
                o.val = dcnt[o.sem]
                o.inc = True
            elif o.inc:
                cnt[o.eng] += 1
                o.val = cnt[o.eng]
        streams = {e: [] for e in ("pe", "dve", "act", "pool", "sp")}
        for o in self.ops:
            streams[o.eng].append(o)
        for e, lst in streams.items():
            have = {}
            for o in lst:
                need = {}
                for d in o.deps:
                    if d.eng == "pe" and o.eng == "pe" and not d.isdma and not o.isdma:
                        continue
                    key = ("d", d.sem) if d.isdma else ("c", d.eng)
                    if have.get(key, 0) < d.val and need.get(key, 0) < d.val:
                        need[key] = d.val
                for k, v in need.items():
                    have[k] = v
                o.waits = list(need.items())
        self.sem_max = dict(cnt)
        import contextlib
        with contextlib.ExitStack() as es:
            csem = {e: es.enter_context(nc.semaphore(f"s_{e}")) for e in COMPUTE}
            dsem = [es.enter_context(nc.semaphore(f"s_d{i}")) for i in range(N_DMA_SEM)]
            fin = es.enter_context(nc.semaphore("s_fin"))
            block = es.enter_context(nc.Block())

            def run(e, name):
                for o in streams[name]:
                    for (kind, k), v in o.waits:
                        e.wait_ge(csem[k] if kind == "c" else dsem[k], v)
                    ins = o.fn(e)
                    if o.isdma:
                        ins.then_inc(dsem[o.sem], 16)
                    elif o.inc:
                        ins.then_inc(csem[o.eng], 1)
                if name == "sp":
                    for o in final_ops:
                        if o.isdma:
                            e.wait_ge(dsem[o.sem], o.val)
                        else:
                            e.wait_ge(csem[o.eng], o.val)

            @block.tensor
            def _(e):
                run(e, "pe")

            @block.vector
            def _(e):
                run(e, "dve")

            @block.scalar
            def _(e):
                run(e, "act")

            @block.gpsimd
            def _(e):
                run(e, "pool")

            @block.sync
            def _(e):
                run(e, "sp")


VEC_SPEC = [
    ("mix_pre_g", 1024), ("mix_post_g", 1024), ("ffn_pre_g", 1024), ("ffn_post_g", 1024),
    ("lb0", 1024), ("lb1", 1024), ("onorm_g", 128), ("mu", 3328), ("vres_mu", 32),
    ("w0", 1024), ("a0", 1024), ("v0", 1024), ("k_k", 1024), ("k_a", 1024), ("r_k", 1024),
    ("lnx_g", 1024), ("lnx_b", 1024), ("q_norm_g", 384), ("kv_norm_g", 256),
]
VOFF = {}
_o = 0
for _n, _f in VEC_SPEC:
    VOFF[_n] = _o
    _o += (_f + 127) // 128
NV = _o


def pack_vecs(inp, l):
    lv = max(l - 1, 0)
    src = {
        "mix_pre_g": inp["mix_pre_g"][l], "mix_post_g": inp["mix_post_g"][l],
        "ffn_pre_g": inp["ffn_pre_g"][l], "ffn_post_g": inp["ffn_post_g"][l],
        "lb0": inp["hgrn_lb_logits"][0], "lb1": inp["hgrn_lb_logits"][1],
        "onorm_g": inp["hgrn_onorm_g"][l], "mu": inp["rwkv_mu"][l], "vres_mu": inp["rwkv_vres_mu"][lv],
        "w0": inp["rwkv_w0"][l], "a0": inp["rwkv_a0"][l], "v0": inp["rwkv_v0"][lv],
        "k_k": inp["rwkv_k_k"][l], "k_a": inp["rwkv_k_a"][l], "r_k": inp["rwkv_r_k"][l],
        "lnx_g": inp["rwkv_lnx_g"][l], "lnx_b": inp["rwkv_lnx_b"][l],
        "q_norm_g": inp["mla_q_norm_g"][l], "kv_norm_g": inp["mla_kv_norm_g"][l],
    }
    out = np.zeros((128, NV), np.float32)
    for n, f in VEC_SPEC:
        v = np.asarray(src[n], np.float32).reshape(-1)
        nch = (f + 127) // 128
        pad = np.zeros(nch * 128, np.float32)
        pad[:f] = v
        out[:, VOFF[n]:VOFF[n] + nch] = pad.reshape(nch, 128).T
    return out


N_ZC = 89


def zcols(fc):
    if fc < 63:
        return fc * 128, 128
    if fc == 63:
        return 8064, 64
    return 8128 + (fc - 64) * 128, 128


class Builder:
    def __init__(self, S_LEN=S_LEN, nlayers=DEPTH, debug=()):
        self.S = S_LEN
        self.TG = min(512, S_LEN)
        self.NTG = S_LEN // self.TG
        self.L = nlayers
        self.debug = set(debug)
        nc = bass.Bass("TRN2", target_bir_lowering=False)
        self.nc = nc
        self.sch = Sched(nc)
        S = S_LEN

        def di(name, shape, dt=F32):
            return nc.dram_tensor(name, list(shape), dt, kind="ExternalInput").ap()

        self.xT = di("xT", [D, S])
        self.pos = di("pos", [S], I32)
        self.vecs = di("vecs", [DEPTH, 128, NV])
        self.w_in = di("w_in", [DEPTH, D, IN_W])
        self.w_vres = di("w_vres", [D, 32])
        self.w_up = di("w_up", [DEPTH, 64, D])
        self.a_up = di("a_up", [DEPTH, 64, D])
        self.g_up = di("g_up", [DEPTH, 128, D])
        self.v_up = di("v_up", [32, D])
        self.w_uq = di("w_uq", [DEPTH, 384, 1536])
        self.w_ukv = di("w_ukv", [DEPTH, 256, 2048])
        self.w_branch = di("w_branch", [DEPTH, 3, D, D])
        self.w_out = di("w_out", [DEPTH, D, D])
        self.w_ffn_in = di("w_ffn_in", [DEPTH, D, 2 * D_FF])
        self.w_ffn_out = di("w_ffn_out", [DEPTH, D_FF, D])
        self.outT = nc.dram_tensor("outT", [D, S], F32, kind="ExternalOutput").ap()
        self.dbg = {}

        def dscr(name, shape, dt=F32):
            kind = "ExternalOutput" if name in self.debug else "Internal"
            t = nc.dram_tensor(name, list(shape), dt, kind=kind).ap()
            return t

        self.zT = dscr("zT", [N_ZC, 128, S])
        self.zT_res = [Res() for _ in range(N_ZC)]
        self.xcur = dscr("xcur", [8, 128, S])
        self.xcur_res = [Res() for _ in range(self.NTG)]
        self.vtok = dscr("vtok", [S, D], BF16)
        self.vtok_res = [Res() for _ in range(S // 128)]
        self.obr = dscr("obr", [3, 8, 128, S], BF16)
        self.obr_res = [[Res() for _ in range(8)] for _ in range(3)]
        self.aT = dscr("aT", [D_FF // 128, 128, S], BF16)
        self.aT_res = [Res() for _ in range(D_FF // 128)]
        self.vfirst = dscr("vfirst", [8, 128, S])
        self.vfirst_res = [Res() for _ in range(8)]
        self.final_ops = []

        sch = self.sch
        self.PS = [Tile(nc.alloc_psum_tensor(f"psb{i}", [128, 512], F32), 1) for i in range(8)]
        self.ones_bf = sch.sb([128, 128], BF16)
        self.ident_bf = sch.sb([128, 128], BF16)
        self.ident_f = sch.sb([128, 128], F32)
        self.eps6 = sch.sb([128, 1], F32)
        self.vec = [sch.sb([128, NV], F32) for _ in range(DEPTH)]
        sch.op("dve", lambda e: e.memset(self.ones_bf[:], 1.0), writes=self.ones_bf.res)
        sch.op("dve", lambda e: e.memset(self.eps6[:], 1e-6), writes=self.eps6.res)
        sch.op("pool", lambda e: e.memset(self.ident_f[:], 1.0), writes=self.ident_f.res)
        sch.op("pool", lambda e: e.affine_select(out=self.ident_f[:], in_=self.ident_f[:], pattern=[[-1, 128]],
                                                 compare_op=ALU.is_equal, fill=0.0, base=0, channel_multiplier=1),
               reads=self.ident_f.res, writes=self.ident_f.res)
        sch.op("dve", lambda e: e.tensor_copy(out=self.ident_bf[:], in_=self.ident_f[:]),
               reads=self.ident_f.res, writes=self.ident_bf.res)
        for l in range(DEPTH):
            sch.dma("sp", self.vec[l][:], self.vecs[l], writes=self.vec[l].res)

    def V(self, l, name, c=0, n=1):
        o = VOFF[name] + c
        return self.vec[l][:, o:o + n]

    def norm_hT(self, l, src, src_res, gname, hT):
        sch, TG = self.sch, self.TG
        m = sch.mark()
        xb = sch.sb([128, 2, 8, TG], F32, nres=2)
        sq = sch.sb([128, 2, 8, TG], BF16, nres=2)
        rs = sch.sb([128, 2, TG], F32, nres=2)
        for tg in range(self.NTG):
            s = tg % 2
            ts = slice(tg * TG, (tg + 1) * TG)
            sch.dma("sp", xb[:, s], src[:, :, ts].rearrange("c p s -> p c s"), reads=[src_res[tg]], writes=[xb.res[s]])
            sch.op("act", lambda e, s=s: e.activation(out=sq[:, s], in_=xb[:, s], func=AF.Square),
                   reads=[xb.res[s]], writes=[sq.res[s]])
            bank = self.PS[tg % 2]
            for kc in range(8):
                sch.op("pe", lambda e, s=s, kc=kc, bank=bank: e.matmul(bank[:, 0:TG], lhsT=self.ones_bf[:], rhs=sq[:, s, kc, :],
                                                                     start=(kc == 0), stop=(kc == 7)),
                       reads=[sq.res[s], self.ones_bf.res[0]], writes=bank.res)
            sch.op("act", lambda e, s=s, bank=bank: e.activation(out=rs[:, s], in_=bank[:, 0:TG], func=AF.Ln,
                                                                scale=1.0 / D, bias=self.eps6[:, 0:1]),
                   reads=bank.res + self.eps6.res, writes=[rs.res[s]])
            sch.op("act", lambda e, s=s: e.activation(out=rs[:, s], in_=rs[:, s], func=AF.Exp, scale=-0.5), reads=[rs.res[s]], writes=[rs.res[s]])
            for kc in range(8):
                sch.op("dve", lambda e, s=s, kc=kc, ts=ts: e.scalar_tensor_tensor(
                    out=hT[:, kc, ts], in0=xb[:, s, kc, :], scalar=self.V(l, gname, kc), in1=rs[:, s],
                    op0=ALU.mult, op1=ALU.mult),
                    reads=[xb.res[s], rs.res[s], self.vec[l].res[0]], writes=[hT.res[tg]])
        sch.release(m)

    def gemm(self, fcs, KC, kp, wsrc, rhs, evac, banks, post_fc=None, ntg=None):
        sch, TG = self.sch, self.TG
        ntg = self.NTG if ntg is None else ntg
        m = sch.mark()
        wb = sch.sb([128, 2, KC, 128], BF16, nres=2)
        it = 0
        for i, fc in enumerate(fcs):
            s = i % 2
            ap, ncols = wsrc(fc)
            sch.dma("pool", wb[0:kp, s, :, 0:ncols], ap, writes=[wb.res[s]])
            for tg in range(ntg):
                bank = banks[it % len(banks)]
                it += 1
                for kc in range(KC):
                    r_ap, r_res = rhs(kc, tg)
                    sch.op("pe", lambda e, s=s, kc=kc, bank=bank, r_ap=r_ap, ncols=ncols: e.matmul(
                        bank[0:ncols, 0:TG], lhsT=wb[0:kp, s, kc, 0:ncols], rhs=r_ap, start=(kc == 0), stop=(kc == KC - 1)),
                        reads=[wb.res[s]] + r_res, writes=bank.res)
                evac(fc, tg, bank, ncols)
            if post_fc is not None:
                post_fc(fc)
        sch.release(m)

    def stage_inproj(self, l):
        sch, S, TG = self.sch, self.S, self.TG
        m = sch.mark()
        hT = sch.sb([128, 8, S], BF16, nres=self.NTG)
        self.norm_hT(l, self.xcur, self.xcur_res, "mix_pre_g", hT)
        zt = sch.sb([128, 2, S], F32, nres=2)
        cnt = [0]
        fcs = [fc for fc in range(88 if l == 0 else 89) if not (16 <= fc < 24)]
        pos = {fc: i for i, fc in enumerate(fcs)}

        def wsrc(fc):
            if fc == 88:
                return self.w_vres.rearrange("(kc p) f -> p kc f", p=128), 32
            c0, ncols = zcols(fc)
            return self.w_in[l][:, c0:c0 + ncols].rearrange("(kc p) f -> p kc f", p=128), ncols

        def rhs(kc, tg):
            return hT[:, kc, tg * TG:(tg + 1) * TG], [hT.res[tg]]

        def evac(fc, tg, bank, ncols):
            s = pos[fc] % 2
            cnt[0] += 1
            if 64 <= fc < 88:
                sch.op("act", lambda e: e.activation(out=zt[0:ncols, s, tg * TG:(tg + 1) * TG], in_=bank[0:ncols, 0:TG], func=AF.Sigmoid),
                       reads=bank.res, writes=[zt.res[s]])
            elif cnt[0] % 2:
                sch.op("act", lambda e: e.copy(out=zt[0:ncols, s, tg * TG:(tg + 1) * TG], in_=bank[0:ncols, 0:TG]),
                       reads=bank.res, writes=[zt.res[s]])
            else:
                sch.op("dve", lambda e: e.tensor_copy(out=zt[0:ncols, s, tg * TG:(tg + 1) * TG], in_=bank[0:ncols, 0:TG]),
                       reads=bank.res, writes=[zt.res[s]])

        def post_fc(fc):
            s = pos[fc] % 2
            ncols = wsrc(fc)[1]
            sch.dma("sp", self.zT[fc, 0:ncols, :], zt[0:ncols, s, :], reads=[zt.res[s]], writes=[self.zT_res[fc]])

        self.gemm(fcs, 8, 128, wsrc, rhs, evac, self.PS[0:4], post_fc)
        m2 = sch.mark()
        wv = sch.sb([128, 8, 1024], BF16)
        vs = sch.sb([128, 2, 1024], BF16, nres=2)
        for q in range(4):
            sch.dma("pool", wv[:, :, q * 256:(q + 1) * 256],
                    self.w_in[l][:, 2048 + q * 256:2048 + (q + 1) * 256].rearrange("(kc p) f -> p kc f", p=128), writes=wv.res)
        for tt in range(S // 128):
            s = tt % 2
            tg = (tt * 128) // TG
            for hf in range(2):
                bank = self.PS[4 + (tt * 2 + hf) % 4]
                for kc in range(8):
                    sch.op("pe", lambda e, kc=kc, bank=bank, tt=tt, hf=hf: e.matmul(
                        bank[:, :], lhsT=hT[:, kc, tt * 128:(tt + 1) * 128], rhs=wv[:, kc, hf * 512:(hf + 1) * 512],
                        start=(kc == 0), stop=(kc == 7)), reads=[hT.res[tg]] + wv.res, writes=bank.res)
                if hf == 0:
                    sch.op("act", lambda e, s=s, bank=bank: e.copy(out=vs[:, s, 0:512], in_=bank[:, :]), reads=bank.res, writes=[vs.res[s]])
                else:
                    sch.op("dve", lambda e, s=s, bank=bank: e.tensor_copy(out=vs[:, s, 512:1024], in_=bank[:, :]), reads=bank.res, writes=[vs.res[s]])
            sch.dma("sp", self.vtok[tt * 128:(tt + 1) * 128, :], vs[:, s, :], reads=[vs.res[s]], writes=[self.vtok_res[tt]])
        sch.release(m2)
        sch.release(m)

    def pn_bufs(self, nb=2):
        sch, TG = self.sch, self.TG
        return dict(sq=sch.sb([128, nb, 8, TG], BF16, nres=nb), rs=sch.sb([128, nb, TG], F32, nres=nb),
                    xb=sch.sb([128, nb, 8, TG], F32, nres=nb), t=sch.sb([128, 2, TG], F32, nres=2), n=[0], nb=nb)

    def postnorm_residual(self, l, gname, pn, mo_ap, mo_res, tg, to_out):
        sch, TG = self.sch, self.TG
        s = pn["n"][0] % pn["nb"]
        pn["n"][0] += 1
        sq, rs, xb, tt = pn["sq"], pn["rs"], pn["xb"], pn["t"]
        ts = slice(tg * TG, (tg + 1) * TG)
        sch.dma("sp", xb[:, s], self.xcur[:, :, ts].rearrange("c p s -> p c s"), reads=[self.xcur_res[tg]], writes=[xb.res[s]])
        sch.op("act", lambda e: e.activation(out=sq[:, s], in_=mo_ap, func=AF.Square), reads=mo_res, writes=[sq.res[s]])
        bank = self.PS[6 + s]
        for kc in range(8):
            sch.op("pe", lambda e, kc=kc: e.matmul(bank[:, 0:TG], lhsT=self.ones_bf[:], rhs=sq[:, s, kc, :],
                                                   start=(kc == 0), stop=(kc == 7)),
                   reads=[sq.res[s], self.ones_bf.res[0]], writes=bank.res)
        sch.op("act", lambda e: e.activation(out=rs[:, s], in_=bank[:, 0:TG], func=AF.Ln, scale=1.0 / D, bias=self.eps6[:, 0:1]),
               reads=bank.res + self.eps6.res, writes=[rs.res[s]])
        sch.op("act", lambda e: e.activation(out=rs[:, s], in_=rs[:, s], func=AF.Exp, scale=-0.5), reads=[rs.res[s]], writes=[rs.res[s]])
        for kc in range(8):
            sch.op("dve", lambda e, kc=kc: e.scalar_tensor_tensor(out=tt[:, kc % 2], in0=mo_ap[:, kc, :], scalar=self.V(l, gname, kc),
                                                                 in1=rs[:, s], op0=ALU.mult, op1=ALU.mult),
                   reads=mo_res + [rs.res[s], self.vec[l].res[0]], writes=[tt.res[kc % 2]])
            sch.op("pool", lambda e, kc=kc: e.tensor_tensor(out=xb[:, s, kc, :], in0=xb[:, s, kc, :], in1=tt[:, kc % 2], op=ALU.add),
                   reads=[tt.res[kc % 2], xb.res[s]], writes=[xb.res[s]])
        if to_out:
            sch.dma("sp", self.outT.rearrange("(c p) s -> p c s", p=128)[:, :, ts], xb[:, s], reads=[xb.res[s]], writes=[self.xcur_res[tg]])
        else:
            sch.dma("sp", self.xcur[:, :, ts].rearrange("c p s -> p c s"), xb[:, s], reads=[xb.res[s]], writes=[self.xcur_res[tg]])

    def stage_merge(self, l):
        sch, S, TG = self.sch, self.S, self.TG
        TB = min(S, 2048)
        nb = TB // TG
        m = sch.mark()
        mg = sch.sb([128, 8, TB], F32, nres=8)
        ob = sch.sb([128, 8, TB], BF16, nres=8)
        gt = sch.sb([128, 4, TG], F32, nres=4)
        tmp = sch.sb([128, 4, TG], F32, nres=4)
        pn = self.pn_bufs(1)
        cnt = [0]
        for tb in range(S // TB):
            t0 = tb * TB
            for b in range(3):
                for kc in range(8):
                    sch.dma("sp", ob[:, kc, :], self.obr[b, kc, :, t0:t0 + TB], reads=[self.obr_res[b][kc]], writes=[ob.res[kc]])

                def wsrc(fc, b=b):
                    return self.w_branch[l, b][:, fc * 128:(fc + 1) * 128].rearrange("(kc p) f -> p kc f", p=128), 128

                def rhs(kc, tg):
                    return ob[:, kc, tg * TG:(tg + 1) * TG], [ob.res[kc]]

                def evac(fc, tg, bank, ncols, b=b, t0=t0):
                    s = cnt[0] % 4
                    cnt[0] += 1
                    zc = 64 + b * 8 + fc
                    tl = slice(tg * TG, (tg + 1) * TG)
                    sch.dma("sp", gt[:, s], self.zT[zc, :, t0 + tg * TG:t0 + (tg + 1) * TG], reads=[self.zT_res[zc]], writes=[gt.res[s]])
                    if b == 0:
                        sch.op("dve", lambda e: e.tensor_tensor(out=mg[:, fc, tl], in0=bank[:, 0:TG], in1=gt[:, s], op=ALU.mult),
                               reads=bank.res + [gt.res[s]], writes=[mg.res[fc]])
                    else:
                        sch.op("dve", lambda e: e.tensor_tensor(out=tmp[:, s], in0=bank[:, 0:TG], in1=gt[:, s], op=ALU.mult),
                               reads=bank.res + [gt.res[s]], writes=[tmp.res[s]])
                        sch.op("pool", lambda e: e.tensor_tensor(out=mg[:, fc, tl], in0=mg[:, fc, tl], in1=tmp[:, s], op=ALU.add),
                               reads=[tmp.res[s], mg.res[fc]], writes=[mg.res[fc]])

                self.gemm(list(range(8)), 8, 128, wsrc, rhs, evac, self.PS[0:4], ntg=nb)
            for kc in range(8):
                eng = "act" if kc % 2 else "pool"
                if eng == "act":
                    sch.op("act", lambda e, kc=kc: e.copy(out=ob[:, kc, :], in_=mg[:, kc, :]), reads=[mg.res[kc]], writes=[ob.res[kc]])
                else:
                    sch.op("pool", lambda e, kc=kc: e.tensor_copy(out=ob[:, kc, :], in_=mg[:, kc, :]), reads=[mg.res[kc]], writes=[ob.res[kc]])

            def wsrc2(fc):
                return self.w_out[l][:, fc * 128:(fc + 1) * 128].rearrange("(kc p) f -> p kc f", p=128), 128

            def rhs2(kc, tg):
                return ob[:, kc, tg * TG:(tg + 1) * TG], [ob.res[kc]]

            def evac2(fc, tg, bank, ncols):
                cnt[0] += 1
                tl = slice(tg * TG, (tg + 1) * TG)
                if cnt[0] % 2:
                    sch.op("act", lambda e: e.copy(out=mg[:, fc, tl], in_=bank[:, 0:TG]), reads=bank.res, writes=[mg.res[fc]])
                else:
                    sch.op("dve", lambda e: e.tensor_copy(out=mg[:, fc, tl], in_=bank[:, 0:TG]), reads=bank.res, writes=[mg.res[fc]])

            self.gemm(list(range(8)), 8, 128, wsrc2, rhs2, evac2, self.PS[0:4], ntg=nb)
            for tg in range(nb):
                self.postnorm_residual(l, "mix_post_g", pn, mg[:, :, tg * TG:(tg + 1) * TG], list(mg.res), tb * nb + tg, False)
        sch.release(m)

    def stage_ffn(self, l, last):
        sch, S, TG = self.sch, self.S, self.TG
        NJ = D_FF // 128
        m = sch.mark()
        hT = sch.sb([128, 8, S], BF16, nres=self.NTG)
        self.norm_hT(l, self.xcur, self.xcur_res, "ffn_pre_g", hT)
        at = sch.sb([128, 2, S], BF16, nres=2)
        sg = sch.sb([128, S], F32, nres=self.NTG)

        def wsrc(fc):
            j, u = fc // 2, fc % 2
            c0 = u * D_FF + j * 128
            return self.w_ffn_in[l][:, c0:c0 + 128].rearrange("(kc p) f -> p kc f", p=128), 128

        def rhs(kc, tg):
            return hT[:, kc, tg * TG:(tg + 1) * TG], [hT.res[tg]]

        def evac(fc, tg, bank, ncols):
            j, u = fc // 2, fc % 2
            ts = slice(tg * TG, (tg + 1) * TG)
            if u == 0:
                sch.op("act", lambda e: e.activation(out=sg[:, ts], in_=bank[:, 0:TG], func=AF.Silu), reads=bank.res, writes=[sg.res[tg]])
            else:
                sch.op("dve", lambda e: e.tensor_tensor(out=at[:, j % 2, ts], in0=bank[:, 0:TG], in1=sg[:, ts], op=ALU.mult),
                       reads=bank.res + [sg.res[tg]], writes=[at.res[j % 2]])

        def post_fc(fc):
            j, u = fc // 2, fc % 2
            if u == 1:
                sch.dma("sp", self.aT[j], at[:, j % 2, :], reads=[at.res[j % 2]], writes=[self.aT_res[j]])

        self.gemm(list(range(2 * NJ)), 8, 128, wsrc, rhs, evac, self.PS[0:4], post_fc)
        sch.release(m)
        m = sch.mark()
        w2 = sch.sb([128, NJ, 1024], BF16, nres=NJ)
        ab = sch.sb([128, 2, NJ, TG], BF16, nres=2)
        mo = sch.sb([128, 2, 8, TG], F32, nres=2)
        pn = self.pn_bufs(1)
        for kc in range(NJ):
            sch.dma("pool", w2[:, kc, :], self.w_ffn_out[l][kc * 128:(kc + 1) * 128, :], writes=[w2.res[kc]])
        it = 0
        for tg in range(self.NTG + 1):
            s = tg % 2
            if tg < self.NTG:
                ts = slice(tg * TG, (tg + 1) * TG)
                sch.dma("sp", ab[:, s], self.aT[:, :, ts].rearrange("j p s -> p j s"), reads=self.aT_res, writes=[ab.res[s]])
            for fc in range(8):
                if tg < self.NTG:
                    bank = self.PS[it % 4]
                    it += 1
                    for kc in range(NJ):
                        sch.op("pe", lambda e, kc=kc, bank=bank, fc=fc, s=s: e.matmul(
                            bank[:, 0:TG], lhsT=w2[:, kc, fc * 128:(fc + 1) * 128], rhs=ab[:, s, kc, :], start=(kc == 0), stop=(kc == NJ - 1)),
                            reads=[w2.res[kc], ab.res[s]], writes=bank.res)
                    if fc % 2:
                        sch.op("act", lambda e, bank=bank, fc=fc, s=s: e.copy(out=mo[:, s, fc, :], in_=bank[:, 0:TG]), reads=bank.res, writes=[mo.res[s]])
                    else:
                        sch.op("dve", lambda e, bank=bank, fc=fc, s=s: e.tensor_copy(out=mo[:, s, fc, :], in_=bank[:, 0:TG]), reads=bank.res, writes=[mo.res[s]])
                if fc == 1 and tg > 0:
                    self.postnorm_residual(l, "ffn_post_g", pn, mo[:, 1 - s], [mo.res[1 - s]], tg - 1, last)
        sch.release(m)

    def hgrn_consts(self):
        sch = self.sch
        c = {}
        c["mbd"] = sch.sb([128, 128], F32)
        c["cm3"] = sch.sb([128, 4, 128], F32)
        c["eps6"] = self.eps6
        mbd, cm3 = c["mbd"], c["cm3"]
        sch.op("pool", lambda e: e.memset(mbd[:], 1.0), writes=mbd.res)
        sch.op("pool", lambda e: e.affine_select(out=mbd[:], in_=mbd[:], pattern=[[1, 128]], compare_op=ALU.is_ge, fill=0.0,
                                                 base=0, channel_multiplier=-1), reads=mbd.res, writes=mbd.res)
        sch.op("pool", lambda e: e.affine_select(out=mbd[:].rearrange("p (c i) -> p c i", i=32), in_=mbd[:].rearrange("p (c i) -> p c i", i=32),
                                                 pattern=[[-32, 4], [0, 32]], compare_op=ALU.is_ge, fill=0.0, base=0, channel_multiplier=1),
               reads=mbd.res, writes=mbd.res)
        sch.op("pool", lambda e: e.memset(cm3[:], 1.0), writes=cm3.res)
        sch.op("pool", lambda e: e.affine_select(out=cm3[:], in_=cm3[:], pattern=[[-32, 4], [0, 128]], compare_op=ALU.is_ge, fill=0.0,
                                                 base=0, channel_multiplier=1), reads=cm3.res, writes=cm3.res)
        sch.op("pool", lambda e: e.affine_select(out=cm3[:], in_=cm3[:], pattern=[[32, 4], [0, 128]], compare_op=ALU.is_ge, fill=0.0,
                                                 base=31, channel_multiplier=-1), reads=cm3.res, writes=cm3.res)
        return c

    def stage_hgrn(self, l):
        sch, S = self.sch, self.S
        T = min(S, 512)
        NB = S // T
        nt = T // 128
        nch = T // 32
        TG = min(T, 512)
        m = sch.mark()
        c = self.hgrn_consts()
        mbd, cm3 = c["mbd"], c["cm3"]
        mask32 = sch.sb([128, T], F32)
        sch.op("pool", lambda e: e.memset(mask32[:], 1.0), writes=mask32.res)
        sch.op("pool", lambda e: e.memset(mask32[:].rearrange("p (c i) -> p c i", i=32)[:, :, 0:1], 0.0), writes=mask32.res)
        lbv = sch.sb([128, 8], F32)
        oml = sch.sb([128, 8], F32)
        if l == 0:
            sch.op("dve", lambda e: e.memset(lbv[:], 0.0), writes=lbv.res)
        else:
            sch.op("dve", lambda e: e.tensor_tensor(out=lbv[:], in0=self.V(l, "lb1", 0, 8), in1=self.V(l, "lb0", 0, 8), op=ALU.subtract),
                   reads=self.vec[l].res, writes=lbv.res)
            sch.op("act", lambda e: e.activation(out=lbv[:], in_=lbv[:], func=AF.Sigmoid), reads=lbv.res, writes=lbv.res)
        sch.op("dve", lambda e: e.tensor_scalar(out=oml[:], in0=lbv[:], scalar1=-1.0, scalar2=1.0, op0=ALU.mult, op1=ALU.add),
               reads=lbv.res, writes=oml.res)
        NS = 4
        bufs = []
        for i in range(NS):
            bufs.append(dict(
                zq=sch.sb([128, T], F32), zf=sch.sb([128, T], F32), zog=sch.sb([128, T], F32), vt=sch.sb([128, nt, 128], BF16),
                lf=sch.sb([128, T], F32), kk=sch.sb([128, T], F32), b=sch.sb([128, T], F32), eb=sch.sb([128, T], F32),
                tmp=sch.sb([128, T], F32), qt32=sch.sb([128, T], F32), qt=sch.sb([128, T], BF16), kt=sch.sb([128, T], BF16), kh=sch.sb([128, T], BF16),
                oT=sch.sb([128, T], F32), ob=sch.sb([128, T], BF16)))
        pT = sch.sb([128, 4, 128], BF16, nres=4)
        k4 = sch.sb([128, 4, 4, 128], BF16, nres=4)
        u4 = sch.sb([128, 4, 4, 128], F32, nres=4)
        st32 = sch.sb([128, 16, 128], F32, nres=16)
        sqs = [sch.sb([128, TG], BF16) for _ in range(2)]
        rss = [sch.sb([128, TG], F32) for _ in range(2)]
        PS = self.PS
        sidxs = [[0], [0]]

        def prep_load(h, tb, B):
            t0 = tb * T
            zq, zf, zog, vt, lf, kk, b, eb, tmp, qt32, qt, kt, kh, oT, ob = (B[k] for k in (
                "zq", "zf", "zog", "vt", "lf", "kk", "b", "eb", "tmp", "qt32", "qt", "kt", "kh", "oT", "ob"))
            sch.dma("sp", zq[:], self.zT[h, :, t0:t0 + T], reads=[self.zT_res[h]], writes=zq.res)
            sch.dma("sp", zf[:], self.zT[8 + h, :, t0:t0 + T], reads=[self.zT_res[8 + h]], writes=zf.res)
            sch.dma("sp", zog[:], self.zT[24 + h, :, t0:t0 + T], reads=[self.zT_res[24 + h]], writes=zog.res)
            sch.dma("sp", vt[:], self.vtok[t0:t0 + T, h * 128:(h + 1) * 128].rearrange("(n p) f -> p n f", p=128),
                    reads=self.vtok_res[t0 // 128:(t0 + T) // 128], writes=vt.res)

        def prep(h, tb, B):
            zq, zf, zog, vt, lf, kk, b, eb, tmp, qt32, qt, kt, kh, oT, ob = (B[k] for k in (
                "zq", "zf", "zog", "vt", "lf", "kk", "b", "eb", "tmp", "qt32", "qt", "kt", "kh", "oT", "ob"))
            sch.op("act", lambda e, zf=zf: e.activation(out=zf[:], in_=zf[:], func=AF.Sigmoid), reads=zf.res, writes=zf.res)
            sch.op("act", lambda e, zog=zog: e.activation(out=zog[:], in_=zog[:], func=AF.Silu), reads=zog.res, writes=zog.res)
            sch.op("dve", lambda e, zf=zf, h=h: e.tensor_scalar(out=zf[:], in0=zf[:], scalar1=oml[:, h:h + 1], scalar2=lbv[:, h:h + 1],
                                                              op0=ALU.mult, op1=ALU.add), reads=zf.res + oml.res + lbv.res, writes=zf.res)
            sch.op("act", lambda e, zf=zf, lf=lf: e.activation(out=lf[:], in_=zf[:], func=AF.Ln), reads=zf.res, writes=lf.res)
            sch.op("pool", lambda e, zf=zf, kk=kk: e.tensor_scalar(out=kk[:], in0=zf[:], scalar1=-1.0, scalar2=1.0, op0=ALU.mult, op1=ALU.add),
                   reads=zf.res, writes=kk.res)
            sch.op("dve", lambda e, b=b, lf=lf: e.tensor_tensor_scan(out=b[:], data0=mask32[:], data1=lf[:], initial=0.0,
                                                                    op0=ALU.mult, op1=ALU.add), reads=lf.res + mask32.res, writes=b.res)
            sch.op("act", lambda e, b=b, eb=eb: e.activation(out=eb[:], in_=b[:], func=AF.Exp), reads=b.res, writes=eb.res)
            sch.op("dve", lambda e, qt32=qt32, zq=zq, eb=eb: e.tensor_tensor(out=qt32[:], in0=zq[:], in1=eb[:], op=ALU.mult),
                   reads=zq.res + eb.res, writes=qt32.res)
            sch.op("pool", lambda e, qt32=qt32, qt=qt: e.tensor_copy(out=qt[:], in_=qt32[:]), reads=qt32.res, writes=qt.res)
            sch.op("act", lambda e, b=b, tmp=tmp: e.activation(out=tmp[:], in_=b[:], func=AF.Exp, scale=-1.0), reads=b.res, writes=tmp.res)
            sch.op("dve", lambda e, kt=kt, kk=kk, tmp=tmp: e.tensor_tensor(out=kt[:], in0=kk[:], in1=tmp[:], op=ALU.mult),
                   reads=kk.res + tmp.res, writes=kt.res)

        def tiles(h, tb, B, X):
            sidx = sidxs[X]
            sq, rs = sqs[X], rss[X]
            t0 = tb * T
            zq, zf, zog, vt, lf, kk, b, eb, tmp, qt32, qt, kt, kh, oT, ob = (B[k] for k in (
                "zq", "zf", "zog", "vt", "lf", "kk", "b", "eb", "tmp", "qt32", "qt", "kt", "kh", "oT", "ob"))
            if tb == 0:
                cur = 8 * X + sidx[0] % 8
                sch.op("pool", lambda e, cur=cur: e.memset(st32[:, cur], 0.0), writes=[st32.res[cur]])
            eb3 = eb[:].rearrange("p (c i) -> p c i", i=32)

            def phA(n):
                tl = slice(n * 128, (n + 1) * 128)
                s2 = 2 * X + n % 2
                pa, pb_, pu, po = PS[4 * X + 0], PS[4 * X + 1], PS[4 * X + 2], PS[4 * X + 3]
                sch.op("pe", lambda e, pa=pa, kt=kt, qt=qt, tl=tl: e.matmul(pa[:, 0:128], lhsT=kt[:, tl], rhs=qt[:, tl], start=True, stop=True),
                       reads=kt.res + qt.res, writes=pa.res)
                sch.op("dve", lambda e, pa=pa, s2=s2: e.tensor_tensor(out=pT[:, s2], in0=pa[:, 0:128], in1=mbd[:], op=ALU.mult),
                       reads=pa.res + mbd.res, writes=[pT.res[s2]])
                pbb = pb_[:].bitcast(BF16)
                sch.op("pe", lambda e, pbb=pbb, kt=kt, tl=tl: e.transpose(out=pbb[:, 0:128], in_=kt[:, tl], identity=self.ident_bf[:]),
                       reads=kt.res + self.ident_bf.res, writes=pb_.res)
                sch.op("dve", lambda e, pbb=pbb, s2=s2: e.tensor_tensor(out=k4[:, s2], in0=pbb[:, 0:128].unsqueeze(1).broadcast_to([128, 4, 128]),
                                                                       in1=cm3[:], op=ALU.mult), reads=pb_.res + cm3.res, writes=[k4.res[s2]])
                for cc in range(4):
                    sch.op("pe", lambda e, pu=pu, s2=s2, cc=cc, vt=vt, n=n: e.matmul(pu[:, cc * 128:(cc + 1) * 128], lhsT=k4[:, s2, cc, :],
                                                                                 rhs=vt[:, n, :], start=True, stop=True),
                           reads=[k4.res[s2]] + vt.res, writes=pu.res)
                for cc in range(4):
                    sch.op("act", lambda e, pu=pu, s2=s2, cc=cc, n=n: e.activation(out=u4[:, s2, cc, :], in_=pu[:, cc * 128:(cc + 1) * 128], func=AF.Copy,
                                                                               scale=eb3[:, n * 4 + cc, 31:32]),
                           reads=pu.res + eb.res, writes=[u4.res[s2]])

            def phB(n):
                tl = slice(n * 128, (n + 1) * 128)
                s2 = 2 * X + n % 2
                po = PS[4 * X + 3]
                sch.op("pe", lambda e, po=po, vt=vt, n=n, s2=s2: e.matmul(po[:, 0:128], lhsT=vt[:, n, :], rhs=pT[:, s2], start=True, stop=False),
                       reads=vt.res + [pT.res[s2]], writes=po.res)
                for cc in range(4):
                    cur = 8 * X + sidx[0] % 8
                    nxt = 8 * X + (sidx[0] + 1) % 8
                    sidx[0] += 1
                    ch = n * 4 + cc
                    cs = slice(n * 128 + cc * 32, n * 128 + (cc + 1) * 32)
                    sch.op("pe", lambda e, po=po, cur=cur, qt32=qt32, cs=cs, cc=cc: e.matmul(po[:, cc * 32:(cc + 1) * 32], lhsT=st32[:, cur], rhs=qt32[:, cs],
                                                                                    start=False, stop=(cc == 3)),
                           reads=[st32.res[cur]] + qt32.res, writes=po.res)
                    sch.op("dve", lambda e, cur=cur, nxt=nxt, eb3=eb3, ch=ch, s2=s2, cc=cc: e.scalar_tensor_tensor(
                        out=st32[:, nxt], in0=st32[:, cur], scalar=eb3[:, ch, 31:32], in1=u4[:, s2, cc, :], op0=ALU.mult, op1=ALU.add),
                        reads=[st32.res[cur], u4.res[s2]] + eb.res, writes=[st32.res[nxt]])
                sch.op("act", lambda e, po=po, oT=oT, tl=tl: e.copy(out=oT[:, tl], in_=po[:, 0:128]), reads=po.res, writes=oT.res)

            phA(0)
            for n in range(nt):
                if n + 1 < nt:
                    phA(n + 1)
                phB(n)
                yield
            for g in range(T // TG):
                gs = slice(g * TG, (g + 1) * TG)
                bank = PS[4 * X + g % 2]
                sch.op("act", lambda e, oT=oT, gs=gs: e.activation(out=sq[:], in_=oT[:, gs], func=AF.Square), reads=oT.res, writes=sq.res)
                sch.op("pe", lambda e, bank=bank: e.matmul(bank[:, 0:TG], lhsT=self.ones_bf[:], rhs=sq[:], start=True, stop=True),
                       reads=sq.res + self.ones_bf.res, writes=bank.res)
                sch.op("act", lambda e, bank=bank: e.activation(out=rs[:], in_=bank[:, 0:TG], func=AF.Ln, scale=1.0 / 128, bias=self.eps6[:, 0:1]),
                       reads=bank.res + self.eps6.res, writes=rs.res)
                sch.op("act", lambda e: e.activation(out=rs[:], in_=rs[:], func=AF.Exp, scale=-0.5), reads=rs.res, writes=rs.res)
                sch.op("dve", lambda e, oT=oT, gs=gs: e.scalar_tensor_tensor(out=oT[:, gs], in0=oT[:, gs], scalar=self.V(l, "onorm_g"), in1=rs[:],
                                                                           op0=ALU.mult, op1=ALU.mult), reads=oT.res + rs.res + self.vec[l].res, writes=oT.res)
                sch.op("dve", lambda e, oT=oT, ob=ob, zog=zog, gs=gs: e.tensor_tensor(out=ob[:, gs], in0=oT[:, gs], in1=zog[:, gs], op=ALU.mult),
                       reads=oT.res + zog.res, writes=ob.res)
            sch.dma("sp", self.obr[0, h, :, t0:t0 + T], ob[:], reads=ob.res, writes=[self.obr_res[0][h]])


        def stream(X):
            items = [(h, tb) for h in range(4 * X, 4 * X + 4) for tb in range(NB)]
            prep_load(items[0][0], items[0][1], bufs[2 * X])
            prep(items[0][0], items[0][1], bufs[2 * X])
            for i, (h, tb) in enumerate(items):
                nxt_ = items[i + 1] if i + 1 < len(items) else None
                if nxt_ is not None:
                    prep_load(nxt_[0], nxt_[1], bufs[2 * X + (i + 1) % 2])
                first = True
                for _ in tiles(h, tb, bufs[2 * X + i % 2], X):
                    if first and nxt_ is not None:
                        prep(nxt_[0], nxt_[1], bufs[2 * X + (i + 1) % 2])
                    first = False
                    yield

        gens = [stream(0), stream(1)]
        for _ in range(2):
            next(gens[0])
        while gens:
            for g_ in list(gens):
                try:
                    next(g_)
                except StopIteration:
                    gens.remove(g_)
        sch.release(m)

    def stage_rope_tables(self):
        sch, S, nc = self.sch, self.S, self.nc
        self.cs = nc.dram_tensor("cs_tab", [2, 64, S], F32, kind=("ExternalOutput" if "cs_tab" in self.debug else "Internal")).ap()
        self.cs_res = [Res(), Res()]
        m = sch.mark()
        pi = sch.sb([64, S], I32)
        ang = sch.sb([64, S], F32)
        u = sch.sb([64, S], F32)
        ki = sch.sb([64, S], I32)
        kf = sch.sb([64, S], F32)
        idx = sch.sb([64, 1], I32)
        invf = sch.sb([64, 1], F32)
        zero = sch.sb([64, 1], F32)
        sch.op("dve", lambda e: e.memset(zero[:], 0.0), writes=zero.res)
        sch.dma("sp", pi[:], self.pos.partition_broadcast(64), writes=pi.res)
        sch.op("pool", lambda e: e.iota(idx[0:32, :], pattern=[[0, 1]], base=0, channel_multiplier=1), writes=idx.res)
        sch.op("pool", lambda e: e.iota(idx[32:64, :], pattern=[[0, 1]], base=0, channel_multiplier=1), writes=idx.res)
        sch.op("dve", lambda e: e.tensor_copy(out=invf[:], in_=idx[:]), reads=idx.res, writes=invf.res)
        sch.op("act", lambda e: e.activation(out=invf[:], in_=invf[:], func=AF.Exp, scale=-(2.0 / 64) * float(np.log(10000.0))),
               reads=invf.res, writes=invf.res)
        sch.op("dve", lambda e: e.tensor_copy(out=ang[:], in_=pi[:]), reads=pi.res, writes=ang.res)
        sch.op("dve", lambda e: e.tensor_scalar(out=ang[:], in0=ang[:], scalar1=invf[:, 0:1], scalar2=1.0 / (2 * np.pi), op0=ALU.mult, op1=ALU.mult),
               reads=ang.res + invf.res, writes=ang.res)
        for which, off in ((0, 0.75), (1, 0.5)):
            sch.op("dve", lambda e, off=off: e.tensor_scalar(out=u[:], in0=ang[:], scalar1=off, scalar2=None, op0=ALU.add), reads=ang.res, writes=u.res)
            sch.op("dve", lambda e: e.tensor_copy(out=ki[:], in_=u[:]), reads=u.res, writes=ki.res)
            sch.op("dve", lambda e: e.tensor_copy(out=kf[:], in_=ki[:]), reads=ki.res, writes=kf.res)
            sch.op("dve", lambda e: e.tensor_tensor(out=u[:], in0=u[:], in1=kf[:], op=ALU.subtract), reads=u.res + kf.res, writes=u.res)
            sch.op("dve", lambda e: e.tensor_scalar(out=kf[:], in0=u[:], scalar1=0.0, scalar2=None, op0=ALU.is_lt), reads=u.res, writes=kf.res)
            sch.op("dve", lambda e: e.tensor_tensor(out=u[:], in0=u[:], in1=kf[:], op=ALU.add), reads=u.res + kf.res, writes=u.res)
            sch.op("dve", lambda e: e.tensor_scalar(out=u[:], in0=u[:], scalar1=-0.5, scalar2=2 * np.pi, op0=ALU.add, op1=ALU.mult), reads=u.res, writes=u.res)
            sch.op("dve", lambda e: e.tensor_scalar(out=u[:], in0=u[:], scalar1=3.14159, scalar2=-3.14159, op0=ALU.min, op1=ALU.max), reads=u.res, writes=u.res)
            sch.op("act", lambda e: e.activation(out=u[:], in_=u[:], func=AF.Sin, bias=zero[:, 0:1]), reads=u.res + zero.res, writes=u.res)
            sch.dma("sp", self.cs[which], u[:], reads=u.res, writes=[self.cs_res[which]])
        sch.release(m)

    def stage_mla(self, l):
        sch, S, TG = self.sch, self.S, self.TG
        NTG = self.NTG
        nt = S // 128
        PS = self.PS
        scale = float((128 + 64) ** -0.5)
        m = sch.mark()
        cqn = sch.sb([128, 3, S], BF16, nres=NTG)
        ckvn = sch.sb([128, 2, S], BF16, nres=NTG)
        cos2 = sch.sb([64, S], F32)
        sin2 = sch.sb([64, S], F32)
        krT = sch.sb([64, S], BF16, nres=NTG)
        tri = sch.sb([128, 128], BF16)
        trif = sch.sb([128, 128], F32)
        rt = sch.sb([64, 64], F32)
        sch.dma("sp", cos2[:], self.cs[0], reads=[self.cs_res[0]], writes=cos2.res)
        sch.dma("sp", sin2[:], self.cs[1], reads=[self.cs_res[1]], writes=sin2.res)
        sch.op("pool", lambda e: e.memset(trif[:], 1.0), writes=trif.res)
        sch.op("pool", lambda e: e.affine_select(out=trif[:], in_=trif[:], pattern=[[1, 128]], compare_op=ALU.is_ge, fill=0.0, base=0,
                                                 channel_multiplier=-1), reads=trif.res, writes=trif.res)
        sch.op("pool", lambda e: e.tensor_copy(out=tri[:], in_=trif[:]), reads=trif.res, writes=tri.res)
        rt2 = sch.sb([64, 64], F32)
        sch.op("pool", lambda e: e.memset(rt[:], -1.0), writes=rt.res)
        sch.op("pool", lambda e: e.affine_select(out=rt[:], in_=rt[:], pattern=[[-1, 64]], compare_op=ALU.is_equal, fill=0.0, base=-32,
                                                 channel_multiplier=1), reads=rt.res, writes=rt.res)
        sch.op("pool", lambda e: e.memset(rt2[:], 1.0), writes=rt2.res)
        sch.op("pool", lambda e: e.affine_select(out=rt2[:], in_=rt2[:], pattern=[[-1, 64]], compare_op=ALU.is_equal, fill=0.0, base=32,
                                                 channel_multiplier=1), reads=rt2.res, writes=rt2.res)
        sch.op("pool", lambda e: e.tensor_tensor(out=rt[:], in0=rt[:], in1=rt2[:], op=ALU.add), reads=rt.res + rt2.res, writes=rt.res)
        m1 = sch.mark()
        zb = sch.sb([128, 2, 3, TG], F32, nres=2)
        sq = sch.sb([128, 2, 3, TG], BF16, nres=2)
        rs = sch.sb([128, 2, TG], F32, nres=2)
        zk = sch.sb([64, 2, TG], F32, nres=2)
        t1 = sch.sb([64, 2, TG], F32, nres=2)
        t2 = sch.sb([64, 2, TG], F32, nres=2)
        it = 0
        for tg in range(NTG):
            ts = slice(tg * TG, (tg + 1) * TG)
            for (c0, ncn, dst, gname, width) in ((58, 3, cqn, "q_norm_g", 384), (61, 2, ckvn, "kv_norm_g", 256)):
                s = it % 2
                it += 1
                sch.dma("sp", zb[:, s, 0:ncn], self.zT[c0:c0 + ncn, :, ts].rearrange("c p s -> p c s"),
                        reads=self.zT_res[c0:c0 + ncn], writes=[zb.res[s]])
                sch.op("act", lambda e, s=s, ncn=ncn: e.activation(out=sq[:, s, 0:ncn], in_=zb[:, s, 0:ncn], func=AF.Square),
                       reads=[zb.res[s]], writes=[sq.res[s]])
                bank = PS[s]
                for kc in range(ncn):
                    sch.op("pe", lambda e, s=s, kc=kc, bank=bank, ncn=ncn: e.matmul(bank[:, 0:TG], lhsT=self.ones_bf[:], rhs=sq[:, s, kc, :],
                                                                                 start=(kc == 0), stop=(kc == ncn - 1)),
                           reads=[sq.res[s]] + self.ones_bf.res, writes=bank.res)
                sch.op("act", lambda e, s=s, bank=bank, width=width: e.activation(out=rs[:, s], in_=bank[:, 0:TG], func=AF.Ln, scale=1.0 / width,
                                                                               bias=self.eps6[:, 0:1]), reads=bank.res + self.eps6.res, writes=[rs.res[s]])
                sch.op("act", lambda e, s=s: e.activation(out=rs[:, s], in_=rs[:, s], func=AF.Exp, scale=-0.5), reads=[rs.res[s]], writes=[rs.res[s]])
                for kc in range(ncn):
                    sch.op("dve", lambda e, s=s, kc=kc, dst=dst, gname=gname, ts=ts: e.scalar_tensor_tensor(
                        out=dst[:, kc, ts], in0=zb[:, s, kc, :], scalar=self.V(l, gname, kc), in1=rs[:, s], op0=ALU.mult, op1=ALU.mult),
                        reads=[zb.res[s], rs.res[s]] + self.vec[l].res, writes=[dst.res[tg]])
            s = tg % 2
            sch.dma("sp", zk[:, s], self.zT[63, 0:64, ts], reads=[self.zT_res[63]], writes=[zk.res[s]])
            bank = PS[2 + s]
            sch.op("pe", lambda e, s=s, bank=bank: e.matmul(bank[0:64, 0:TG], lhsT=rt[:], rhs=zk[:, s], start=True, stop=True),
                   reads=[zk.res[s]] + rt.res, writes=bank.res)
            sch.op("dve", lambda e, s=s, ts=ts: e.tensor_tensor(out=t1[:, s], in0=zk[:, s], in1=cos2[:, ts], op=ALU.mult),
                   reads=[zk.res[s]] + cos2.res, writes=[t1.res[s]])
            sch.op("dve", lambda e, s=s, ts=ts, bank=bank: e.tensor_tensor(out=t2[:, s], in0=bank[0:64, 0:TG], in1=sin2[:, ts], op=ALU.mult),
                   reads=bank.res + sin2.res, writes=[t2.res[s]])
            sch.op("pool", lambda e, s=s, ts=ts: e.tensor_tensor(out=krT[:, ts], in0=t1[:, s], in1=t2[:, s], op=ALU.add),
                   reads=[t1.res[s], t2.res[s]], writes=[krT.res[tg]])
        sch.release(m1)
        wq = sch.sb([128, 3, 192], BF16)
        wqr = sch.sb([128, 3, 64], BF16)
        wkv = sch.sb([128, 2, 256], BF16)
        kT = sch.sb([128, S], BF16, nres=NTG)
        vh = sch.sb([128, nt, 128], BF16, nres=nt)
        qn = sch.sb([128, S], BF16, nres=NTG)
        qr = sch.sb([64, S], BF16, nres=NTG)
        oT = sch.sb([128, S], BF16)
        pT = sch.sb([128, 4, 512], BF16, nres=4)
        rec = sch.sb([128, 2, 512], F32, nres=2)
        t1h = sch.sb([64, 2, TG], F32, nres=2)
        t2h = sch.sb([64, 2, TG], F32, nres=2)
        pit = 0
        for h in range(8):
            sch.dma("pool", wq[:], self.w_uq[l][:, h * 192:(h + 1) * 192].rearrange("(kc p) f -> p kc f", p=128), writes=wq.res)
            sch.dma("pool", wkv[:], self.w_ukv[l][:, h * 256:(h + 1) * 256].rearrange("(kc p) f -> p kc f", p=128), writes=wkv.res)
            sch.op("pool", lambda e: e.tensor_scalar(out=wqr[:, :, 0:32], in0=wq[:, :, 160:192], scalar1=-1.0, scalar2=None, op0=ALU.mult),
                   reads=wq.res, writes=wqr.res)
            sch.op("pool", lambda e: e.tensor_copy(out=wqr[:, :, 32:64], in_=wq[:, :, 128:160]), reads=wq.res, writes=wqr.res)
            for tg in range(NTG):
                ts = slice(tg * TG, (tg + 1) * TG)
                s = tg % 2
                bk, bq, br, br2 = PS[0], PS[1], PS[2], PS[3]
                for kc in range(2):
                    sch.op("pe", lambda e, kc=kc, ts=ts: e.matmul(bk[:, 0:TG], lhsT=wkv[:, kc, 0:128], rhs=ckvn[:, kc, ts], start=(kc == 0), stop=(kc == 1)),
                           reads=wkv.res + [ckvn.res[tg]], writes=bk.res)
                sch.op("act", lambda e, ts=ts: e.copy(out=kT[:, ts], in_=bk[:, 0:TG]), reads=bk.res, writes=[kT.res[tg]])
                for kc in range(3):
                    sch.op("pe", lambda e, kc=kc, ts=ts: e.matmul(bq[:, 0:TG], lhsT=wq[:, kc, 0:128], rhs=cqn[:, kc, ts], start=(kc == 0), stop=(kc == 2)),
                           reads=wq.res + [cqn.res[tg]], writes=bq.res)
                sch.op("dve", lambda e, ts=ts: e.tensor_copy(out=qn[:, ts], in_=bq[:, 0:TG]), reads=bq.res, writes=[qn.res[tg]])
                for kc in range(3):
                    sch.op("pe", lambda e, kc=kc, ts=ts: e.matmul(br[0:64, 0:TG], lhsT=wq[:, kc, 128:192], rhs=cqn[:, kc, ts], start=(kc == 0), stop=(kc == 2)),
                           reads=wq.res + [cqn.res[tg]], writes=br.res)
                for kc in range(3):
                    sch.op("pe", lambda e, kc=kc, ts=ts: e.matmul(br2[0:64, 0:TG], lhsT=wqr[:, kc, :], rhs=cqn[:, kc, ts], start=(kc == 0), stop=(kc == 2)),
                           reads=wqr.res + [cqn.res[tg]], writes=br2.res)
                sch.op("dve", lambda e, s=s, ts=ts: e.tensor_tensor(out=t1h[:, s], in0=br[0:64, 0:TG], in1=cos2[:, ts], op=ALU.mult),
                       reads=br.res + cos2.res, writes=[t1h.res[s]])
                sch.op("dve", lambda e, s=s, ts=ts: e.tensor_tensor(out=t2h[:, s], in0=br2[0:64, 0:TG], in1=sin2[:, ts], op=ALU.mult),
                       reads=br2.res + sin2.res, writes=[t2h.res[s]])
                sch.op("pool", lambda e, s=s, ts=ts: e.tensor_tensor(out=qr[:, ts], in0=t1h[:, s], in1=t2h[:, s], op=ALU.add),
                       reads=[t1h.res[s], t2h.res[s]], writes=[qr.res[tg]])
            for g in range((nt + 3) // 4):
                bank = PS[4 + g % 2]
                na = min(4, nt - g * 4)
                for a in range(na):
                    tt = g * 4 + a
                    tg = (tt * 128) // TG
                    for kc in range(2):
                        sch.op("pe", lambda e, kc=kc, tt=tt, a=a, bank=bank: e.matmul(bank[:, a * 128:(a + 1) * 128], lhsT=ckvn[:, kc, tt * 128:(tt + 1) * 128],
                                                                                   rhs=wkv[:, kc, 128:256], start=(kc == 0), stop=(kc == 1)),
                               reads=wkv.res + [ckvn.res[tg]], writes=bank.res)
                sch.op("act", lambda e, g=g, na=na, bank=bank: e.copy(out=vh[:, g * 4:g * 4 + na, :], in_=bank[:, 0:na * 128].rearrange("p (a v) -> p a v", v=128)),
                       reads=bank.res, writes=vh.res[g * 4:g * 4 + na])
            GQ = min(4, nt)
            for G in range(nt // GQ):
                W = GQ * 128
                q0 = G * W
                qtg = q0 // TG
                po, pd = PS[4 + 2 * (G % 2)], PS[5 + 2 * (G % 2)]
                nkb = G * GQ + GQ
                slots = {}

                def scores(j, G=G, W=W, q0=q0, qtg=qtg):
                    nonlocal pit
                    a = max(0, j - G * GQ)
                    c0 = a * 128
                    s = pit % 4
                    pit += 1
                    slots[j] = (s, c0)
                    bank = PS[s % 4]
                    ks = slice(j * 128, (j + 1) * 128)
                    ktg = (j * 128) // TG
                    qs = slice(q0 + c0, q0 + W)
                    sch.op("pe", lambda e: e.matmul(bank[:, c0:W], lhsT=kT[:, ks], rhs=qn[:, qs], start=True, stop=False),
                           reads=[kT.res[ktg], qn.res[qtg]], writes=bank.res)
                    sch.op("pe", lambda e: e.matmul(bank[:, c0:W], lhsT=krT[:, ks], rhs=qr[:, qs], start=False, stop=True),
                           reads=[krT.res[ktg], qr.res[qtg]], writes=bank.res)
                    sch.op("act", lambda e: e.activation(out=pT[:, s, c0:W], in_=bank[:, c0:W], func=AF.Exp, scale=scale),
                           reads=bank.res, writes=[pT.res[s]])
                    if j >= G * GQ:
                        sch.op("pool", lambda e: e.tensor_tensor(out=pT[:, s, c0:c0 + 128], in0=pT[:, s, c0:c0 + 128], in1=tri[:], op=ALU.mult),
                               reads=[pT.res[s]] + tri.res, writes=[pT.res[s]])

                def pv(j, W=W, po=po, pd=pd, nkb=nkb):
                    s, c0 = slots[j]
                    sch.op("pe", lambda e: e.matmul(po[:, c0:W], lhsT=vh[:, j, :], rhs=pT[:, s, c0:W], start=(j == 0), stop=(j == nkb - 1)),
                           reads=[vh.res[j], pT.res[s]], writes=po.res)
                    sch.op("pe", lambda e: e.matmul(pd[:, c0:W], lhsT=self.ones_bf[:], rhs=pT[:, s, c0:W], start=(j == 0), stop=(j == nkb - 1)),
                           reads=self.ones_bf.res + [pT.res[s]], writes=pd.res)

                scores(0)
                if nkb > 1:
                    scores(1)
                for j in range(nkb):
                    if j + 2 < nkb:
                        scores(j + 2)
                    pv(j)
                s2 = G % 2
                sch.op("dve", lambda e, s2=s2, pd=pd, W=W: e.reciprocal(out=rec[:, s2, 0:W], in_=pd[:, 0:W]), reads=pd.res, writes=[rec.res[s2]])
                sch.op("dve", lambda e, s2=s2, po=po, W=W, q0=q0: e.tensor_tensor(out=oT[:, q0:q0 + W], in0=po[:, 0:W], in1=rec[:, s2, 0:W], op=ALU.mult),
                       reads=po.res + [rec.res[s2]], writes=oT.res)
            sch.dma("sp", self.obr[2, h], oT[:], reads=oT.res, writes=[self.obr_res[2][h]])
        sch.release(m)

    def stage_rwkv(self, l):
        sch, S = self.sch, self.S
        T = min(S, 512)
        NBK = S // T
        NC = T // 64
        NQ = NC // 4
        PS = self.PS
        C0 = 0.6065306597126334
        m = sch.mark()
        blk = sch.sb([128, 128], BF16)
        blkf = sch.sb([128, 128], F32)
        mus = sch.sb([128, 128], F32)
        mui = sch.sb([128, 128], F32)
        mls = sch.sb([128, 128], F32)
        mask64 = sch.sb([128, T], F32)
        tiny = sch.sb([128, 1], F32)
        eps_ln = sch.sb([128, 1], F32)
        sch.op("pool", lambda e: e.memset(tiny[:], 1e-12), writes=tiny.res)
        sch.op("pool", lambda e: e.memset(eps_ln[:], 64e-5), writes=eps_ln.res)
        sch.op("pool", lambda e: e.memset(blkf[:], 0.0), writes=blkf.res)
        sch.op("pool", lambda e: e.memset(blkf[0:64, 0:64], 1.0), writes=blkf.res)
        sch.op("pool", lambda e: e.memset(blkf[64:128, 64:128], 1.0), writes=blkf.res)
        sch.op("pool", lambda e: e.tensor_copy(out=blk[:], in_=blkf[:]), reads=blkf.res, writes=blk.res)
        for (mt, op_, base, cm, pat) in ((mus, ALU.is_gt, 0, -1, 1), (mui, ALU.is_ge, 0, -1, 1), (mls, ALU.is_gt, 0, 1, -1)):
            sch.op("pool", lambda e, mt=mt: e.memset(mt[:], 1.0), writes=mt.res)
            sch.op("pool", lambda e, mt=mt, op_=op_, base=base, cm=cm, pat=pat: e.affine_select(
                out=mt[:], in_=mt[:], pattern=[[pat, 128]], compare_op=op_, fill=0.0, base=base, channel_multiplier=cm),
                reads=mt.res, writes=mt.res)
        sch.op("pool", lambda e: e.memset(mask64[:], 1.0), writes=mask64.res)
        sch.op("pool", lambda e: e.memset(mask64[:].rearrange("p (c i) -> p c i", i=64)[:, :, 0:1], 0.0), writes=mask64.res)

        def shift(dst_ap, zl, mu_ap, np_, eng_r, dt):
            sch.op("dve", lambda e: e.tensor_tensor(out=dt[0:np_, 0:T], in0=zl[0:np_, 0:T], in1=zl[0:np_, 1:T + 1], op=ALU.subtract),
                   reads=zl.res, writes=dt.res)
            sch.op("dve", lambda e: e.scalar_tensor_tensor(out=dst_ap, in0=dt[0:np_, 0:T], scalar=mu_ap, in1=zl[0:np_, 1:T + 1], op0=ALU.mult, op1=ALU.add),
                   reads=zl.res + dt.res + self.vec[l].res, writes=eng_r)

        def load_prev(zl, zc, t0, np_=128, p0=0):
            if t0 == 0:
                sch.op("pool", lambda e: e.memset(zl[p0:p0 + np_, 0:1], 0.0), writes=zl.res)
                sch.dma("sp", zl[p0:p0 + np_, 1:T + 1], self.zT[zc, p0:p0 + np_, 0:T], reads=[self.zT_res[zc]], writes=zl.res)
            else:
                sch.dma("sp", zl[p0:p0 + np_, 0:T + 1], self.zT[zc, p0:p0 + np_, t0 - 1:t0 + T], reads=[self.zT_res[zc]], writes=zl.res)

        tw = sch.sb([128, S], BF16)
        sgg = sch.sb([128, S], BF16)
        zvv = sch.sb([32, S], BF16)
        m0 = sch.mark()
        zl0 = [sch.sb([128, T + 1], F32) for _ in range(2)]
        tmp0 = [sch.sb([128, T], F32) for _ in range(2)]
        dt0 = sch.sb([128, T], F32)
        for tb in range(NBK):
            t0 = tb * T
            zl, tmp = zl0[tb % 2], tmp0[tb % 2]
            load_prev(zl, 56, t0)
            shift(tmp[:, :], zl, self.V(l, "mu", 24), 128, tmp.res, dt0)
            sch.op("act", lambda e, tmp=tmp, t0=t0: e.activation(out=tw[0:64, t0:t0 + T], in_=tmp[0:64, :], func=AF.Tanh), reads=tmp.res, writes=tw.res)
            sch.op("pool", lambda e, tmp=tmp, t0=t0: e.tensor_copy(out=tw[64:128, t0:t0 + T], in_=tmp[64:128, :]), reads=tmp.res, writes=tw.res)
        for tb in range(NBK):
            t0 = tb * T
            zl, tmp = zl0[tb % 2], tmp0[tb % 2]
            load_prev(zl, 57, t0)
            shift(tmp[:, :], zl, self.V(l, "mu", 25), 128, tmp.res, dt0)
            sch.op("act", lambda e, tmp=tmp, t0=t0: e.activation(out=sgg[:, t0:t0 + T], in_=tmp[:, :], func=AF.Sigmoid), reads=tmp.res, writes=sgg.res)
        if l > 0:
            for tb in range(NBK):
                t0 = tb * T
                zl, tmp = zl0[tb % 2], tmp0[tb % 2]
                load_prev(zl, 88, t0, 32)
                shift(tmp[0:32, :], zl, self.V(l, "vres_mu")[0:32, :], 32, tmp.res, dt0)
                sch.op("act", lambda e, tmp=tmp, t0=t0: e.copy(out=zvv[:, t0:t0 + T], in_=tmp[0:32, :]), reads=tmp.res, writes=zvv.res)
        sch.release(m0)
        f32n = ["zr", "zk", "zv"]
        zls = {n: sch.sb([128, T + 1], F32) for n in f32n}
        A = {n: sch.sb([128, T], F32) for n in ("rs", "ks", "v", "sig", "a", "kkn", "kmod", "b", "cw", "t1", "t2", "vf")}
        A["e2"] = A["a"]
        Bb = {n: sch.sb([128, T], BF16) for n in ("kk2", "rkb")}
        P2 = [{n: sch.sb([128, NC, 128], BF16) for n in ("a", "r", "b", "k", "B", "K", "V")} for _ in range(2)]
        for Pq in P2:
            for n in Pq:
                sch.op("pool", lambda e, n=n, Pq=Pq: e.memset(Pq[n][:], 0.0), writes=Pq[n].res)
        E1 = [sch.sb([128, T], F32) for _ in range(2)]
        GB3 = [dict(g=sch.sb([128, T], F32), bonus=sch.sb([128, T], F32)) for _ in range(3)]
        Q = {n: sch.sb([128, 2, 4, 128], BF16, nres=2) for n in ("Ta", "TB", "TK", "TV", "NT", "Nn", "Aak", "Arb", "Ark", "nA0", "nA1", "nB0", "nB1",
                                                               "PT0", "PT1")}
        Q["Ap"], Q["X"], Q["Vp"] = Q["nA1"], Q["nB0"], Q["nB1"]
        Sbd = sch.sb([128, 2, 128], BF16, nres=2)
        wup = sch.sb([128, 128], BF16)
        gup = sch.sb([128, 128], BF16)
        vup = sch.sb([32, 128], BF16)
        omka = sch.sb([128, 1], F32)
        bankc = [0, 0]

        def nb():
            bankc[0] += 1
            return PS[bankc[0] % 6]

        def nbp():
            bankc[1] += 1
            return PS[6 + bankc[1] % 2]

        evc = [0]

        def evac_copy(out_ap, in_ap, reads, writes):
            sch.op("act", lambda e: e.copy(out=out_ap, in_=in_ap), reads=reads, writes=writes)

        def v4(bank, bf=False):
            if bf:
                return bank[:].bitcast(BF16)[:, 0:512].rearrange("p (a q) -> p a q", q=128)
            return bank[:, :].rearrange("p (a q) -> p a q", q=128)

        def bc4(t):
            return t[:].unsqueeze(1).broadcast_to([128, 4, 128])

        D2 = [dict(Yp=sch.sb([128, NC, 128], F32), Gs=sch.sb([128, NC, 128], F32), MT=sch.sb([128, NC, 128], BF16), RT=sch.sb([128, NC, 128], BF16),
                   yT=sch.sb([128, T], F32), ob=sch.sb([128, T], BF16)) for _ in range(2)]
        PT_ = dict(t1=sch.sb([128, T], F32), t2=sch.sb([128, T], F32), ybf=sch.sb([128, T], BF16), ysq=sch.sb([128, T], BF16))
        sidx = [0]
        pending = None
        itc = [0]

        def quad_gen(qi, Dd, e1tile, Pp):
            e13 = e1tile[:].rearrange("p (c i) -> p c i", i=64)
            s = qi % 2
            cs = slice(qi * 4, qi * 4 + 4)

            def mm4(bank, L, Rr, lres, rres, start=True, stop=True, bf=False, tr=False):
                for a in range(4):
                    la = L(a)
                    if tr:
                        ov = bank[:].bitcast(BF16)[:, a * 128:(a + 1) * 128]
                        sch.op("pe", lambda e, ov=ov, la=la: e.transpose(out=ov, in_=la, identity=self.ident_bf[:]), reads=lres + self.ident_bf.res, writes=bank.res)
                    else:
                        ra = Rr(a)
                        sch.op("pe", lambda e, a=a, la=la, ra=ra: e.matmul(bank[:, a * 128:(a + 1) * 128], lhsT=la, rhs=ra, start=start, stop=stop),
                               reads=lres + rres, writes=bank.res)

            def pq(n):
                return lambda a, n=n, qi=qi: Pp[n][:, qi * 4 + a, :]

            def qq(n):
                return lambda a, n=n, s=s: Q[n][:, s, a, :]

            for src, dst in (("a", "Ta"), ("B", "TB"), ("K", "TK"), ("V", "TV")):
                bank = nb()
                mm4(bank, pq(src), None, Pp[src].res, [], tr=True)
                evac_copy(Q[dst][:, s], v4(bank, True), bank.res, [Q[dst].res[s]])
                yield
            for Ln, Rn, mk, dst in (("b", "a", mus, "NT"), ("a", "b", mls, "Nn"), ("k", "a", mus, "Aak"), ("b", "r", mui, "Arb"), ("k", "r", mui, "Ark")):
                bank = nb()
                mm4(bank, pq(Ln), pq(Rn), Pp[Ln].res, Pp[Rn].res)
                sch.op("dve", lambda e, s=s, cs=cs, qi=qi, bank=bank, dst=dst, mk=mk: e.tensor_tensor(out=Q[dst][:, s], in0=v4(bank), in1=bc4(mk), op=ALU.mult),
                       reads=bank.res + mk.res, writes=[Q[dst].res[s]])
            sch.op("pool", lambda e, s=s, cs=cs, qi=qi: e.tensor_tensor(out=Q["PT0"][:, s], in0=Q["NT"][:, s], in1=bc4(self.ident_bf), op=ALU.add),
                   reads=[Q["NT"].res[s]] + self.ident_bf.res, writes=[Q["PT0"].res[s]])
            curT, cur = "NT", "Nn"
            for lv in range(5):
                nA, nB = f"nA{lv % 2}", f"nB{lv % 2}"
                pin, pout = f"PT{lv % 2}", f"PT{(lv + 1) % 2}"
                bank = nb()
                mm4(bank, qq(curT), qq(cur), [Q[curT].res[s]], [Q[cur].res[s]])
                evac_copy(Q[nA][:, s], v4(bank), bank.res, [Q[nA].res[s]])
                yield
                if lv < 4:
                    bank = nb()
                    mm4(bank, qq(cur), qq(curT), [Q[cur].res[s]], [Q[curT].res[s]])
                    evac_copy(Q[nB][:, s], v4(bank), bank.res, [Q[nB].res[s]])
                    yield
                bank = nb()
                for a in range(4):
                    sch.op("pe", lambda e, a=a, bank=bank, pin=pin: e.matmul(bank[:, a * 128:(a + 1) * 128], lhsT=self.ident_bf[:], rhs=Q[pin][:, s, a, :], start=True, stop=False),
                           reads=self.ident_bf.res + [Q[pin].res[s]], writes=bank.res)
                    sch.op("pe", lambda e, a=a, bank=bank, pin=pin, nA=nA: e.matmul(bank[:, a * 128:(a + 1) * 128], lhsT=Q[nA][:, s, a, :], rhs=Q[pin][:, s, a, :], start=False, stop=True),
                           reads=[Q[nA].res[s], Q[pin].res[s]], writes=bank.res)
                evac_copy(Q[pout][:, s], v4(bank), bank.res, [Q[pout].res[s]])
                yield
                curT, cur = nB, nA
            TT = "PT1"
            bank = nb()
            mm4(bank, qq(TT), qq("Ta"), [Q[TT].res[s]], [Q["Ta"].res[s]])
            evac_copy(Q["Ap"][:, s], v4(bank), bank.res, [Q["Ap"].res[s]])
            yield
            bank = nb()
            mm4(bank, qq("Aak"), qq("TV"), [Q["Aak"].res[s]], [Q["TV"].res[s]])
            evac_copy(Q["X"][:, s], v4(bank), bank.res, [Q["X"].res[s]])
            yield
            bank = nb()
            mm4(bank, qq(TT), qq("X"), [Q[TT].res[s]], [Q["X"].res[s]])
            evac_copy(Q["Vp"][:, s], v4(bank), bank.res, [Q["Vp"].res[s]])
            yield
            bank = nb()
            for a in range(4):
                sch.op("pe", lambda e, s=s, cs=cs, qi=qi, a=a, bank=bank: e.matmul(bank[:, a * 128:(a + 1) * 128], lhsT=Q["Vp"][:, s, a, :], rhs=Q["Arb"][:, s, a, :], start=True, stop=False),
                       reads=[Q["Vp"].res[s], Q["Arb"].res[s]], writes=bank.res)
                sch.op("pe", lambda e, s=s, cs=cs, qi=qi, a=a, bank=bank: e.matmul(bank[:, a * 128:(a + 1) * 128], lhsT=Q["TV"][:, s, a, :], rhs=Q["Ark"][:, s, a, :], start=False, stop=True),
                       reads=[Q["TV"].res[s], Q["Ark"].res[s]], writes=bank.res)
            evac_copy(Dd["Yp"][:, cs, :], v4(bank), bank.res, Dd["Yp"].res)
            yield
            bank = nb()
            for a in range(4):
                sch.op("pe", lambda e, s=s, cs=cs, qi=qi, a=a, bank=bank: e.matmul(bank[:, a * 128:(a + 1) * 128], lhsT=Q["TB"][:, s, a, :], rhs=Q["Vp"][:, s, a, :], start=True, stop=False),
                       reads=[Q["TB"].res[s], Q["Vp"].res[s]], writes=bank.res)
                sch.op("pe", lambda e, s=s, cs=cs, qi=qi, a=a, bank=bank: e.matmul(bank[:, a * 128:(a + 1) * 128], lhsT=Q["TK"][:, s, a, :], rhs=Q["TV"][:, s, a, :], start=False, stop=True),
                       reads=[Q["TK"].res[s], Q["TV"].res[s]], writes=bank.res)
            evac_copy(Dd["Gs"][:, cs, :], v4(bank), bank.res, Dd["Gs"].res)
            yield
            bank = nb()
            mm4(bank, qq("Ap"), qq("TB"), [Q["Ap"].res[s]], [Q["TB"].res[s]])
            for a in range(4):
                c = qi * 4 + a
                sch.op("dve", lambda e, s=s, cs=cs, qi=qi, a=a, c=c, bank=bank, e13=e13: e.scalar_tensor_tensor(out=Dd["MT"][:, c, :], in0=self.ident_f[:], scalar=e13[:, c, 63:64],
                                                                                           in1=bank[:, a * 128:(a + 1) * 128], op0=ALU.mult, op1=ALU.add),
                       reads=bank.res + self.ident_f.res + e1tile.res, writes=Dd["MT"].res)
            bank = nb()
            for a in range(4):
                sch.op("pe", lambda e, a=a, bank=bank: e.matmul(bank[:, a * 128:(a + 1) * 128], lhsT=self.ident_bf[:], rhs=Pp["r"][:, qi * 4 + a, :], start=True, stop=False),
                       reads=self.ident_bf.res + Pp["r"].res, writes=bank.res)
                sch.op("pe", lambda e, a=a, bank=bank: e.matmul(bank[:, a * 128:(a + 1) * 128], lhsT=Q["Ap"][:, s, a, :], rhs=Q["Arb"][:, s, a, :], start=False, stop=True),
                       reads=[Q["Ap"].res[s], Q["Arb"].res[s]], writes=bank.res)
            evac_copy(Dd["RT"][:, cs, :], v4(bank), bank.res, Dd["RT"].res)

        def seq_gen(Dd, reset):
            if reset:
                cur0 = sidx[0] % 2
                sch.op("pool", lambda e: e.memset(Sbd[:, cur0], 0.0), writes=[Sbd.res[cur0]])
            for c in range(NC):
                cur = sidx[0] % 2
                nxt = (sidx[0] + 1) % 2
                sidx[0] += 1
                by, bs = nb(), nb()
                sch.op("pe", lambda e, by=by, cur=cur, c=c: e.matmul(by[:, 0:128], lhsT=Sbd[:, cur], rhs=Dd["RT"][:, c, :], start=True, stop=True),
                       reads=[Sbd.res[cur]] + Dd["RT"].res, writes=by.res)
                sch.op("pe", lambda e, bs=bs, cur=cur, c=c: e.matmul(bs[:, 0:128], lhsT=Dd["MT"][:, c, :], rhs=Sbd[:, cur], start=True, stop=True),
                       reads=[Sbd.res[cur]] + Dd["MT"].res, writes=bs.res)
                sch.op("dve", lambda e, bs=bs, nxt=nxt, c=c: e.tensor_tensor(out=Sbd[:, nxt], in0=bs[:, 0:128], in1=Dd["Gs"][:, c, :], op=ALU.add),
                       reads=bs.res + Dd["Gs"].res, writes=[Sbd.res[nxt]])
                for hh in range(2):
                    ps_ = slice(hh * 64, hh * 64 + 64)
                    sch.op("dve", lambda e, by=by, ps_=ps_, c=c, hh=hh: e.tensor_tensor(out=Dd["yT"][ps_, c * 64:(c + 1) * 64], in0=by[ps_, hh * 64:hh * 64 + 64],
                                                                                  in1=Dd["Yp"][ps_, c, hh * 64:hh * 64 + 64], op=ALU.add),
                           reads=by.res + Dd["Yp"].res, writes=Dd["yT"].res)
                yield

        def post_gen(Dd, j, tsl, gb):
            sch.op("act", lambda e: e.copy(out=PT_["ybf"][:], in_=Dd["yT"][:]), reads=Dd["yT"].res, writes=PT_["ybf"].res)
            sch.op("act", lambda e: e.activation(out=PT_["ysq"][:], in_=Dd["yT"][:], func=AF.Square), reads=Dd["yT"].res, writes=PT_["ysq"].res)
            yield
            bm, bq = nb(), nb()
            sch.op("pe", lambda e: e.matmul(bm[:, 0:T], lhsT=blk[:], rhs=PT_["ybf"][:], start=True, stop=True), reads=blk.res + PT_["ybf"].res, writes=bm.res)
            sch.op("pe", lambda e: e.matmul(bq[:, 0:T], lhsT=blk[:], rhs=PT_["ysq"][:], start=True, stop=True), reads=blk.res + PT_["ysq"].res, writes=bq.res)
            sch.op("act", lambda e: e.activation(out=PT_["t1"][:], in_=bm[:, 0:T], func=AF.Square, scale=1.0 / 64), reads=bm.res, writes=PT_["t1"].res)
            sch.op("dve", lambda e: e.scalar_tensor_tensor(out=PT_["t1"][:], in0=bq[:, 0:T], scalar=1.0 / 64, in1=PT_["t1"][:], op0=ALU.mult, op1=ALU.subtract),
                   reads=bq.res + PT_["t1"].res, writes=PT_["t1"].res)
            sch.op("dve", lambda e: e.scalar_tensor_tensor(out=PT_["t2"][:], in0=bm[:, 0:T], scalar=-1.0 / 64, in1=Dd["yT"][:], op0=ALU.mult, op1=ALU.add),
                   reads=bm.res + Dd["yT"].res, writes=PT_["t2"].res)
            yield
            sch.op("act", lambda e: e.activation(out=PT_["t1"][:], in_=PT_["t1"][:], func=AF.Ln, bias=eps_ln[:, 0:1]), reads=PT_["t1"].res + eps_ln.res, writes=PT_["t1"].res)
            sch.op("act", lambda e: e.activation(out=PT_["t1"][:], in_=PT_["t1"][:], func=AF.Exp, scale=-0.5), reads=PT_["t1"].res, writes=PT_["t1"].res)
            yield
            sch.op("dve", lambda e: e.scalar_tensor_tensor(out=PT_["t2"][:], in0=PT_["t2"][:], scalar=self.V(l, "lnx_g", j), in1=PT_["t1"][:], op0=ALU.mult, op1=ALU.mult),
                   reads=PT_["t2"].res + PT_["t1"].res + self.vec[l].res, writes=PT_["t2"].res)
            sch.op("dve", lambda e: e.scalar_tensor_tensor(out=PT_["t2"][:], in0=PT_["t2"][:], scalar=self.V(l, "lnx_b", j), in1=gb["bonus"][:], op0=ALU.add, op1=ALU.add),
                   reads=PT_["t2"].res + gb["bonus"].res + self.vec[l].res, writes=PT_["t2"].res)
            yield
            sch.op("dve", lambda e: e.tensor_tensor(out=Dd["ob"][:], in0=PT_["t2"][:], in1=gb["g"][:], op=ALU.mult), reads=PT_["t2"].res + gb["g"].res, writes=Dd["ob"].res)
            sch.dma("sp", self.obr[1, j, :, tsl], Dd["ob"][:], reads=Dd["ob"].res, writes=[self.obr_res[1][j]])

        def seq_post(pend):
            yield from seq_gen(pend[0], pend[3])
            yield from post_gen(pend[0], pend[1], pend[2], pend[4])

        def prep_gen(j, tb, Dd, Pp, e1t, gb):
            t0 = tb * T
            tsl = slice(t0, t0 + T)
            if tb == 0:
                fs = slice(j * 128, (j + 1) * 128)
                sch.dma("pool", wup[0:64, :], self.w_up[l][:, fs], writes=wup.res)
                sch.dma("pool", wup[64:128, :], self.a_up[l][:, fs], writes=wup.res)
                sch.dma("pool", gup[:], self.g_up[l][:, fs], writes=gup.res)
                if l > 0:
                    sch.dma("pool", vup[:], self.v_up[:, fs], writes=vup.res)
                sch.op("dve", lambda e: e.tensor_scalar(out=omka[:], in0=self.V(l, "k_a", j), scalar1=-1.0, scalar2=1.0, op0=ALU.mult, op1=ALU.add),
                       reads=self.vec[l].res, writes=omka.res)
            for n, zc, dst in (("zr", 32 + j, "rs"), ("zk", 40 + j, "ks"), ("zv", 48 + j, "v")):
                load_prev(zls[n], zc, t0)
                shift(A[dst][:, :], zls[n], self.V(l, "mu", zc - 32), 128, A[dst].res, A["t1"])
            b0, b1, b2, b3 = nbp(), nbp(), nbp(), nbp()
            sch.op("pe", lambda e, b0=b0, tsl=tsl: e.matmul(b0[:, 0:T], lhsT=wup[0:64, :], rhs=tw[0:64, tsl], start=True, stop=True),
                   reads=wup.res + tw.res, writes=b0.res)
            sch.op("act", lambda e, b0=b0, j=j: e.activation(out=A["sig"][:], in_=b0[:, 0:T], func=AF.Sigmoid, bias=self.V(l, "w0", j)),
                   reads=b0.res + self.vec[l].res, writes=A["sig"].res)
            yield
            sch.op("pe", lambda e, b1=b1, tsl=tsl: e.matmul(b1[:, 0:T], lhsT=wup[64:128, :], rhs=tw[64:128, tsl], start=True, stop=True),
                   reads=wup.res + tw.res, writes=b1.res)
            sch.op("act", lambda e, b1=b1, j=j: e.activation(out=A["a"][:], in_=b1[:, 0:T], func=AF.Sigmoid, bias=self.V(l, "a0", j)),
                   reads=b1.res + self.vec[l].res, writes=A["a"].res)
            yield
            sch.op("pe", lambda e, b2=b2, tsl=tsl: e.matmul(b2[:, 0:T], lhsT=gup[:], rhs=sgg[:, tsl], start=True, stop=True),
                   reads=gup.res + sgg.res, writes=b2.res)
            sch.op("act", lambda e, b2=b2, Dd=Dd: e.copy(out=gb["g"][:], in_=b2[:, 0:T]), reads=b2.res, writes=gb["g"].res)
            yield
            if l == 0:
                sch.dma("sp", self.vfirst[j, :, tsl], A["v"][:], reads=A["v"].res, writes=[self.vfirst_res[j]])
            else:
                sch.dma("sp", A["vf"][:], self.vfirst[j, :, tsl], reads=[self.vfirst_res[j]], writes=A["vf"].res)
                sch.op("pe", lambda e, b3=b3, tsl=tsl: e.matmul(b3[:, 0:T], lhsT=vup[0:32, :], rhs=zvv[0:32, tsl], start=True, stop=True),
                       reads=vup.res + zvv.res, writes=b3.res)
                sch.op("act", lambda e, b3=b3, j=j: e.activation(out=A["t1"][:], in_=b3[:, 0:T], func=AF.Sigmoid, bias=self.V(l, "v0", j)),
                       reads=b3.res + self.vec[l].res, writes=A["t1"].res)
                sch.op("pool", lambda e: e.tensor_tensor(out=A["vf"][:], in0=A["vf"][:], in1=A["v"][:], op=ALU.subtract),
                       reads=A["vf"].res + A["v"].res, writes=A["vf"].res)
                sch.op("dve", lambda e: e.tensor_tensor(out=A["vf"][:], in0=A["vf"][:], in1=A["t1"][:], op=ALU.mult),
                       reads=A["vf"].res + A["t1"].res, writes=A["vf"].res)
                sch.op("pool", lambda e: e.tensor_tensor(out=A["v"][:], in0=A["v"][:], in1=A["vf"][:], op=ALU.add),
                       reads=A["vf"].res + A["v"].res, writes=A["v"].res)
            sch.op("act", lambda e, j=j: e.activation(out=Bb["kk2"][:], in_=A["ks"][:], func=AF.Square, scale=self.V(l, "k_k", j)),
                   reads=A["ks"].res + self.vec[l].res, writes=Bb["kk2"].res)
            b4 = nbp()
            sch.op("pe", lambda e, b4=b4: e.matmul(b4[:, 0:T], lhsT=blk[:], rhs=Bb["kk2"][:], start=True, stop=True), reads=blk.res + Bb["kk2"].res, writes=b4.res)
            yield
            sch.op("act", lambda e, b4=b4: e.activation(out=A["t2"][:], in_=b4[:, 0:T], func=AF.Ln, bias=tiny[:, 0:1]), reads=b4.res + tiny.res, writes=A["t2"].res)
            sch.op("act", lambda e: e.activation(out=A["t2"][:], in_=A["t2"][:], func=AF.Exp, scale=-0.5), reads=A["t2"].res, writes=A["t2"].res)
            yield
            sch.op("dve", lambda e, j=j: e.scalar_tensor_tensor(out=A["kkn"][:], in0=A["ks"][:], scalar=self.V(l, "k_k", j), in1=A["t2"][:], op0=ALU.mult, op1=ALU.mult),
                   reads=A["ks"].res + A["t2"].res + self.vec[l].res, writes=A["kkn"].res)
            sch.op("dve", lambda e, j=j: e.tensor_scalar(out=A["t2"][:], in0=A["a"][:], scalar1=self.V(l, "k_a", j), scalar2=omka[:, 0:1], op0=ALU.mult, op1=ALU.add),
                   reads=A["a"].res + omka.res + self.vec[l].res, writes=A["t2"].res)
            yield
            sch.op("dve", lambda e: e.tensor_tensor(out=A["kmod"][:], in0=A["ks"][:], in1=A["t2"][:], op=ALU.mult), reads=A["ks"].res + A["t2"].res, writes=A["kmod"].res)
            sch.op("pool", lambda e: e.tensor_tensor(out=A["b"][:], in0=A["kkn"][:], in1=A["a"][:], op=ALU.mult), reads=A["kkn"].res + A["a"].res, writes=A["b"].res)
            yield
            sch.op("pool", lambda e: e.tensor_tensor(out=A["t2"][:], in0=A["rs"][:], in1=A["kmod"][:], op=ALU.mult), reads=A["rs"].res + A["kmod"].res, writes=A["t2"].res)
            sch.op("dve", lambda e, j=j: e.tensor_scalar(out=Bb["rkb"][:], in0=A["t2"][:], scalar1=self.V(l, "r_k", j), scalar2=None, op0=ALU.mult),
                   reads=A["t2"].res + self.vec[l].res, writes=Bb["rkb"].res)
            yield
            b5 = nbp()
            sch.op("pe", lambda e, b5=b5: e.matmul(b5[:, 0:T], lhsT=blk[:], rhs=Bb["rkb"][:], start=True, stop=True), reads=blk.res + Bb["rkb"].res, writes=b5.res)
            sch.op("dve", lambda e, b5=b5, Dd=Dd: e.tensor_tensor(out=gb["bonus"][:], in0=b5[:, 0:T], in1=A["v"][:], op=ALU.mult), reads=b5.res + A["v"].res, writes=gb["bonus"].res)
            yield
            sch.op("dve", lambda e: e.tensor_tensor_scan(out=A["cw"][:], data0=mask64[:], data1=A["sig"][:], initial=0.0, op0=ALU.mult, op1=ALU.add),
                   reads=A["sig"].res + mask64.res, writes=A["cw"].res)
            sch.op("act", lambda e: e.activation(out=e1t[:], in_=A["cw"][:], func=AF.Exp, scale=-C0), reads=A["cw"].res, writes=e1t.res)
            yield
            sch.op("act", lambda e: e.activation(out=A["e2"][:], in_=A["cw"][:], func=AF.Exp, scale=C0), reads=A["cw"].res, writes=A["e2"].res)
            sch.op("pool", lambda e: e.tensor_tensor(out=A["t1"][:], in0=A["cw"][:], in1=A["sig"][:], op=ALU.subtract), reads=A["cw"].res + A["sig"].res, writes=A["t1"].res)
            yield
            sch.op("act", lambda e: e.activation(out=A["t1"][:], in_=A["t1"][:], func=AF.Exp, scale=-C0), reads=A["t1"].res, writes=A["t1"].res)
            cw3 = A["cw"][:].rearrange("p (c i) -> p c i", i=64)
            sch.op("dve", lambda e, cw3=cw3: e.tensor_tensor(out=A["t2"][:].rearrange("p (c i) -> p c i", i=64), in0=cw3[:, :, 63:64].broadcast_to([128, NC, 64]),
                                                            in1=cw3, op=ALU.subtract), reads=A["cw"].res, writes=A["t2"].res)
            yield
            sch.op("act", lambda e: e.activation(out=A["t2"][:], in_=A["t2"][:], func=AF.Exp, scale=-C0), reads=A["t2"].res, writes=A["t2"].res)

            def padw(dst, fn, reads):
                for hh in range(2):
                    ps_ = slice(hh * 64, hh * 64 + 64)
                    o_ap = Pp[dst][ps_, :, hh * 64:hh * 64 + 64]
                    sch.op("dve" if hh == 0 else "pool", lambda e, o_ap=o_ap, ps_=ps_: fn(e, o_ap, ps_), reads=reads, writes=Pp[dst].res)

            def v3(n, ps_):
                return A[n][ps_, :].rearrange("p (c i) -> p c i", i=64)

            padw("r", lambda e, o, ps_: e.tensor_tensor(out=o, in0=v3("rs", ps_), in1=e1t[ps_, :].rearrange("p (c i) -> p c i", i=64), op=ALU.mult), A["rs"].res + e1t.res)
            yield
            for hh in range(2):
                ps_ = slice(hh * 64, hh * 64 + 64)
                sch.op("dve", lambda e, ps_=ps_, hh=hh: e.scalar_tensor_tensor(out=Pp["a"][ps_, :, hh * 64:hh * 64 + 64], in0=v3("kkn", ps_), scalar=-1.0,
                                                                            in1=v3("t1", ps_), op0=ALU.mult, op1=ALU.mult),
                       reads=A["kkn"].res + A["t1"].res, writes=Pp["a"].res)
            padw("b", lambda e, o, ps_: e.tensor_tensor(out=o, in0=v3("b", ps_), in1=v3("e2", ps_), op=ALU.mult), A["b"].res + A["e2"].res)
            padw("k", lambda e, o, ps_: e.tensor_tensor(out=o, in0=v3("kmod", ps_), in1=v3("e2", ps_), op=ALU.mult), A["kmod"].res + A["e2"].res)
            yield
            padw("B", lambda e, o, ps_: e.tensor_tensor(out=o, in0=v3("b", ps_), in1=v3("t2", ps_), op=ALU.mult), A["b"].res + A["t2"].res)
            padw("K", lambda e, o, ps_: e.tensor_tensor(out=o, in0=v3("kmod", ps_), in1=v3("t2", ps_), op=ALU.mult), A["kmod"].res + A["t2"].res)
            yield
            padw("V", lambda e, o, ps_: e.tensor_copy(out=o, in_=v3("v", ps_)), A["v"].res)
            e13 = e1t[:].rearrange("p (c i) -> p c i", i=64)

        items = [(j, tb) for j in range(8) for tb in range(NBK)]
        for g_ in prep_gen(items[0][0], items[0][1], D2[0], P2[0], E1[0], GB3[0]):
            pass
        for it_, (j, tb) in enumerate(items):
            if True:
                t0 = tb * T
                tsl = slice(t0, t0 + T)
                Dd = D2[it_ % 2]
                gens = [quad_gen(qi, Dd, E1[it_ % 2], P2[it_ % 2]) for qi in range(NQ)]
                if pending is not None:
                    gens.append(seq_post(pending))
                if it_ + 1 < len(items):
                    jn, tbn = items[it_ + 1]
                    gens.append(prep_gen(jn, tbn, D2[(it_ + 1) % 2], P2[(it_ + 1) % 2], E1[(it_ + 1) % 2], GB3[(it_ + 1) % 3]))
                while gens:
                    for g_ in list(gens):
                        try:
                            next(g_)
                        except StopIteration:
                            gens.remove(g_)
                pending = (Dd, j, tsl, tb == 0, GB3[it_ % 3])
        for g_ in seq_post(pending):
            pass
        sch.release(m)

    def load_x(self):
        sch, TG = self.sch, self.TG
        for tg in range(self.NTG):
            ts = slice(tg * TG, (tg + 1) * TG)
            sch.dma("sp", self.xcur[:, :, ts], self.xT.rearrange("(c p) s -> c p s", p=128)[:, :, ts], writes=[self.xcur_res[tg]])

    def finish(self):
        sch = self.sch
        sch.barrier()
        fo = [d for d in sch.dma_last if d is not None]
        sch.emit(fo)
        return self.nc


def make_in_maps(inp, n_cores=8):
    f = lambda a: np.ascontiguousarray(np.asarray(a, np.float32))
    vecs = np.stack([pack_vecs(inp, l) for l in range(DEPTH)])
    shared = {
        "vecs": vecs, "w_in": f(inp["w_in"]), "w_vres": f(inp["w_vres_down"][0]),
        "w_up": f(inp["rwkv_w_up"]), "a_up": f(inp["rwkv_a_up"]), "g_up": f(inp["rwkv_g_up"]),
        "v_up": f(inp["rwkv_v_up"][0]), "w_uq": f(inp["mla_w_uq"]), "w_ukv": f(inp["mla_w_ukv"]),
        "w_branch": f(inp["w_branch"]), "w_out": f(inp["w_out"]), "w_ffn_in": f(inp["w_ffn_in"]),
        "w_ffn_out": f(inp["w_ffn_out"]),
    }
    x = np.asarray(inp["x"], np.float32)
    pos = np.asarray(inp["positions"], np.int32)
    maps = []
    for b in range(n_cores):
        m = dict(shared)
        m["xT"] = np.ascontiguousarray(x[b].T)
        m["pos"] = np.ascontiguousarray(pos[b])
        maps.append(m)
    return maps


def build_program(S=S_LEN, nlayers=DEPTH, debug=()):
    B = Builder(S, nlayers, debug)
    B.load_x()
    B.stage_rope_tables()
    for l in range(nlayers):
        B.stage_inproj(l)
        B.stage_hgrn(l)
        B.stage_rwkv(l)
        B.stage_mla(l)
        B.stage_merge(l)
        B.stage_ffn(l, l == nlayers - 1)
    return B.finish(), B


def kernel(**inputs):
    x = np.asarray(inputs["x"])
    nb, S, _ = x.shape
    nc, _ = build_program(S, DEPTH)
    in_maps = make_in_maps(inputs, nb)
    res = run_bass_kernel_spmd(nc, in_maps, core_ids=list(range(nb)))
    out = np.stack([np.ascontiguousarray(np.asarray(r["outT"]).T) for r in res.results]).astype(np.float32)
    return out
```
